# Optimizing a Trainium2 kernel written in Bass

```python
import math
import jax
import jax.numpy as jnp
from jax import lax
import numpy as np

D_MODEL = 1024
BATCH = 16
SEQ = 4096
DEPTH = 1
DEC_BATCH = 128
DEC_SEQ = 8
PAST_LEN = 8192
PAGE_SIZE = 128

A_HEADS = 8
A_HEAD_DIM = 64
A_WIDTH = A_HEADS * A_HEAD_DIM
DILATED_PATTERNS = ((128, 1), (512, 4), (2048, 16))
MAX_WINDOW = max(w for w, _ in DILATED_PATTERNS)
ATTN_SCALE = A_HEAD_DIM ** -0.5
B_HEADS = 4
B_KEY_DIM = 128
B_VAL_DIM = 128
B_WIDTH = B_HEADS * B_VAL_DIM
CONV_WIDTH = 4
CONV_DIM = B_HEADS * (2 * B_KEY_DIM + B_VAL_DIM)
CHUNK = 64
MIX_WIDTH = A_WIDTH + B_WIDTH
IN_SPLITS = (A_WIDTH, 2 * A_WIDTH, 3 * A_WIDTH, 3 * A_WIDTH + CONV_DIM,
             3 * A_WIDTH + CONV_DIM + B_WIDTH, 3 * A_WIDTH + CONV_DIM + B_WIDTH + B_HEADS)
N_IN = 3 * A_WIDTH + CONV_DIM + B_WIDTH + 2 * B_HEADS
N_GROUPS = 4
EXPERTS_PER_GROUP = 4
N_EXPERTS = N_GROUPS * EXPERTS_PER_GROUP
TOP_K = 2
D_EXPERT = 512
RMS_EPS = 1e-6
NEG_INF = -1e30

kernel_name = 'dilated_attn_gdn_hier_moe_step'


def _rmsnorm(x, w):
    xf = x.astype(jnp.float32)
    y = xf * lax.rsqrt(jnp.mean(xf * xf, -1, keepdims=True) + RMS_EPS)
    return (y * w.astype(jnp.float32)).astype(x.dtype)


def _l2norm(x):
    return x * lax.rsqrt(jnp.sum(x * x, -1, keepdims=True) + RMS_EPS)


def _in_proj(x, norm1_w, w_in):
    p = _rmsnorm(x, norm1_w) @ w_in
    return jnp.split(p, IN_SPLITS, axis=-1)


def _a_heads(aq, ak, av, qnorm_w, knorm_w):
    b, s, _ = aq.shape
    r = lambda t: t.reshape(b, s, A_HEADS, A_HEAD_DIM)
    return _rmsnorm(r(aq), qnorm_w), _rmsnorm(r(ak), knorm_w), r(av)


def _attend(scores, mask, v, eq):
    s = jnp.where(mask, scores, NEG_INF)
    m = jnp.max(s, -1, keepdims=True)
    p = jnp.exp(s - m)
    l = jnp.sum(p, -1)
    o = jnp.einsum(eq, p, v.astype(jnp.float32)) / l[..., None]
    return o, m[..., 0] + jnp.log(l)


def _dilated_prompt(q, k, v, window, dilation):
    b, s, h, dh = q.shape
    L = s // dilation
    wc = window // dilation
    bd = b * dilation
    fold = lambda t: t.reshape(b, L, dilation, h, dh).transpose(0, 2, 1, 3, 4).reshape(bd, L, h, dh)
    nb = -(-L // wc)
    pad = nb * wc - L
    blk = lambda t: jnp.pad(fold(t), ((0, 0), (0, pad), (0, 0), (0, 0))).reshape(bd, nb, wc, h, dh)
    qb, kb, vb = blk(q), blk(k), blk(v)
    prev = lambda t: jnp.concatenate([jnp.zeros_like(t[:, :1]), t[:, :-1]], axis=1)
    kk = jnp.concatenate([prev(kb), kb], axis=2)
    vv = jnp.concatenate([prev(vb), vb], axis=2)
    scores = jnp.einsum('bnqhd,bnkhd->bnhqk', qb, kk).astype(jnp.float32) * ATTN_SCALE
    ki = jnp.arange(2 * wc)
    dist = (jnp.arange(wc) + wc)[:, None] - ki[None, :]
    band = (dist >= 0) & (dist <= wc)
    key_ok = (jnp.arange(nb)[:, None] > 0) | (ki[None, :] >= wc)
    mask = band[None] & key_ok[:, None, :]
    o, lse = _attend(scores, mask[None, :, None], vv, 'bnhqk,bnkhd->bnhqd')
    o = o.transpose(0, 1, 3, 2, 4).reshape(bd, nb * wc, h, dh)[:, :L]
    lse = lse.transpose(0, 1, 3, 2).reshape(bd, nb * wc, h)[:, :L]
    o = o.reshape(b, dilation, L, h, dh).transpose(0, 2, 1, 3, 4).reshape(b, s, h, dh)
    lse = lse.reshape(b, dilation, L, h).transpose(0, 2, 1, 3).reshape(b, s, h)
    return o, lse


def _dilated_sample(q, k_all, v_all, window, dilation, n_buf):
    t = q.shape[1]
    j = jnp.arange(window // dilation + 1)
    idx = n_buf + jnp.arange(t)[:, None] - j[None, :] * dilation
    valid = idx >= 0
    idx = jnp.maximum(idx, 0)
    kg = k_all[:, idx]
    vg = v_all[:, idx]
    scores = jnp.einsum('bthd,btjhd->bthj', q, kg).astype(jnp.float32) * ATTN_SCALE
    return _attend(scores, valid[None, :, None, :], vg, 'bthj,btjhd->bthd')


def _combine(parts):
    o = jnp.stack([p[0] for p in parts], 0)
    lse = jnp.stack([p[1] for p in parts], 0)
    w = jax.nn.softmax(lse, axis=0)
    out = jnp.sum(w[..., None] * o, axis=0)
    return out.reshape(out.shape[0], out.shape[1], A_WIDTH)


def _short_conv(x, state, conv_w):
    s = x.shape[1]
    xp = jnp.concatenate([state.astype(x.dtype), x], axis=1)
    y = xp[:, 0:s] * conv_w[0]
    for i in range(1, CONV_WIDTH):
        y = y + xp[:, i:i + s] * conv_w[i]
    return jax.nn.silu(y), xp[:, -(CONV_WIDTH - 1):]


def _gated_delta_chunked(q, k, v, g, beta, s0):
    b, L, h, dk = q.shape
    dv = v.shape[-1]
    n = -(-L // CHUNK)
    pad = n * CHUNK - L

    def blk(t):
        t = jnp.pad(t, ((0, 0), (0, pad)) + ((0, 0),) * (t.ndim - 2))
        t = t.reshape((b, n, CHUNK) + t.shape[2:])
        return jnp.moveaxis(t, 3, 1)

    qc, kc, vc, gc, bc = blk(q), blk(k), blk(v), blk(g), blk(beta)
    decay = jnp.cumsum(gc, axis=-1)
    ci = jnp.arange(CHUNK)
    lower = ci[:, None] >= ci[None, :]
    strict = ci[:, None] > ci[None, :]
    gam = jnp.exp(jnp.where(lower, decay[..., :, None] - decay[..., None, :], NEG_INF))
    kb = kc * bc[..., None]
    vb = vc * bc[..., None]
    a_mat = jnp.where(strict, jnp.einsum('bhncd,bhnjd->bhncj', kb, kc) * gam, 0.0)
    m = a_mat + jnp.eye(CHUNK, dtype=jnp.float32)
    u = lax.linalg.triangular_solve(m, vb, left_side=True, lower=True, unit_diagonal=True)
    w = lax.linalg.triangular_solve(m, kb * jnp.exp(decay)[..., None], left_side=True, lower=True,
                                    unit_diagonal=True)
    attn = jnp.where(lower, jnp.einsum('bhncd,bhnjd->bhncj', qc, kc) * gam, 0.0)
    qd = qc * jnp.exp(decay)[..., None]
    last = decay[..., -1]
    kd = kc * jnp.exp(last[..., None] - decay)[..., None]

    def step(S, xs):
        qd_n, kd_n, u_n, w_n, attn_n, last_n = xs
        v_new = u_n - jnp.einsum('bhcd,bhde->bhce', w_n, S)
        o = jnp.einsum('bhcd,bhde->bhce', qd_n, S) + jnp.einsum('bhcj,bhje->bhce', attn_n, v_new)
        S = S * jnp.exp(last_n)[..., None, None] + jnp.einsum('bhcd,bhce->bhde', kd_n, v_new)
        return S, o

    xs = tuple(jnp.moveaxis(t, 2, 0) for t in (qd, kd, u, w, attn, last))
    S, o = lax.scan(step, s0, xs)
    o = jnp.moveaxis(o, 0, 2).reshape(b, h, n * CHUNK, dv)[:, :, :L].transpose(0, 2, 1, 3)
    return o, S


def _mixer_b(qkv, z, b_logit, a_logit, conv_state, ssm_state, conv_w, a_log, dt_bias, onorm_w):
    b, s, _ = qkv.shape
    f32 = jnp.float32
    c, new_conv = _short_conv(qkv, conv_state, conv_w)
    q, k, v = jnp.split(c, [B_HEADS * B_KEY_DIM, 2 * B_HEADS * B_KEY_DIM], axis=-1)
    q = _l2norm(q.reshape(b, s, B_HEADS, B_KEY_DIM).astype(f32)) * (B_KEY_DIM ** -0.5)
    k = _l2norm(k.reshape(b, s, B_HEADS, B_KEY_DIM).astype(f32))
    v = v.reshape(b, s, B_HEADS, B_VAL_DIM).astype(f32)
    beta = jax.nn.sigmoid(b_logit.astype(f32))
    g = -jnp.exp(a_log.astype(f32)) * jax.nn.softplus(a_logit.astype(f32) + dt_bias.astype(f32))
    o, s_new = _gated_delta_chunked(q, k, v, g, beta, ssm_state.astype(f32))
    o = _rmsnorm(o, onorm_w) * jax.nn.silu(z.reshape(b, s, B_HEADS, B_VAL_DIM).astype(f32))
    return o.reshape(b, s, B_WIDTH).astype(qkv.dtype), new_conv, s_new


def _hier_moe(x, w_group, b_group, w_expert_router, b_expert_router, w_gate_up, w_down):
    b, s, d = x.shape
    f32 = jnp.float32
    xt = x.reshape(b * s, d)
    g_logit = (xt @ w_group).astype(f32) + b_group.astype(f32)
    g_prob = jax.nn.softmax(g_logit, axis=-1)
    gi = jnp.argmax(g_logit, axis=-1)
    g_sel = jnp.take_along_axis(g_prob, gi[:, None], axis=1)[:, 0]
    e_logit = jnp.einsum('nd,gde->nge', xt, w_expert_router).astype(f32) + b_expert_router.astype(f32)
    e_logit = jnp.take_along_axis(e_logit, gi[:, None, None], axis=1)[:, 0]
    tv, ti = lax.top_k(e_logit, TOP_K)
    tw = jax.nn.softmax(tv, axis=-1) * g_sel[:, None]
    eid = gi[:, None] * EXPERTS_PER_GROUP + ti
    gates = jnp.sum(jax.nn.one_hot(eid, N_EXPERTS, dtype=f32) * tw[..., None], axis=1)
    out = jnp.zeros((b * s, d), f32)
    for e in range(N_EXPERTS):
        gate, up = jnp.split(xt @ w_gate_up[e], 2, axis=-1)
        ye = (jax.nn.silu(gate) * up) @ w_down[e]
        out = out + gates[:, e:e + 1] * ye.astype(f32)
    return out.reshape(b, s, d).astype(x.dtype)


def _finish(x, o_a, o_b, w_out, norm2_w, w_group, b_group, w_expert_router, b_expert_router, w_gate_up, w_down):
    h = x + jnp.concatenate([o_a.astype(x.dtype), o_b.astype(x.dtype)], axis=-1) @ w_out
    return h + _hier_moe(_rmsnorm(h, norm2_w), w_group, b_group, w_expert_router, b_expert_router,
                         w_gate_up, w_down)


def setup_inputs(seed: int = 0) -> dict:
    key = jax.random.key(seed)
    ks = jax.random.split(key, 24)
    f32 = jnp.float32
    n_buf = min(MAX_WINDOW, PAST_LEN)
    nrm = lambda k, shape, scale: scale * jax.random.normal(k, shape, f32)
    dt = jnp.exp(jax.random.uniform(ks[10], (B_HEADS,), f32, math.log(1e-3), math.log(1e-1)))
    return {
        'x_prompt': nrm(ks[0], (BATCH, SEQ, D_MODEL), 1.0),
        'x_sample': nrm(ks[1], (DEC_BATCH, DEC_SEQ, D_MODEL), 1.0),
        'cache_win_k': nrm(ks[2], (DEC_BATCH, n_buf, A_HEADS, A_HEAD_DIM), 1.0),
        'cache_win_v': nrm(ks[3], (DEC_BATCH, n_buf, A_HEADS, A_HEAD_DIM), 1.0),
        'state_conv': nrm(ks[4], (DEC_BATCH, CONV_WIDTH - 1, CONV_DIM), 1.0),
        'state_ssm': nrm(ks[5], (DEC_BATCH, B_HEADS, B_KEY_DIM, B_VAL_DIM), 0.1),
        'norm1_w': 1.0 + nrm(ks[6], (D_MODEL,), 0.02),
        'w_in': nrm(ks[7], (D_MODEL, N_IN), D_MODEL ** -0.5),
        'qnorm_w': 1.0 + nrm(ks[8], (A_HEAD_DIM,), 0.02),
        'knorm_w': 1.0 + nrm(ks[9], (A_HEAD_DIM,), 0.02),
        'conv_w': nrm(ks[11], (CONV_WIDTH, CONV_DIM), CONV_WIDTH ** -0.5),
        'a_log': jnp.log(jax.random.uniform(ks[12], (B_HEADS,), f32, 1.0, 16.0)),
        'dt_bias': dt + jnp.log(-jnp.expm1(-dt)),
        'onorm_w': 1.0 + nrm(ks[13], (B_VAL_DIM,), 0.02),
        'w_out': nrm(ks[14], (MIX_WIDTH, D_MODEL), MIX_WIDTH ** -0.5),
        'norm2_w': 1.0 + nrm(ks[15], (D_MODEL,), 0.02),
        'w_group': nrm(ks[16], (D_MODEL, N_GROUPS), D_MODEL ** -0.5),
        'b_group': nrm(ks[17], (N_GROUPS,), 0.01),
        'w_expert_router': nrm(ks[18], (N_GROUPS, D_MODEL, EXPERTS_PER_GROUP), D_MODEL ** -0.5),
        'b_expert_router': nrm(ks[19], (N_GROUPS, EXPERTS_PER_GROUP), 0.01),
        'w_gate_up': nrm(ks[20], (N_EXPERTS, D_MODEL, 2 * D_EXPERT), D_MODEL ** -0.5),
        'w_down': nrm(ks[21], (N_EXPERTS, D_EXPERT, D_MODEL), D_EXPERT ** -0.5),
    }


def reference(x_prompt, x_sample, cache_win_k, cache_win_v, state_conv, state_ssm, norm1_w, w_in,
              qnorm_w, knorm_w, conv_w, a_log, dt_bias, onorm_w, w_out, norm2_w, w_group, b_group,
              w_expert_router, b_expert_router, w_gate_up, w_down):
    aq, ak, av, bqkv, bz, bb, ba = _in_proj(x_prompt, norm1_w, w_in)
    q, k, v = _a_heads(aq, ak, av, qnorm_w, knorm_w)
    o_a = _combine([_dilated_prompt(q, k, v, w, d) for (w, d) in DILATED_PATTERNS])
    b, s, _ = x_prompt.shape
    conv0 = jnp.zeros((b, CONV_WIDTH - 1, CONV_DIM), x_prompt.dtype)
    ssm0 = jnp.zeros((b, B_HEADS, B_KEY_DIM, B_VAL_DIM), jnp.float32)
    o_b, conv_prompt, ssm_prompt = _mixer_b(bqkv, bz, bb, ba, conv0, ssm0, conv_w, a_log, dt_bias, onorm_w)
    y_prompt = _finish(x_prompt, o_a, o_b, w_out, norm2_w, w_group, b_group, w_expert_router,
                       b_expert_router, w_gate_up, w_down)
    keep = min(MAX_WINDOW, s)
    win_k_prompt = k[:, s - keep:]
    win_v_prompt = v[:, s - keep:]

    aq, ak, av, bqkv, bz, bb, ba = _in_proj(x_sample, norm1_w, w_in)
    qs, ksn, vsn = _a_heads(aq, ak, av, qnorm_w, knorm_w)
    n_buf = cache_win_k.shape[1]
    k_all = jnp.concatenate([cache_win_k.astype(ksn.dtype), ksn], axis=1)
    v_all = jnp.concatenate([cache_win_v.astype(vsn.dtype), vsn], axis=1)
    o_a = _combine([_dilated_sample(qs, k_all, v_all, w, d, n_buf) for (w, d) in DILATED_PATTERNS])
    o_b, conv_sample, ssm_sample = _mixer_b(bqkv, bz, bb, ba, state_conv, state_ssm, conv_w, a_log,
                                            dt_bias, onorm_w)
    y_sample = _finish(x_sample, o_a, o_b, w_out, norm2_w, w_group, b_group, w_expert_router,
                       b_expert_router, w_gate_up, w_down)
    win_k_sample = ksn
    win_v_sample = vsn
    return (y_prompt, y_sample, win_k_prompt, win_v_prompt, conv_prompt, ssm_prompt,
            win_k_sample, win_v_sample, conv_sample, ssm_sample)
```

```python
import contextlib
import os
import numpy as np
import concourse.bass as bass
import concourse.mybir as mybir
from concourse.bass_utils import run_bass_kernel_spmd

F32 = mybir.dt.float32
BF16 = mybir.dt.bfloat16
AF = mybir.ActivationFunctionType
ALU = mybir.AluOpType
AX = mybir.AxisListType

EPOCH = 30000
ENGS = ("pe", "act", "dve", "pool", "sp")

SEQ = 4096
D = 1024
NIN = 3592
EPS = 1e-6
PATTERNS = (1, 4, 16)


class Prog:
    def __init__(self, nc):
        self.nc = nc
        self.ops = {e: [] for e in ENGS}
        self.count = {e: 0 for e in ENGS}
        self.dmacount = {}
        self.last_w = {}
        self.readers = {}
        self.waited = {e: {} for e in ENGS}
        self.pending = {e: {} for e in ENGS}
        self.semkeys = []
        self.semkey_set = set()
        self.excl = set()

    def _tok_sem(self, key):
        if key not in self.semkey_set:
            self.semkey_set.add(key)
            self.semkeys.append(key)

    def barrier(self):
        cur = {}
        for e in ENGS:
            c = self.count[e]
            if c:
                cur[("e", e, (c - 1) // EPOCH)] = (c - 1) % EPOCH + 1
        for k, c in self.dmacount.items():
            cur[("d", k)] = c
        for e in ENGS:
            p = self.pending[e]
            for k, v in cur.items():
                if p.get(k, 0) < v:
                    p[k] = v

    def op(self, eng, fn, reads=(), writes=(), dma=None):
        deps = dict(self.pending[eng])
        self.pending[eng] = {}
        writes = list(writes) + [r for r in reads if r in self.excl]

        def add(tok):
            if tok is None:
                return
            k, v = tok
            if deps.get(k, 0) < v:
                deps[k] = v

        for r in reads:
            add(self.last_w.get(r))
        for w in writes:
            add(self.last_w.get(w))
            for k, v in self.readers.get(w, {}).items():
                add((k, v))
        if dma is None:
            c = self.count[eng]
            self.count[eng] = c + 1
            tok = (("e", eng, c // EPOCH), c % EPOCH + 1)
        else:
            c = self.dmacount.get(dma, 0) + 16
            self.dmacount[dma] = c
            tok = (("d", dma), c)
        self._tok_sem(tok[0])
        waits = []
        wd = self.waited[eng]
        for k, v in deps.items():
            if eng == "pe" and k[0] == "e" and k[1] == "pe":
                continue
            if wd.get(k, 0) >= v:
                continue
            wd[k] = v
            waits.append((k, v))
        self.ops[eng].append((fn, waits, tok))
        for r in reads:
            d = self.readers.setdefault(r, {})
            if d.get(tok[0], 0) < tok[1]:
                d[tok[0]] = tok[1]
        for w in writes:
            self.last_w[w] = tok
            self.readers[w] = {}
        return tok

    def emit(self):
        nc = self.nc
        with contextlib.ExitStack() as es:
            sems = {}
            for i, k in enumerate(self.semkeys):
                sems[k] = es.enter_context(nc.semaphore("s%d" % i))
            final = []
            for e in ENGS:
                c = self.count[e]
                if c:
                    final.append((("e", e, (c - 1) // EPOCH), (c - 1) % EPOCH + 1))
            for k, c in self.dmacount.items():
                final.append((("d", k), c))
            block = es.enter_context(nc.Block())

            def run(engname, e):
                for fn, waits, tok in self.ops[engname]:
                    for k, v in waits:
                        e.wait_ge(sems[k], v)
                    ins = fn(e)
                    ins.then_inc(sems[tok[0]], 16 if tok[0][0] == "d" else 1)
                if engname == "sp":
                    for k, v in final:
                        e.wait_ge(sems[k], v)

            @block.tensor
            def _(e):
                run("pe", e)

            @block.scalar
            def _(e):
                run("act", e)

            @block.vector
            def _(e):
                run("dve", e)

            @block.gpsimd
            def _(e):
                run("pool", e)

            @block.sync
            def _(e):
                run("sp", e)


class Rot:
    def __init__(self, alloc, name, shape, dtype, n):
        self.bufs = [alloc(name + str(i), shape, dtype) for i in range(n)]
        self.keys = [name + str(i) for i in range(n)]
        self.i = 0

    def next(self):
        j = self.i % len(self.bufs)
        self.i += 1
        return self.bufs[j], self.keys[j]


def build_program(n_pseq, n_sseq, stages=("A", "B", "D"), debug=False, moe_g=512, n_exp=16):
    nc = bass.Bass("TRN2", target_bir_lowering=False)
    NP = n_pseq * SEQ
    NS = n_sseq * 8
    NT = NP + NS
    assert NS <= 128
    dk = "ExternalOutput" if debug else "Internal"

    def din(name, shape):
        return nc.dram_tensor(name, list(shape), F32, kind="ExternalInput").ap()

    def dout(name, shape):
        return nc.dram_tensor(name, list(shape), F32, kind="ExternalOutput").ap()

    xp = din("xp", [NP, D])
    xs = din("xs", [max(NS, 1), D])
    cache_k = din("cache_k", [max(n_sseq, 1), 2048, 512])
    cache_v = din("cache_v", [max(n_sseq, 1), 2048, 512])
    state_conv = din("state_conv", [max(n_sseq, 1), 3, 1536])
    state_ssm = din("state_ssm", [max(n_sseq, 1), 4, 128, 128])
    norm1_w = din("norm1_w", [D])
    w_in = din("w_in", [D, NIN])
    qnorm_w = din("qnorm_w", [64])
    knorm_w = din("knorm_w", [64])
    conv_w = din("conv_w", [4, 1536])
    a_log = din("a_log", [4])
    dt_bias = din("dt_bias", [4])
    onorm_w = din("onorm_w", [128])
    w_out = din("w_out", [D, D])
    norm2_w = din("norm2_w", [D])
    w_group = din("w_group", [D, 4])
    b_group = din("b_group", [4])
    w_er = din("w_er", [4, D, 4])
    b_er = din("b_er", [16])
    w_gate_up = din("w_gate_up", [16, D, 1024])
    w_down = din("w_down", [16, 512, D])

    y_p = dout("y_p", [NP, D])
    y_s = dout("y_s", [max(NS, 1), D])
    wk_p = dout("wk_p", [n_pseq, 2048, 512])
    wv_p = dout("wv_p", [n_pseq, 2048, 512])
    conv_p = dout("conv_p", [n_pseq, 3, 1536])
    ssm_p = dout("ssm_p", [n_pseq, 4, 128, 128])
    wk_s = dout("wk_s", [max(NS, 1), 512])
    wv_s = dout("wv_s", [max(NS, 1), 512])
    conv_s = dout("conv_s", [max(n_sseq, 1), 3, 1536])
    ssm_s = dout("ssm_s", [max(n_sseq, 1), 4, 128, 128])

    qT_d = nc.dram_tensor("qT_d", [4, 128, NT], BF16, kind=dk).ap()
    kT_d = nc.dram_tensor("kT_d", [4, 128, NT], BF16, kind=dk).ap()
    V_d = nc.dram_tensor("V_d", [NT, 640], BF16, kind=dk).ap()
    gT_d = nc.dram_tensor("gT_d", [12, 128, NT], BF16, kind=dk).ap()
    zbg_d = nc.dram_tensor("zbg_d", [NT, 520], F32, kind=dk).ap()
    oT_d = nc.dram_tensor("oT_d", [8, 128, NT], BF16, kind=dk).ap()

    wgu_bf = nc.dram_tensor("wgu_bf", [16, D, 1024], BF16, kind="Internal").ap()
    wd_bf = nc.dram_tensor("wd_bf", [16, 512, D], BF16, kind="Internal").ap()

    P = Prog(nc)
    es = contextlib.ExitStack()
    if "D" in stages:
        for ex in range(16):
            P.op("pool", lambda e, ex=ex: e.dma_start(out=wgu_bf[ex], in_=w_gate_up[ex]), writes=["wbf"], dma="wbf")
            P.op("pool", lambda e, ex=ex: e.dma_start(out=wd_bf[ex], in_=w_down[ex]), writes=["wbf"], dma="wbf")
    if debug:
        dbg_g = nc.dram_tensor("dbg_g", [NT // 128, 128, 16], F32, kind="ExternalOutput").ap()
        dbg_hn = nc.dram_tensor("dbg_hn", [8, 128, NT], BF16, kind="ExternalOutput").ap()
        dbg_h = nc.dram_tensor("dbg_h", [NT, D], F32, kind="ExternalOutput").ap()
        xdump = nc.dram_tensor("xdump", [NT // 512, 128, 8, 512], BF16, kind="ExternalOutput").ap()

    def sb(name, shape, dtype, stack=None):
        return (stack or es).enter_context(nc.sbuf_tensor(name, list(shape), dtype))

    def ps(name, shape, dtype, stack=None):
        P.excl.add(name)
        return (stack or es).enter_context(nc.psum_tensor(name, list(shape), dtype))

    ident = sb("ident", [128, 128], BF16)
    P.op("pool", lambda e: e.memset(ident[:], 0.0), writes=["ident"])
    P.op("pool", lambda e: e.affine_select(out=ident[:], in_=ident[:], pattern=[[-1, 128]],
                                           compare_op=ALU.not_equal, fill=1.0, base=0,
                                           channel_multiplier=1), reads=["ident"], writes=["ident"])
    n1col = sb("n1col", [128, 8], F32)
    n2col = sb("n2col", [128, 8], F32)
    identf = sb("identf", [128, 128], F32)
    P.op("pool", lambda e: e.memset(identf[:], 0.0), writes=["identf"])
    P.op("pool", lambda e: e.affine_select(out=identf[:], in_=identf[:], pattern=[[-1, 128]],
                                           compare_op=ALU.not_equal, fill=1.0, base=0,
                                           channel_multiplier=1), reads=["identf"], writes=["identf"])
    nrow = sb("nrow", [8, 2, 128], F32)
    P.op("sp", lambda e: e.dma_start(out=nrow[:, 0, :], in_=norm1_w.rearrange("(k p) -> k p", p=128)), writes=["nrow"], dma="c0")
    P.op("sp", lambda e: e.dma_start(out=nrow[:, 1, :], in_=norm2_w.rearrange("(k p) -> k p", p=128)), writes=["nrow"], dma="c0")
    with nc.psum_tensor("pN", [128, 16], F32) as pN:
        P.op("pe", lambda e: e.matmul(out=pN[:, 0:8], lhsT=nrow[:, 0, :], rhs=identf[0:8, 0:8], start=True, stop=True), reads=["nrow", "identf"], writes=["pN"])
        P.op("pe", lambda e: e.matmul(out=pN[:, 8:16], lhsT=nrow[:, 1, :], rhs=identf[0:8, 0:8], start=True, stop=True), reads=["nrow", "identf"], writes=["pN"])
        P.op("dve", lambda e: e.tensor_copy(out=n1col[:], in_=pN[:, 0:8]), reads=["pN"], writes=["n1col"])
        P.op("dve", lambda e: e.tensor_copy(out=n2col[:], in_=pN[:, 8:16]), reads=["pN"], writes=["n2col"])
    P.barrier()

    tiles = []
    for t in range(NP // 128):
        tiles.append(("p", t))
    if NS:
        tiles.append(("s", 0))

    def x_rows(kind, t, n=128):
        if kind == "p":
            return xp[t * 128:t * 128 + n, :]
        return xs[0:n, :]

    def gtok(kind, t):
        return t * 128 if kind == "p" else NP

    if "A" in stages:
        with contextlib.ExitStack() as pa:
            A = lambda name, shape, dtype: sb(name, shape, dtype, pa)
            win = A("win", [128, 8, NIN], BF16)
            wst_r = Rot(A, "wst", [128, NIN], F32, 2)
            for k in range(8):
                wst, wstk = wst_r.next()
                P.op("sp", lambda e, k=k, wst=wst: e.dma_start(out=wst[:], in_=w_in[k * 128:(k + 1) * 128, :]), writes=[wstk], dma=wstk)
                P.op("pool" if k % 2 else "dve", lambda e, k=k, wst=wst: e.tensor_copy(out=win[:, k, :], in_=wst[:]), reads=[wstk], writes=[("win", k)])
            winr = [("win", k) for k in range(8)]
            qw_rep = A("qw_rep", [128, 8, 64], F32)
            kw_rep = A("kw_rep", [128, 8, 64], F32)
            for h in range(8):
                P.op("sp", lambda e, h=h: e.dma_start(out=qw_rep[:, h, :], in_=qnorm_w.partition_broadcast(128)),
                     writes=["qw_rep"], dma="qwr")
                P.op("sp", lambda e, h=h: e.dma_start(out=kw_rep[:, h, :], in_=knorm_w.partition_broadcast(128)),
                     writes=["kw_rep"], dma="kwr")
            qwr = ["qw_rep"]
            kwr = ["kw_rep"]

            xt_r = Rot(A, "xt", [128, D], F32, 2)
            junk = A("junk", [128, D], BF16)
            ssq_r = Rot(A, "ssq", [128, 1], F32, 2)
            xn_r = Rot(A, "xn", [128, D], BF16, 2)
            xnT_r = Rot(A, "xnT", [128, 8, 512], BF16, 2)
            sq_r = Rot(A, "sq", [128, 512], F32, 2)
            ss8_r = Rot(A, "ss8", [128, 8], F32, 2)
            qn32_r = Rot(A, "qn32", [128, 8, 64], F32, 2)
            kn32_r = Rot(A, "kn32", [128, 8, 64], F32, 2)
            qnb_r = Rot(A, "qnb", [128, 512], BF16, 2)
            v32_r = Rot(A, "v32", [128, 512], F32, 2)
            vext_r = Rot(A, "vext", [128, 8, 80], BF16, 2)
            for vb, vk in zip(vext_r.bufs, vext_r.keys):
                P.op("pool", lambda e, vb=vb: e.memset(vb[:], 1.0), writes=[vk])
            zbg_r = Rot(A, "zbg", [128, 520], F32, 2)
            qT_r = Rot(A, "qTs", [128, 4, 512], BF16, 2)
            kT_r = Rot(A, "kTs", [128, 4, 512], BF16, 2)
            gT_r = Rot(A, "gTs", [128, 12, 512], BF16, 2)
            cv_r = Rot(A, "cv", [128, 1536], F32, 1)

            pp = contextlib.ExitStack()
            pT_r = Rot(lambda n, s, d: ps(n, s, d, pp), "pT", [128, 8, 128], BF16, 2)
            pM_r = Rot(lambda n, s, d: ps(n, s, d, pp), "pM", [128, 512], F32, 4)
            pS_r = Rot(lambda n, s, d: ps(n, s, d, pp), "pS", [128, 512], F32, 1)

            def rstd_from(ssq, key, scale):
                P.op("dve", lambda e: e.tensor_scalar(out=ssq, in0=ssq, scalar1=scale, scalar2=EPS,
                                                      op0=ALU.mult, op1=ALU.add), reads=[key], writes=[key])
                P.op("act", lambda e: e.activation(out=ssq, in_=ssq, func=AF.Sqrt), reads=[key], writes=[key])
                P.op("dve", lambda e: e.reciprocal(out=ssq, in_=ssq), reads=[key], writes=[key])

            def qk_norm(pm, pmk, wrep, wrk, out32, out32k):
                sq, sqk = sq_r.next()
                ss8, ss8k = ss8_r.next()
                P.op("act", lambda e: e.activation(out=sq[:], in_=pm[:], func=AF.Square), reads=[pmk], writes=[sqk])
                P.op("dve", lambda e: e.tensor_reduce(out=ss8[:], in_=sq[:].rearrange("p (h d) -> p h d", d=64),
                                                      axis=AX.X, op=ALU.add), reads=[sqk], writes=[ss8k])
                rstd_from(ss8[:], ss8k, 1.0 / 64)
                P.op("dve", lambda e: e.tensor_tensor(out=out32[:], in0=pm[:].rearrange("p (h d) -> p h d", d=64),
                                                      in1=ss8[:].unsqueeze(2).to_broadcast([128, 8, 64]), op=ALU.mult),
                     reads=[pmk, ss8k], writes=[out32k])
                P.op("pool", lambda e: e.tensor_tensor(out=out32[:], in0=out32[:], in1=wrep[:], op=ALU.mult),
                     reads=[out32k] + wrk, writes=[out32k])

            sts = []
            for s in range(n_pseq):
                for u in range(SEQ // 512):
                    sts.append([("p", s * 32 + u * 4 + j) for j in range(4)])
            if NS:
                sts.append([("s", 0)])
            if os.environ.get("MAXST"):
                sts = sts[-int(os.environ["MAXST"]):]
            for st in sts:
                nt = len(st)
                N = nt * 128
                g0 = gtok(*st[0])
                xnT, xnTk = xnT_r.next()
                qTs, qTk = qT_r.next()
                kTs, kTk = kT_r.next()
                for j, (kind, t) in enumerate(st):
                    nrows = 128 if kind == "p" else NS
                    xt, xtk = xt_r.next()
                    if nrows < 128:
                        P.op("pool", lambda e, xt=xt: e.memset(xt[:], 0.0), writes=[xtk])
                    P.op("sp", lambda e, xt=xt, kind=kind, t=t, nrows=nrows: e.dma_start(out=xt[0:nrows, :], in_=x_rows(kind, t, nrows)),
                         writes=[xtk], dma=xtk)
                    ssq, ssqk = ssq_r.next()
                    P.op("act", lambda e, xt=xt, ssq=ssq: e.activation(out=junk[:], in_=xt[:], func=AF.Square, accum_out=ssq[:]),
                         reads=[xtk], writes=["junk", ssqk])
                    rstd_from(ssq[:], ssqk, 1.0 / D)
                    xn, xnk = xn_r.next()
                    P.op("dve", lambda e, xn=xn, xt=xt, ssq=ssq: e.tensor_scalar(out=xn[:], in0=xt[:], scalar1=ssq[:], scalar2=None, op0=ALU.mult),
                         reads=[xtk, ssqk], writes=[xnk])
                    pT, pTk = pT_r.next()
                    for k in range(8):
                        P.op("pe", lambda e, pT=pT, xn=xn, k=k: e.transpose(out=pT[:, k, :], in_=xn[:, k * 128:(k + 1) * 128], identity=ident[:]),
                             reads=[xnk, "ident"], writes=[pTk])
                    P.op("dve", lambda e, pT=pT, xnT=xnT, j=j: e.tensor_tensor(
                        out=xnT[:, :, j * 128:(j + 1) * 128], in0=pT[:],
                        in1=n1col[:].unsqueeze(2).to_broadcast([128, 8, 128]), op=ALU.mult),
                        reads=[pTk, "n1col"], writes=[(xnTk, j)])
                    xr = [(xnTk, j)]
                    sl = slice(j * 128, (j + 1) * 128)
                    gt = gtok(kind, t)
                    CUT = int(os.environ.get("CUT", "9"))
                    if CUT < 2:
                        continue
                    pm, pmk = pM_r.next()
                    for k in range(8):
                        P.op("pe", lambda e, pm=pm, xnT=xnT, k=k, sl=sl: e.matmul(out=pm[:], lhsT=xnT[:, k, sl], rhs=win[:, k, 0:512], start=(k == 0), stop=(k == 7)),
                             reads=xr + winr, writes=[pmk])
                    qn32, qn32k = qn32_r.next()
                    qk_norm(pm, pmk, qw_rep, qwr, qn32, qn32k)
                    qnb, qnbk = qnb_r.next()
                    P.op("act", lambda e, qnb=qnb, qn32=qn32: e.copy(out=qnb[:], in_=qn32[:].rearrange("p h d -> p (h d)")), reads=[qn32k], writes=[qnbk])
                    pT, pTk = pT_r.next()
                    for c in range(4):
                        P.op("pe", lambda e, pT=pT, qnb=qnb, c=c: e.transpose(out=pT[:, c, :], in_=qnb[:, c * 128:(c + 1) * 128], identity=ident[:]),
                             reads=[qnbk, "ident"], writes=[pTk])
                    P.op("act", lambda e, pT=pT, qTs=qTs, sl=sl: e.copy(out=qTs[:, :, sl], in_=pT[:, 0:4, :]), reads=[pTk], writes=[(qTk, j)])
                    if CUT < 3:
                        continue
                    pm, pmk = pM_r.next()
                    for k in range(8):
                        P.op("pe", lambda e, pm=pm, xnT=xnT, k=k, sl=sl: e.matmul(out=pm[:], lhsT=xnT[:, k, sl], rhs=win[:, k, 512:1024], start=(k == 0), stop=(k == 7)),
                             reads=xr + winr, writes=[pmk])
                    kn32, kn32k = kn32_r.next()
                    qk_norm(pm, pmk, kw_rep, kwr, kn32, kn32k)
                    if kind == "p":
                        pos = (t * 128) % SEQ
                        if pos >= SEQ - 2048:
                            sidx = (t * 128) // SEQ
                            P.op("sp", lambda e, kn32=kn32, sidx=sidx, pos=pos: e.dma_start(
                                out=wk_p[sidx, pos - (SEQ - 2048):pos - (SEQ - 2048) + 128, :], in_=kn32[:].rearrange("p h d -> p (h d)")),
                                reads=[kn32k], dma=kn32k)
                    elif not os.environ.get("NOWK"):
                        P.op("sp", lambda e, kn32=kn32: e.dma_start(out=wk_s[0:NS, :], in_=kn32[0:NS].rearrange("p h d -> p (h d)")),
                             reads=[kn32k], dma=kn32k)
                    qnb, qnbk = qnb_r.next()
                    P.op("act", lambda e, qnb=qnb, kn32=kn32: e.copy(out=qnb[:], in_=kn32[:].rearrange("p h d -> p (h d)")), reads=[kn32k], writes=[qnbk])
                    pT, pTk = pT_r.next()
                    for c in range(4):
                        P.op("pe", lambda e, pT=pT, qnb=qnb, c=c: e.transpose(out=pT[:, c, :], in_=qnb[:, c * 128:(c + 1) * 128], identity=ident[:]),
                             reads=[qnbk, "ident"], writes=[pTk])
                    P.op("act", lambda e, pT=pT, kTs=kTs, sl=sl: e.copy(out=kTs[:, :, sl], in_=pT[:, 0:4, :]), reads=[pTk], writes=[(kTk, j)])
                    if CUT < 4:
                        continue
                    pm, pmk = pM_r.next()
                    for k in range(8):
                        P.op("pe", lambda e, pm=pm, xnT=xnT, k=k, sl=sl: e.matmul(out=pm[:], lhsT=xnT[:, k, sl], rhs=win[:, k, 1024:1536], start=(k == 0), stop=(k == 7)),
                             reads=xr + winr, writes=[pmk])
                    v32, v32k = v32_r.next()
                    P.op("act", lambda e, v32=v32, pm=pm: e.copy(out=v32[:], in_=pm[:]), reads=[pmk], writes=[v32k])
                    vext, vextk = vext_r.next()
                    P.op("dve", lambda e, vext=vext, v32=v32: e.tensor_copy(out=vext[:, :, 0:64], in_=v32[:].rearrange("p (h d) -> p h d", d=64)),
                         reads=[v32k], writes=[vextk])
                    if kind == "p":
                        pos = (t * 128) % SEQ
                        if pos >= SEQ - 2048:
                            sidx = (t * 128) // SEQ
                            P.op("sp", lambda e, v32=v32, sidx=sidx, pos=pos: e.dma_start(
                                out=wv_p[sidx, pos - (SEQ - 2048):pos - (SEQ - 2048) + 128, :], in_=v32[:]),
                                reads=[v32k], dma=v32k)
                    else:
                        P.op("sp", lambda e, v32=v32: e.dma_start(out=wv_s[0:NS, :], in_=v32[0:NS, :]), reads=[v32k], dma=v32k)
                    P.op("sp", lambda e, vext=vext, gt=gt: e.dma_start(out=V_d[gt:gt + 128, :], in_=vext[:].rearrange("p h d -> p (h d)")),
                         reads=[vextk], dma=vextk)
                    if CUT < 5:
                        continue
                    pm, pmk = pM_r.next()
                    for k in range(8):
                        P.op("pe", lambda e, pm=pm, xnT=xnT, k=k, sl=sl: e.matmul(out=pm[:], lhsT=xnT[:, k, sl], rhs=win[:, k, 3072:3584], start=(k == 0), stop=(k == 7)),
                             reads=xr + winr, writes=[pmk])
                    pS, pSk = pS_r.next()
                    for k in range(8):
                        P.op("pe", lambda e, pS=pS, xnT=xnT, k=k, sl=sl: e.matmul(out=pS[:, 0:8], lhsT=xnT[:, k, sl], rhs=win[:, k, 3584:3592], start=(k == 0), stop=(k == 7)),
                             reads=xr + winr, writes=[pSk])
                    zbg, zbgk = zbg_r.next()
                    P.op("act", lambda e, zbg=zbg, pm=pm: e.copy(out=zbg[:, 0:512], in_=pm[:]), reads=[pmk], writes=[(zbgk, 0)])
                    P.op("dve", lambda e, zbg=zbg, pS=pS: e.tensor_copy(out=zbg[:, 512:520], in_=pS[:, 0:8]), reads=[pSk], writes=[(zbgk, 1)])
                    P.op("sp", lambda e, zbg=zbg, gt=gt: e.dma_start(out=zbg_d[gt:gt + 128, :], in_=zbg[:]),
                         reads=[(zbgk, 0), (zbgk, 1)], dma=zbgk)
                    if CUT < 6:
                        continue
                    is_last = (kind == "p" and (t * 128) % SEQ == SEQ - 128) or kind == "s"
                    if is_last:
                        cv, cvk = cv_r.next()
                        for c3 in range(3):
                            pm, pmk = pM_r.next()
                            for k in range(8):
                                P.op("pe", lambda e, pm=pm, xnT=xnT, k=k, sl=sl, c3=c3: e.matmul(
                                    out=pm[:], lhsT=xnT[:, k, sl], rhs=win[:, k, 1536 + c3 * 512:1536 + (c3 + 1) * 512], start=(k == 0), stop=(k == 7)),
                                    reads=xr + winr, writes=[pmk])
                            P.op("act", lambda e, cv=cv, pm=pm, c3=c3: e.copy(out=cv[:, c3 * 512:(c3 + 1) * 512], in_=pm[:]), reads=[pmk], writes=[(cvk, c3)])
                        cvr = [(cvk, c3) for c3 in range(3)]
                        if kind == "p":
                            sidx = (t * 128) // SEQ
                            P.op("sp", lambda e, cv=cv, sidx=sidx: e.dma_start(out=conv_p[sidx], in_=cv[125:128, :]), reads=cvr, dma=cvk)
                        else:
                            for b in range(n_sseq):
                                P.op("sp", lambda e, cv=cv, b=b: e.dma_start(out=conv_s[b], in_=cv[b * 8 + 5:b * 8 + 8, :]), reads=cvr, dma=cvk)
                if CUT < 7:
                    continue
                gTs, gTk = gT_r.next()
                for j in range(nt):
                    sl = slice(j * 128, (j + 1) * 128)
                    for c4 in range(3):
                        pm, pmk = pM_r.next()
                        for cc in range(4):
                            c = c4 * 4 + cc
                            for k in range(8):
                                P.op("pe", lambda e, pm=pm, xnT=xnT, k=k, c=c, cc=cc, sl=sl: e.matmul(
                                    out=pm[:, cc * 128:(cc + 1) * 128], lhsT=win[:, k, 1536 + c * 128:1536 + (c + 1) * 128], rhs=xnT[:, k, sl], start=(k == 0), stop=(k == 7)),
                                    reads=[(xnTk, j)] + winr, writes=[pmk])
                        if c4 % 2 == 0:
                            P.op("act", lambda e, pm=pm, gTs=gTs, c4=c4, sl=sl: e.copy(out=gTs[:, c4 * 4:(c4 + 1) * 4, sl], in_=pm[:].rearrange("p (c n) -> p c n", n=128)), reads=[pmk], writes=[(gTk, c4, j)])
                        else:
                            P.op("dve", lambda e, pm=pm, gTs=gTs, c4=c4, sl=sl: e.tensor_copy(out=gTs[:, c4 * 4:(c4 + 1) * 4, sl], in_=pm[:].rearrange("p (c n) -> p c n", n=128)), reads=[pmk], writes=[(gTk, c4, j)])
                if debug and os.environ.get("SNAP") and N == 512:
                    P.op("sp", lambda e, xnT=xnT, g0=g0: e.dma_start(out=xdump[g0 // 512], in_=xnT[:]), reads=[(xnTk, j) for j in range(nt)], dma="xdump")
                P.op("sp", lambda e, gTs=gTs, g0=g0, N=N: e.dma_start(out=gT_d[:, :, g0:g0 + N].rearrange("c p n -> p c n"), in_=gTs[:, :, 0:N]),
                     reads=[(gTk, c4, j) for c4 in range(3) for j in range(nt)], dma=gTk)
                P.op("sp", lambda e, qTs=qTs, g0=g0, N=N: e.dma_start(out=qT_d[:, :, g0:g0 + N].rearrange("c p n -> p c n"), in_=qTs[:, :, 0:N]),
                     reads=[(qTk, j) for j in range(nt)], dma=qTk)
                P.op("sp", lambda e, kTs=kTs, g0=g0, N=N: e.dma_start(out=kT_d[:, :, g0:g0 + N].rearrange("c p n -> p c n"), in_=kTs[:, :, 0:N]),
                     reads=[(kTk, j) for j in range(nt)], dma=kTk)
            pp.close()
        P.barrier()
        if debug and os.environ.get("SNAP"):
            snap = nc.dram_tensor("snap", [12, 128, NT], BF16, kind="ExternalOutput").ap()
            P.op("sp", lambda e: e.dma_start(out=snap, in_=gT_d), dma="snap")
            P.barrier()

    if "B" in stages:
        with contextlib.ExitStack() as pb:
            Bk = lambda name, shape, dtype: sb(name, shape, dtype, pb)
            mask = Bk("mask", [128, 256], BF16)
            P.op("pool", lambda e: e.memset(mask[:], 1.0), writes=["mask"])
            P.op("pool", lambda e: e.affine_select(out=mask[:, 0:128], in_=mask[:, 0:128], pattern=[[-1, 128]], compare_op=ALU.is_ge,
                                                   fill=0.0, base=0, channel_multiplier=1), reads=["mask"], writes=["mask"])
            P.op("pool", lambda e: e.affine_select(out=mask[:, 128:256], in_=mask[:, 128:256], pattern=[[1, 128]], compare_op=ALU.is_ge,
                                                   fill=0.0, base=0, channel_multiplier=-1), reads=["mask"], writes=["mask"])
            negm = Bk("negm", [128, 1], F32)
            P.op("pool", lambda e: e.memset(negm[:], -8.0), writes=["negm"])
            ones_r = Bk("ones_r", [65, 64], F32)
            P.op("pool", lambda e: e.memset(ones_r[:], 0.0), writes=["ones_r"])
            P.op("pool", lambda e: e.memset(ones_r[64:65, :], 1.0), reads=["ones_r"], writes=["ones_r"])
            qTb_r = Rot(Bk, "qTb", [128, SEQ], BF16, 2)
            kTb_r = Rot(Bk, "kTb", [128, SEQ], BF16, 2)
            vf_r = Rot(Bk, "vf", [128, 32, 2, 80], BF16, 2)
            acc = Bk("acc", [65, 2, SEQ], F32)
            rl = Bk("rl", [65, 2, SEQ], F32)
            P.op("pool", lambda e: e.memset(rl[:], 0.0), writes=["rl"])
            pt_r = Rot(Bk, "pt", [128, 2, 256], BF16, 4)
            oo_r = Rot(Bk, "oo", [64, SEQ], BF16, 2)
            pp = contextlib.ExitStack()
            pS_r = Rot(lambda n, s, d: ps(n, s, d, pp), "pSb", [128, 2, 512], F32, 3)
            pO_r = Rot(lambda n, s, d: ps(n, s, d, pp), "pOb", [128, 512], F32, 2)
            pB_r = pO_r
            for s in range(n_pseq):
                t0 = s * SEQ
                for hp in range(4):
                    qTb, qTbk = qTb_r.next()
                    kTb, kTbk = kTb_r.next()
                    P.op("sp", lambda e, qTb=qTb, hp=hp, t0=t0: e.dma_start(out=qTb[:], in_=qT_d[hp, :, t0:t0 + SEQ]), writes=[qTbk], dma=qTbk)
                    P.op("sp", lambda e, kTb=kTb, hp=hp, t0=t0: e.dma_start(out=kTb[:], in_=kT_d[hp, :, t0:t0 + SEQ]), writes=[kTbk], dma=kTbk)
                    for pi, d in enumerate(PATTERNS):
                        nb = 32 // d
                        vf, vfk = vf_r.next()
                        src = V_d[t0:t0 + SEQ, hp * 160:(hp + 1) * 160].rearrange("(b p r) f -> p r b f", p=128, r=d)
                        for r in range(d):
                            P.op("sp", lambda e, vf=vf, src=src, r=r, nb=nb: e.dma_start(
                                out=vf[:, r * nb:(r + 1) * nb, :, :].rearrange("p b h f -> p b (h f)"), in_=src[:, r, :, :]),
                                writes=[vfk], dma=vfk)
                        for r in range(d):
                            for b in range(nb):
                                c0 = r + 128 * b * d
                                qc = slice(c0, c0 + 127 * d + 1, d)
                                pc = slice(c0 - 128 * d, c0 - d + 1, d)
                                pS, pSk = pS_r.next()
                                for hh in range(2):
                                    rs = slice(64 * hh, 64 * hh + 64)
                                    if b > 0:
                                        P.op("pe", lambda e, pS=pS, hh=hh, rs=rs, pc=pc, qc=qc, kTb=kTb, qTb=qTb: e.matmul(
                                            out=pS[:, hh, 0:128], lhsT=kTb[rs, pc], rhs=qTb[rs, qc], start=True, stop=True),
                                            reads=[kTbk, qTbk], writes=[pSk])
                                    P.op("pe", lambda e, pS=pS, hh=hh, rs=rs, qc=qc, kTb=kTb, qTb=qTb: e.matmul(
                                        out=pS[:, hh, 128:256], lhsT=kTb[rs, qc], rhs=qTb[rs, qc], start=True, stop=True),
                                        reads=[kTbk, qTbk], writes=[pSk])
                                pt, ptk = pt_r.next()
                                cs = slice(0, 256) if b > 0 else slice(128, 256)
                                P.op("act", lambda e, pt=pt, pS=pS, cs=cs: e.activation(out=pt[:, :, cs], in_=pS[:, :, cs], func=AF.Exp, bias=negm[:], scale=0.125),
                                     reads=[pSk, "negm"], writes=[ptk])
                                ncs = 256 if b > 0 else 128
                                P.op("dve", lambda e, pt=pt, cs=cs, ncs=ncs: e.tensor_tensor(
                                    out=pt[:, :, cs], in0=pt[:, :, cs], in1=mask[:, cs].unsqueeze(1).to_broadcast([128, 2, ncs]), op=ALU.mult),
                                    reads=[ptk, "mask"], writes=[ptk])
                                pO, pOk = pO_r.next()
                                for hh in range(2):
                                    if b > 0:
                                        P.op("pe", lambda e, pO=pO, hh=hh, vf=vf, pt=pt, r=r, b=b, nb=nb: e.matmul(
                                            out=pO[0:65, hh * 128:(hh + 1) * 128], lhsT=vf[:, r * nb + b - 1, hh, 0:65], rhs=pt[:, hh, 0:128], start=True, stop=False),
                                            reads=[vfk, ptk], writes=[pOk])
                                    P.op("pe", lambda e, pO=pO, hh=hh, vf=vf, pt=pt, r=r, b=b, nb=nb: e.matmul(
                                        out=pO[0:65, hh * 128:(hh + 1) * 128], lhsT=vf[:, r * nb + b, hh, 0:65], rhs=pt[:, hh, 128:256], start=(b == 0), stop=True),
                                        reads=[vfk, ptk], writes=[pOk])
                                if pi == 0:
                                    P.op("dve", lambda e, pO=pO, qc=qc: e.tensor_copy(out=acc[:, :, qc], in_=pO[0:65, 0:256].rearrange("p (h q) -> p h q", h=2)), reads=[pOk], writes=["acc"])
                                else:
                                    P.op("dve", lambda e, pO=pO, qc=qc: e.tensor_tensor(out=acc[:, :, qc], in0=acc[:, :, qc], in1=pO[0:65, 0:256].rearrange("p (h q) -> p h q", h=2), op=ALU.add),
                                         reads=[pOk, "acc"], writes=["acc"])
                    P.op("dve", lambda e: e.reciprocal(out=rl[64:65, :, :], in_=acc[64:65, :, :]), reads=["acc"], writes=["rl"])
                    for hh in range(2):
                        oo, ook = oo_r.next()
                        for cb in range(SEQ // 512):
                            cs = slice(cb * 512, (cb + 1) * 512)
                            pB, pBk = pB_r.next()
                            P.op("pe", lambda e, pB=pB, hh=hh, cs=cs: e.matmul(out=pB[0:64, :], lhsT=ones_r[:, :], rhs=rl[:, hh, cs], start=True, stop=True),
                                 reads=["rl", "ones_r"], writes=[pBk])
                            P.op("dve", lambda e, pB=pB, oo=oo, hh=hh, cs=cs: e.tensor_tensor(out=oo[:, cs], in0=acc[0:64, hh, cs], in1=pB[0:64, :], op=ALU.mult),
                                 reads=[pBk, "acc"], writes=[(ook, cb)])
                        h = 2 * hp + hh
                        P.op("sp", lambda e, oo=oo, h=h, t0=t0: e.dma_start(out=oT_d[h // 2, (h % 2) * 64:(h % 2) * 64 + 64, t0:t0 + SEQ], in_=oo[:]),
                             reads=[(ook, cb) for cb in range(SEQ // 512)], dma=ook)
            pp.close()
        P.barrier()

    if "E" in stages and NS:
        with contextlib.ExitStack() as pe_:
            Ek = lambda name, shape, dtype: sb(name, shape, dtype, pe_)
            pp = contextlib.ExitStack()
            pTe_r = Rot(lambda n, s_, d: ps(n, s_, d, pp), "pTe", [128, 4, 128], F32, 2)
            pSc = [ps("pSc%d" % hh, [128, 16, 4, 8], F32, pp) for hh in range(2)]
            pSn = [ps("pSn%d" % hh, [128, 512], F32, pp) for hh in range(2)]
            pOe_full = ps("pOe", [128, 512], F32, pp)
            pOe = pOe_full[0:65, 0:64].rearrange("p (h q) -> p h q", q=8)
            pBe_full = ps("pBe", [128, 512], F32, pp)
            pBe = pBe_full[0:64, 0:64]
            I32 = mybir.dt.int32
            Di = Ek("Di", [128, 17, 8], I32)
            Dt = Ek("Dt", [128, 17, 8], I32)
            Df = Ek("Df", [128, 17, 8], F32)
            cm1 = Ek("cm1", [128, 17, 8], F32)
            cm2 = Ek("cm2", [128, 17, 8], F32)
            cnt = Ek("cnt", [128, 17, 8], BF16)
            P.op("pool", lambda e: e.iota(Di[:], pattern=[[-128, 17], [1, 8]], base=2048, channel_multiplier=-1), writes=["Di"])
            P.op("dve", lambda e: e.tensor_copy(out=Df[:], in_=Di[:]), reads=["Di"], writes=["Df"])
            P.op("dve", lambda e: e.tensor_scalar(out=cm1[:], in0=Df[:], scalar1=128.0, scalar2=None, op0=ALU.is_le), reads=["Df"], writes=["cm1"])
            P.op("dve", lambda e: e.tensor_single_scalar(out=Dt[:], in_=Di[:], scalar=3, op=ALU.bitwise_and), reads=["Di"], writes=["Dt"])
            P.op("dve", lambda e: e.tensor_scalar(out=cm2[:], in0=Dt[:], scalar1=0.0, scalar2=None, op0=ALU.is_equal), reads=["Dt"], writes=["cm2"])
            P.op("dve", lambda e: e.scalar_tensor_tensor(out=cm2[:], in0=Df[:], scalar=512.0, in1=cm2[:], op0=ALU.is_le, op1=ALU.mult), reads=["Df", "cm2"], writes=["cm2"])
            P.op("dve", lambda e: e.tensor_tensor(out=cm1[:], in0=cm1[:], in1=cm2[:], op=ALU.add), reads=["cm1", "cm2"], writes=["cm1"])
            P.op("dve", lambda e: e.tensor_single_scalar(out=Dt[:], in_=Di[:], scalar=15, op=ALU.bitwise_and), reads=["Di", "Dt"], writes=["Dt"])
            P.op("dve", lambda e: e.tensor_scalar(out=cm2[:], in0=Dt[:], scalar1=0.0, scalar2=None, op0=ALU.is_equal), reads=["Dt", "cm2"], writes=["cm2"])
            P.op("dve", lambda e: e.tensor_tensor(out=cm1[:], in0=cm1[:], in1=cm2[:], op=ALU.add), reads=["cm1", "cm2"], writes=["cm1"])
            P.op("dve", lambda e: e.scalar_tensor_tensor(out=cnt[:], in0=Df[:], scalar=0.0, in1=cm1[:], op0=ALU.is_ge, op1=ALU.mult), reads=["Df", "cm1"], writes=["cnt"])
            negme = Ek("negme", [128, 1], F32)
            P.op("pool", lambda e: e.memset(negme[:], -8.0), writes=["negme"])
            sele = Ek("sele", [65, 64], F32)
            P.op("pool", lambda e: e.memset(sele[:], 0.0), writes=["sele"])
            P.op("pool", lambda e: e.memset(sele[64:65, :], 1.0), reads=["sele"], writes=["sele"])
            rle = Ek("rle", [65, 64], F32)
            P.op("pool", lambda e: e.memset(rle[:], 0.0), writes=["rle"])
            kc32 = Ek("kc32", [128, 16, 512], F32)
            vc32 = Ek("vc32", [128, 16, 512], F32)
            kcT = Ek("kcT", [128, 4, 2056], BF16)
            Vc = Ek("Vc", [128, 17, 8, 80], BF16)
            P.op("pool", lambda e: e.memset(Vc[:, 16, :, :], 0.0), writes=["Vc16"])
            for t in range(16):
                P.op("pool", lambda e, t=t: e.memset(Vc[:, t, :, 64:65], 1.0), writes=[("Vc", t)])
            qs_r = Rot(Ek, "qse", [128, 4, 8], BF16, 2)
            Pe = [Ek("Pe%d" % hh, [128, 16, 4, 8], BF16) for hh in range(2)]
            Pn = [Ek("Pne%d" % hh, [8, 4, 8], BF16) for hh in range(2)]
            ou = Ek("oue", [64, 64], F32)
            osall = Ek("osall", [64, 8, 128], BF16)
            for b in range(n_sseq):
                c0 = NP + 8 * b
                P.op("sp", lambda e, b=b: e.dma_start(out=kc32[:], in_=cache_k[b].rearrange("(t p) f -> p t f", p=128)), writes=["kc32"], dma="kc32")
                P.op("sp", lambda e, b=b: e.dma_start(out=vc32[:], in_=cache_v[b].rearrange("(t p) f -> p t f", p=128)), writes=["vc32"], dma="vc32")
                P.op("sp", lambda e, c0=c0: e.dma_start(out=kcT[:, :, 2048:2056], in_=kT_d[:, :, c0:c0 + 8].rearrange("c p n -> p c n")), writes=["kcTn"], dma="kcTn")
                P.op("sp", lambda e, c0=c0: e.dma_start(out=Vc[0:8, 16, :, :].rearrange("p h f -> p (h f)"), in_=V_d[c0:c0 + 8, :]), reads=["Vc16"], writes=["Vc16"], dma="Vc16")
                qs, qsk = qs_r.next()
                P.op("sp", lambda e, qs=qs, c0=c0: e.dma_start(out=qs[:], in_=qT_d[:, :, c0:c0 + 8].rearrange("c p n -> p c n")), writes=[qsk], dma=qsk)
                for t in range(16):
                    pT, pTk = pTe_r.next()
                    for c in range(4):
                        P.op("pe", lambda e, pT=pT, t=t, c=c: e.transpose(out=pT[:, c, :], in_=kc32[:, t, c * 128:(c + 1) * 128], identity=identf[:]), reads=["kc32", "identf"], writes=[pTk])
                    if t % 2 == 0:
                        P.op("act", lambda e, pT=pT, t=t: e.copy(out=kcT[:, :, t * 128:(t + 1) * 128], in_=pT[:]), reads=[pTk], writes=[("kcT", t)])
                    else:
                        P.op("dve", lambda e, pT=pT, t=t: e.tensor_copy(out=kcT[:, :, t * 128:(t + 1) * 128], in_=pT[:]), reads=[pTk], writes=[("kcT", t)])
                    P.op("pool", lambda e, t=t: e.tensor_copy(out=Vc[:, t, :, 0:64], in_=vc32[:, t, :].rearrange("p (h d) -> p h d", d=64)), reads=["vc32", ("Vc", t)], writes=[("Vc", t)])
                kr = [("kcT", t) for t in range(16)]
                for hp in range(4):
                    for hh in range(2):
                        rs = slice(64 * hh, 64 * hh + 64)
                        for t in range(16):
                            P.op("pe", lambda e, hp=hp, hh=hh, rs=rs, t=t, qs=qs: e.matmul(out=pSc[hh][:, t, hp, :], lhsT=kcT[rs, hp, t * 128:(t + 1) * 128], rhs=qs[rs, hp, :], start=True, stop=True),
                                 reads=[("kcT", t), qsk], writes=["pSc%d" % hh])
                        P.op("pe", lambda e, hp=hp, hh=hh, rs=rs, qs=qs: e.matmul(out=pSn[hh][0:8, hp * 8:(hp + 1) * 8], lhsT=kcT[rs, hp, 2048:2056], rhs=qs[rs, hp, :], start=True, stop=True),
                             reads=["kcTn", qsk], writes=["pSn%d" % hh])
                for hh in range(2):
                    P.op("act", lambda e, hh=hh: e.activation(out=Pe[hh][:], in_=pSc[hh][:], func=AF.Exp, bias=negme[:], scale=0.125), reads=["pSc%d" % hh, "negme"], writes=["Pe%d" % hh])
                    P.op("pool", lambda e, hh=hh: e.tensor_tensor(out=Pe[hh][:], in0=Pe[hh][:], in1=cnt[:, 0:16, :].unsqueeze(2).to_broadcast([128, 16, 4, 8]), op=ALU.mult),
                         reads=["Pe%d" % hh, "cnt"], writes=["Pe%d" % hh])
                    P.op("act", lambda e, hh=hh: e.activation(out=Pn[hh][:].rearrange("p a q -> p (a q)"), in_=pSn[hh][0:8, 0:32], func=AF.Exp, bias=negme[0:8, :], scale=0.125),
                         reads=["pSn%d" % hh, "negme"], writes=["Pne%d" % hh])
                    P.op("pool", lambda e, hh=hh: e.tensor_tensor(out=Pn[hh][:], in0=Pn[hh][:], in1=cnt[0:8, 16, :].unsqueeze(1).to_broadcast([8, 4, 8]), op=ALU.mult),
                         reads=["Pne%d" % hh, "cnt"], writes=["Pne%d" % hh])
                for h in range(8):
                    hp, hh = h // 2, h % 2
                    for t in range(16):
                        P.op("pe", lambda e, h=h, hp=hp, hh=hh, t=t: e.matmul(out=pOe[:, h, :], lhsT=Vc[:, t, h, 0:65], rhs=Pe[hh][:, t, hp, :], start=(t == 0), stop=False),
                             reads=[("Vc", t), "Pe%d" % hh], writes=["pOe"])
                    P.op("pe", lambda e, h=h, hp=hp, hh=hh: e.matmul(out=pOe[:, h, :], lhsT=Vc[0:8, 16, h, 0:65], rhs=Pn[hh][:, hp, :], start=False, stop=True),
                         reads=["Vc16", "Pne%d" % hh], writes=["pOe"])
                P.op("dve", lambda e: e.reciprocal(out=rle[64:65, :], in_=pOe[64:65, :, :].rearrange("p h q -> p (h q)")), reads=["pOe", "rle"], writes=["rle"])
                P.op("act", lambda e: e.copy(out=ou[:], in_=pOe[0:64, :, :].rearrange("p h q -> p (h q)")), reads=["pOe"], writes=["oue"])
                P.op("pe", lambda e: e.matmul(out=pBe[:], lhsT=sele[:], rhs=rle[:], start=True, stop=True), reads=["sele", "rle"], writes=["pBe"])
                P.op("dve", lambda e, b=b: e.tensor_tensor(out=osall[:, :, 8 * b:8 * b + 8], in0=ou[:].rearrange("p (h q) -> p h q", q=8), in1=pBe[:].rearrange("p (h q) -> p h q", q=8), op=ALU.mult),
                     reads=["oue", "pBe"], writes=[("osall", b)])
            for h in range(8):
                P.op("sp", lambda e, h=h: e.dma_start(out=oT_d[h // 2, (h % 2) * 64:(h % 2) * 64 + 64, NP:NP + NS], in_=osall[:, h, :]), reads=[("osall", b) for b in range(n_sseq)], dma="osall")
            pp.close()
        P.barrier()

    if "C" in stages:
        with contextlib.ExitStack() as pc:
            Ck = lambda name, shape, dtype: sb(name, shape, dtype, pc)
            pp = contextlib.ExitStack()
            pC_r = Rot(lambda n, s_, d: ps(n, s_, d, pp), "pC", [128, 512], F32, 1)
            crow = Ck("crow", [4, 1536], F32)
            P.op("sp", lambda e: e.dma_start(out=crow[:], in_=conv_w), writes=["crow"], dma="crow")
            cw = Ck("cw", [128, 12, 4], F32)
            pcw, pcwk = pC_r.next()
            for c in range(12):
                P.op("pe", lambda e, c=c, pcw=pcw: e.matmul(out=pcw[:, c * 4:(c + 1) * 4], lhsT=crow[0:4, c * 128:(c + 1) * 128], rhs=identf[0:4, 0:4], start=True, stop=True),
                     reads=["crow", "identf"], writes=[pcwk])
            P.op("dve", lambda e, pcw=pcw: e.tensor_copy(out=cw[:].rearrange("p c i -> p (c i)"), in_=pcw[:, 0:48]), reads=[pcwk], writes=["cw"])
            arep = Ck("arep", [128, 4], F32)
            dtrep = Ck("dtrep", [128, 4], F32)
            onrep = Ck("onrep", [128, 128], F32)
            P.op("sp", lambda e: e.dma_start(out=arep[:], in_=a_log.partition_broadcast(128)), writes=["arep"], dma="arep")
            P.op("sp", lambda e: e.dma_start(out=dtrep[:], in_=dt_bias.partition_broadcast(128)), writes=["dtrep"], dma="dtrep")
            P.op("sp", lambda e: e.dma_start(out=onrep[:], in_=onorm_w.partition_broadcast(128)), writes=["onrep"], dma="onrep")
            P.op("act", lambda e: e.activation(out=arep[:], in_=arep[:], func=AF.Exp), reads=["arep"], writes=["arep"])
            P.op("dve", lambda e: e.tensor_scalar(out=arep[:], in0=arep[:], scalar1=-1.0, scalar2=None, op0=ALU.mult), reads=["arep"], writes=["arep"])
            ones_bf = Ck("ones_bf", [128, 128], BF16)
            P.op("pool", lambda e: e.memset(ones_bf[:], 1.0), writes=["ones_bf"])
            ones_f = Ck("ones_f", [128, 128], F32)
            P.op("pool", lambda e: e.memset(ones_f[:], 1.0), writes=["ones_f"])

            def make_masks(bs, tag):
                nb_ = 128 // bs
                Em = Ck("Em" + tag, [nb_, 128], F32)
                P.op("pool", lambda e: e.memset(Em[:], 1.0), writes=["Em" + tag])
                P.op("pool", lambda e: e.affine_select(out=Em[:], in_=Em[:], pattern=[[1, 128]], compare_op=ALU.is_ge, fill=0.0, base=0, channel_multiplier=-bs),
                     reads=["Em" + tag], writes=["Em" + tag])
                P.op("pool", lambda e: e.affine_select(out=Em[:], in_=Em[:], pattern=[[-1, 128]], compare_op=ALU.is_ge, fill=0.0, base=bs - 1, channel_multiplier=bs),
                     reads=["Em" + tag], writes=["Em" + tag])
                pb_, pbk_ = pC_r.next()
                P.op("pe", lambda e: e.matmul(out=pb_[:, 0:128], lhsT=Em[:], rhs=Em[:], start=True, stop=True), reads=["Em" + tag], writes=[pbk_])
                Bones = Ck("Bones" + tag, [128, 128], F32)
                P.op("dve", lambda e: e.tensor_copy(out=Bones[:], in_=pb_[:, 0:128]), reads=[pbk_], writes=["Bones" + tag])
                outs = []
                for nm, pat, cm, base in (("U", [[1, 128]], -1, 0), ("Mi", [[-1, 128]], 1, 0), ("Ms", [[-1, 128]], 1, -1)):
                    t_ = Ck(nm + tag, [128, 128], F32)
                    k_ = nm + tag
                    P.op("pool", lambda e, t_=t_: e.memset(t_[:], 1.0), writes=[k_])
                    P.op("pool", lambda e, t_=t_, pat=pat, cm=cm, base=base: e.affine_select(out=t_[:], in_=t_[:], pattern=pat, compare_op=ALU.is_ge, fill=0.0, base=base, channel_multiplier=cm),
                         reads=[k_], writes=[k_])
                    P.op("pool", lambda e, t_=t_: e.tensor_tensor(out=t_[:], in0=t_[:], in1=Bones[:], op=ALU.mult), reads=[k_, "Bones" + tag], writes=[k_])
                    outs.append((t_, k_))
                return (Bones, "Bones" + tag), outs[0], outs[1], outs[2]

            (Bones, Bonesk), (Um, Uk), (Mi, Mik), (Ms, Msk) = make_masks(64, "p")

            yc_r = Rot(Ck, "yc", [128, 512], F32, 3)
            ex_r = Rot(Ck, "exc", [128, 512], F32, 3)
            sqb_r = Rot(Ck, "sqb", [128, 512], BF16, 2)
            rs_r = Rot(Ck, "rsc", [128, 512], F32, 2)
            sm_r = Rot(Ck, "smc", [128, 40], F32, 2)
            otok_r = Rot(Ck, "otok", [128, 4, 128], F32, 2)
            H = range(4)
            Sst4 = Ck("Sst4", [128, 4, 128], F32)
            Sbf4 = Ck("Sbf4", [128, 4, 128], BF16)
            sq4_r = Rot(Ck, "sq4", [128, 512], F32, 2)
            ss4_r = Rot(Ck, "ss4", [128, 4], F32, 2)
            on_r = Rot(Ck, "onc", [128, 4, 128], F32, 2)
            zs_r = Rot(Ck, "zsc", [128, 512], F32, 2)
            ob_r = Rot(Ck, "obc", [128, 512], BF16, 2)

            def silu_from(yc, yck, nn=512):
                ex, exk = ex_r.next()
                P.op("act", lambda e: e.activation(out=ex[:, 0:nn], in_=yc[:, 0:nn], func=AF.Exp, scale=-1.0), reads=[yck], writes=[exk])
                P.op("dve", lambda e: e.tensor_scalar(out=ex[:, 0:nn], in0=ex[:, 0:nn], scalar1=1.0, scalar2=None, op0=ALU.add), reads=[exk], writes=[exk])
                P.op("dve", lambda e: e.reciprocal(out=ex[:, 0:nn], in_=ex[:, 0:nn]), reads=[exk], writes=[exk])
                return ex, exk

            def rstd_small(ap, key, scale):
                P.op("dve", lambda e: e.tensor_scalar(out=ap, in0=ap, scalar1=scale, scalar2=EPS, op0=ALU.mult, op1=ALU.add), reads=[key], writes=[key])
                P.op("act", lambda e: e.activation(out=ap, in_=ap, func=AF.Sqrt), reads=[key], writes=[key])
                P.op("dve", lambda e: e.reciprocal(out=ap, in_=ap), reads=[key], writes=[key])

            ones_f4 = Ck("ones_f4", [128, 4, 128], F32)
            P.op("pool", lambda e: e.memset(ones_f4[:], 1.0), writes=["ones_f4"])
            grep4_r = Rot(Ck, "grep4", [128, 4, 128], F32, 2)
            Erow4_r = Rot(Ck, "Erow4", [128, 4, 128], F32, 2)
            t4_r = Rot(Ck, "t4", [128, 4, 128], F32, 2)
            Gs4_r = Rot(Ck, "Gs4", [128, 4, 128], F32, 2)
            Gi4_r = Rot(Ck, "Gi4", [128, 4, 128], F32, 2)
            qd4_r = Rot(Ck, "qd4", [128, 4, 128], BF16, 2)
            A4_r = Rot(Ck, "A4", [128, 4, 128], BF16, 2)
            at4_r = Rot(Ck, "at4", [128, 4, 128], BF16, 2)
            attnT4_r = Rot(Ck, "attnT4", [128, 4, 128], BF16, 2)
            Pm4_r = Rot(Ck, "Pm4", [128, 4, 128], BF16, 3)
            PmT4_r = Rot(Ck, "PmT4", [128, 4, 128], BF16, 3)
            R4_r = Rot(Ck, "R4", [128, 4, 128], BF16, 3)
            kbd4_r = Rot(Ck, "kbd4", [128, 4, 128], BF16, 2)
            kd4_r = Rot(Ck, "kd4", [128, 4, 128], BF16, 2)
            vb4_r = Rot(Ck, "vb4", [128, 4, 128], BF16, 2)
            wT4_r = Rot(Ck, "wT4", [128, 4, 128], BF16, 2)
            u4_r = Rot(Ck, "u4", [128, 4, 128], F32, 2)
            vn4_r = Rot(Ck, "vn4", [128, 4, 128], BF16, 2)
            pC4_r = Rot(lambda n, s_, d: ps(n, s_, d, pp), "pC4", [128, 4, 128], F32, 5)
            _pCb4_full = Rot(lambda n, s_, d: ps(n, s_, d, pp), "pCb4", [128, 8, 128], BF16, 2)

            class _HalfRot:
                def next(self_inner):
                    t_, k_ = _pCb4_full.next()
                    return t_[:, 0:4, :], k_
            pCb4_r = _HalfRot()

            def bc(ap_p4):
                return ap_p4.unsqueeze(2).to_broadcast([128, 4, 128])

            def gdn_tile(qnT, qnTk, knT, knTk, vT, vTk, sl, zb, zbk, states, blocks, masks, valid=None):
                (Bones_, Bonesk_), (Um_, Uk_), (Mi_, Mik_), (Ms_, Msk_) = masks
                qk_ = [(qnTk, h) for h in H]
                kk_ = [(knTk, h) for h in H]
                vk_ = [(vTk, h) for h in H]
                sm, smk = sm_r.next()
                P.op("act", lambda e: e.activation(out=sm[:, 0:4], in_=zb[:, 512:516], func=AF.Exp, scale=-1.0), reads=[zbk], writes=[smk])
                P.op("dve", lambda e: e.tensor_scalar(out=sm[:, 0:4], in0=sm[:, 0:4], scalar1=1.0, scalar2=None, op0=ALU.add), reads=[smk], writes=[smk])
                P.op("dve", lambda e: e.reciprocal(out=sm[:, 0:4], in_=sm[:, 0:4]), reads=[smk], writes=[smk])
                P.op("dve", lambda e: e.tensor_tensor(out=sm[:, 8:12], in0=zb[:, 516:520], in1=dtrep[:], op=ALU.add), reads=[zbk, "dtrep", smk], writes=[smk])
                P.op("act", lambda e: e.activation(out=sm[:, 8:12], in_=sm[:, 8:12], func=AF.Exp), reads=[smk], writes=[smk])
                P.op("dve", lambda e: e.tensor_scalar(out=sm[:, 8:12], in0=sm[:, 8:12], scalar1=1.0, scalar2=None, op0=ALU.add), reads=[smk], writes=[smk])
                P.op("act", lambda e: e.activation(out=sm[:, 8:12], in_=sm[:, 8:12], func=AF.Ln), reads=[smk], writes=[smk])
                P.op("dve", lambda e: e.tensor_tensor(out=sm[:, 4:8], in0=sm[:, 8:12], in1=arep[:], op=ALU.mult), reads=[smk, "arep"], writes=[smk])
                if valid is not None:
                    vm, vmk = valid
                    P.op("dve", lambda e: e.tensor_scalar(out=sm[:, 0:8], in0=sm[:, 0:8], scalar1=vm[:, 0:1], scalar2=None, op0=ALU.mult), reads=[smk, vmk], writes=[smk])
                pkk, pkkk = pC4_r.next()
                pqk, pqkk = pC4_r.next()
                for h in H:
                    P.op("pe", lambda e, h=h: e.matmul(out=pkk[:, h, :], lhsT=knT[:, h, sl], rhs=knT[:, h, sl], start=True, stop=True), reads=[(knTk, h)], writes=[pkkk])
                for h in H:
                    P.op("pe", lambda e, h=h: e.matmul(out=pqk[:, h, :], lhsT=qnT[:, h, sl], rhs=knT[:, h, sl], start=True, stop=True), reads=[(qnTk, h), (knTk, h)], writes=[pqkk])
                pbk_, pbkk = pCb4_r.next()
                pbv_, pbvk = pCb4_r.next()
                for h in H:
                    P.op("pe", lambda e, h=h: e.transpose(out=pbk_[:, h, :], in_=knT[:, h, sl], identity=ident[:]), reads=[(knTk, h), "ident"], writes=[pbkk])
                for h in H:
                    P.op("pe", lambda e, h=h: e.transpose(out=pbv_[:, h, :], in_=vT[:, h, sl], identity=ident[:]), reads=[(vTk, h), "ident"], writes=[pbvk])
                pd, pdk = pC4_r.next()
                P.op("pe", lambda e: e.matmul(out=pd[:, 0, 0:4], lhsT=Um_[:], rhs=sm[:, 4:8], start=True, stop=True), reads=[Uk_, smk], writes=[pdk])
                P.op("pe", lambda e: e.matmul(out=pd[:, 0, 4:8], lhsT=Bones_[:], rhs=sm[:, 4:8], start=True, stop=True), reads=[Bonesk_, smk], writes=[pdk])
                P.op("dve", lambda e: e.tensor_copy(out=sm[:, 12:20], in_=pd[:, 0, 0:8]), reads=[pdk, smk], writes=[smk])
                P.op("act", lambda e: e.activation(out=sm[:, 20:24], in_=sm[:, 12:16], func=AF.Exp), reads=[smk], writes=[smk])
                P.op("dve", lambda e: e.tensor_tensor(out=sm[:, 20:24], in0=sm[:, 20:24], in1=sm[:, 0:4], op=ALU.mult), reads=[smk], writes=[smk])
                P.op("dve", lambda e: e.tensor_tensor(out=sm[:, 24:28], in0=sm[:, 16:20], in1=sm[:, 12:16], op=ALU.subtract), reads=[smk], writes=[smk])
                P.op("act", lambda e: e.activation(out=sm[:, 24:28], in_=sm[:, 24:28], func=AF.Exp), reads=[smk], writes=[smk])
                kbd, kbdk = kbd4_r.next()
                kd, kdk = kd4_r.next()
                vbm, vbmk = vb4_r.next()
                P.op("dve", lambda e: e.tensor_tensor(out=kbd[:], in0=pbk_[:], in1=bc(sm[:, 20:24]), op=ALU.mult), reads=[pbkk, smk], writes=[kbdk])
                P.op("dve", lambda e: e.tensor_tensor(out=kd[:], in0=pbk_[:], in1=bc(sm[:, 24:28]), op=ALU.mult), reads=[pbkk, smk], writes=[kdk])
                P.op("dve", lambda e: e.tensor_tensor(out=vbm[:], in0=pbv_[:], in1=bc(sm[:, 0:4]), op=ALU.mult), reads=[pbvk, smk], writes=[vbmk])
                grep, grepk = grep4_r.next()
                P.op("dve", lambda e: e.tensor_tensor(out=grep[:], in0=ones_f4[:], in1=bc(sm[:, 4:8]), op=ALU.mult), reads=["ones_f4", smk], writes=[grepk])
                pdr, pdrk = pC4_r.next()
                for h in H:
                    P.op("pe", lambda e, h=h: e.matmul(out=pdr[:, h, :], lhsT=grep[:, h, :], rhs=Um_[:], start=True, stop=True), reads=[grepk, Uk_], writes=[pdrk])
                Erow, Erowk = Erow4_r.next()
                P.op("act", lambda e: e.activation(out=Erow[:], in_=pdr[:], func=AF.Exp), reads=[pdrk], writes=[Erowk])
                t4, t4k = t4_r.next()
                P.op("dve", lambda e: e.tensor_tensor(out=t4[:], in0=pdr[:], in1=bc(sm[:, 12:16]), op=ALU.subtract), reads=[pdrk, smk], writes=[t4k])
                P.op("act", lambda e: e.activation(out=t4[:], in_=t4[:], func=AF.Exp, scale=-1.0), reads=[t4k], writes=[t4k])
                Gs, Gsk = Gs4_r.next()
                Gi, Gik = Gi4_r.next()
                P.op("dve", lambda e: e.scalar_tensor_tensor(out=Gs[:], in0=t4[:], scalar=1.0, in1=Ms_[:].unsqueeze(1).to_broadcast([128, 4, 128]), op0=ALU.min, op1=ALU.mult),
                     reads=[t4k, Msk_], writes=[Gsk])
                P.op("dve", lambda e: e.scalar_tensor_tensor(out=Gi[:], in0=t4[:], scalar=1.0, in1=Mi_[:].unsqueeze(1).to_broadcast([128, 4, 128]), op0=ALU.min, op1=ALU.mult),
                     reads=[t4k, Mik_], writes=[Gik])
                P.op("dve", lambda e: e.tensor_tensor(out=Gs[:], in0=Gs[:], in1=bc(sm[:, 0:4]), op=ALU.mult), reads=[Gsk, smk], writes=[Gsk])
                qd, qdk = qd4_r.next()
                P.op("pool", lambda e: e.tensor_tensor(out=qd[:], in0=qnT[:, :, sl], in1=Erow[:], op=ALU.mult), reads=qk_ + [Erowk], writes=[qdk])
                Am, Amk = A4_r.next()
                at, atk = at4_r.next()
                P.op("dve", lambda e: e.tensor_tensor(out=Am[:], in0=pkk[:], in1=Gs[:], op=ALU.mult), reads=[pkkk, Gsk], writes=[Amk])
                P.op("dve", lambda e: e.tensor_tensor(out=at[:], in0=pqk[:], in1=Gi[:], op=ALU.mult), reads=[pqkk, Gik], writes=[atk])
                pbA, pbAk = pCb4_r.next()
                pbT, pbTk = pCb4_r.next()
                for h in H:
                    P.op("pe", lambda e, h=h: e.transpose(out=pbA[:, h, :], in_=Am[:, h, :], identity=ident[:]), reads=[Amk, "ident"], writes=[pbAk])
                for h in H:
                    P.op("pe", lambda e, h=h: e.transpose(out=pbT[:, h, :], in_=at[:, h, :], identity=ident[:]), reads=[atk, "ident"], writes=[pbTk])
                Bm, Bmk = Pm4_r.next()
                attnT, attnTk = attnT4_r.next()
                P.op("act", lambda e: e.copy(out=Bm[:], in_=pbA[:]), reads=[pbAk], writes=[Bmk])
                P.op("act", lambda e: e.copy(out=attnT[:], in_=pbT[:]), reads=[pbTk], writes=[attnTk])
                Rm, Rmk = R4_r.next()
                P.op("dve", lambda e, Rm=Rm: e.tensor_tensor(out=Rm[:], in0=ident[:].unsqueeze(1).to_broadcast([128, 4, 128]), in1=Bm[:], op=ALU.subtract), reads=["ident", Bmk], writes=[Rmk])
                Pc, Pck, PcT, PcTk = Bm, Bmk, Am, Amk
                nlev = 5
                for lev in range(1, nlev + 1):
                    last = lev == nlev
                    if not last:
                        pn, pnk = pC4_r.next()
                        for h in H:
                            P.op("pe", lambda e, h=h, pn=pn, Pc=Pc, PcT=PcT: e.matmul(out=pn[:, h, :], lhsT=PcT[:, h, :], rhs=Pc[:, h, :], start=True, stop=True), reads=[Pck, PcTk], writes=[pnk])
                    pnT, pnTk = pC4_r.next()
                    for h in H:
                        P.op("pe", lambda e, h=h, pnT=pnT, Pc=Pc, PcT=PcT: e.matmul(out=pnT[:, h, :], lhsT=Pc[:, h, :], rhs=PcT[:, h, :], start=True, stop=True), reads=[Pck, PcTk], writes=[pnTk])
                    PnT, PnTk = PmT4_r.next()
                    P.op("dve", lambda e, PnT=PnT, pnT=pnT: e.tensor_copy(out=PnT[:], in_=pnT[:]), reads=[pnTk], writes=[PnTk])
                    if not last:
                        Pn, Pnk = Pm4_r.next()
                        P.op("act", lambda e, Pn=Pn, pn=pn: e.copy(out=Pn[:], in_=pn[:]), reads=[pnk], writes=[Pnk])
                    pr, prk = pC4_r.next()
                    for h in H:
                        P.op("pe", lambda e, h=h, pr=pr, PnT=PnT, Rm=Rm: e.matmul(out=pr[:, h, :], lhsT=PnT[:, h, :], rhs=Rm[:, h, :], start=True, stop=True), reads=[PnTk, Rmk], writes=[prk])
                    Rn, Rnk = R4_r.next()
                    P.op("dve", lambda e, Rn=Rn, pr=pr, Rm=Rm: e.tensor_tensor(out=Rn[:], in0=pr[:], in1=Rm[:], op=ALU.add), reads=[prk, Rmk], writes=[Rnk])
                    Rm, Rmk = Rn, Rnk
                    if not last:
                        Pc, Pck = Pn, Pnk
                    PcT, PcTk = PnT, PnTk
                pw, pwk = pC4_r.next()
                pu, puk = pC4_r.next()
                for h in H:
                    P.op("pe", lambda e, h=h, Rm=Rm: e.matmul(out=pw[:, h, :], lhsT=kbd[:, h, :], rhs=Rm[:, h, :], start=True, stop=True), reads=[kbdk, Rmk], writes=[pwk])
                for h in H:
                    P.op("pe", lambda e, h=h, Rm=Rm: e.matmul(out=pu[:, h, :], lhsT=Rm[:, h, :], rhs=vbm[:, h, :], start=True, stop=True), reads=[vbmk, Rmk], writes=[puk])
                wT, wTk = wT4_r.next()
                uu, uuk = u4_r.next()
                P.op("act", lambda e: e.copy(out=wT[:], in_=pw[:]), reads=[pwk], writes=[wTk])
                P.op("dve", lambda e: e.tensor_copy(out=uu[:], in_=pu[:]), reads=[puk], writes=[uuk])
                vn, vnk = vn4_r.next()
                otok, otokk = otok_r.next()
                for bi, (rs, si) in enumerate(blocks):
                    r1 = rs.stop
                    S4, S4k, Sb4, Sb4k = states[si]
                    pW, pWk = pC4_r.next()
                    for h in H:
                        P.op("pe", lambda e, h=h, pW=pW, rs=rs, Sb4=Sb4: e.matmul(out=pW[rs, h, :], lhsT=wT[:, h, rs], rhs=Sb4[:, h, :], start=True, stop=True), reads=[wTk, Sb4k], writes=[pWk])
                    P.op("dve", lambda e, pW=pW, rs=rs: e.tensor_tensor(out=vn[rs, :, :], in0=uu[rs, :, :], in1=pW[rs, :, :], op=ALU.subtract), reads=[pWk, uuk], writes=[(vnk, bi)])
                    pO, pOk = pC4_r.next()
                    for h in H:
                        P.op("pe", lambda e, h=h, pO=pO, rs=rs, Sb4=Sb4: e.matmul(out=pO[rs, h, :], lhsT=qd[:, h, rs], rhs=Sb4[:, h, :], start=True, stop=True), reads=[qdk, Sb4k], writes=[pOk])
                    P.op("act", lambda e, pO=pO, rs=rs: e.copy(out=otok[rs, :, :], in_=pO[rs, :, :]), reads=[pOk], writes=[(otokk, bi)])
                    pO2, pO2k = pC4_r.next()
                    for h in H:
                        P.op("pe", lambda e, h=h, pO2=pO2, rs=rs: e.matmul(out=pO2[rs, h, :], lhsT=attnT[rs, h, rs], rhs=vn[rs, h, :], start=True, stop=True), reads=[attnTk, (vnk, bi)], writes=[pO2k])
                    P.op("dve", lambda e, pO2=pO2, rs=rs: e.tensor_tensor(out=otok[rs, :, :], in0=otok[rs, :, :], in1=pO2[rs, :, :], op=ALU.add), reads=[pO2k, (otokk, bi)], writes=[(otokk, bi)])
                    pN, pNk = pC4_r.next()
                    for h in H:
                        P.op("pe", lambda e, h=h, pN=pN, rs=rs: e.matmul(out=pN[:, h, :], lhsT=kd[rs, h, :], rhs=vn[rs, h, :], start=True, stop=True), reads=[kdk, (vnk, bi)], writes=[pNk])
                    P.op("dve", lambda e, S4=S4, r1=r1: e.tensor_tensor(out=S4[:], in0=S4[:], in1=Erow[:, :, r1 - 1:r1].to_broadcast([128, 4, 128]), op=ALU.mult), reads=[S4k, Erowk], writes=[S4k])
                    P.op("dve", lambda e, S4=S4, pN=pN: e.tensor_tensor(out=S4[:], in0=S4[:], in1=pN[:], op=ALU.add), reads=[S4k, pNk], writes=[S4k])
                    P.op("act", lambda e, S4=S4, Sb4=Sb4: e.copy(out=Sb4[:], in_=S4[:]), reads=[S4k], writes=[Sb4k])
                orr = [(otokk, bi) for bi in range(len(blocks))]
                sq4, sq4k = sq4_r.next()
                ss4, ss4k = ss4_r.next()
                P.op("act", lambda e: e.activation(out=sq4[:], in_=otok[:].rearrange("p h d -> p (h d)"), func=AF.Square), reads=orr, writes=[sq4k])
                P.op("dve", lambda e: e.tensor_reduce(out=ss4[:], in_=sq4[:].rearrange("p (h d) -> p h d", d=128), axis=AX.X, op=ALU.add), reads=[sq4k], writes=[ss4k])
                rstd_small(ss4[:], ss4k, 1.0 / 128)
                on, onk = on_r.next()
                P.op("dve", lambda e: e.tensor_tensor(out=on[:], in0=otok[:], in1=ss4[:].unsqueeze(2).to_broadcast([128, 4, 128]), op=ALU.mult), reads=orr + [ss4k], writes=[onk])
                P.op("pool", lambda e: e.tensor_tensor(out=on[:], in0=on[:], in1=onrep[:].unsqueeze(1).to_broadcast([128, 4, 128]), op=ALU.mult), reads=[onk, "onrep"], writes=[onk])
                zs, zsk = zs_r.next()
                P.op("act", lambda e: e.activation(out=zs[:], in_=zb[:, 0:512], func=AF.Exp, scale=-1.0), reads=[zbk], writes=[zsk])
                P.op("dve", lambda e: e.tensor_scalar(out=zs[:], in0=zs[:], scalar1=1.0, scalar2=None, op0=ALU.add), reads=[zsk], writes=[zsk])
                P.op("dve", lambda e: e.reciprocal(out=zs[:], in_=zs[:]), reads=[zsk], writes=[zsk])
                P.op("pool", lambda e: e.tensor_tensor(out=zs[:], in0=zs[:], in1=zb[:, 0:512], op=ALU.mult), reads=[zsk, zbk], writes=[zsk])
                ob, obk = ob_r.next()
                P.op("dve", lambda e: e.tensor_tensor(out=ob[:], in0=on[:].rearrange("p h d -> p (h d)"), in1=zs[:], op=ALU.mult), reads=[onk, zsk], writes=[obk])
                return ob, obk

            def conv_supertile(gin, gink_list, N, qnT, qnTk, knT, knTk, vT, vTk, sample=False, chunks=range(12)):
                def tap(c, i):
                    if sample:
                        return gin[:, c, :, i:i + 8]
                    return gin[:, c, i:i + N]

                def v3(ap2):
                    return ap2.rearrange("p (b t) -> p b t", t=8) if sample else ap2

                def dsts(dst, h):
                    if sample:
                        return dst[:, h, :].rearrange("p (b s) -> p b s", s=64)[:, :, 0:8]
                    return dst[:, h, 0:N]
                for c in chunks:
                    eng = "dve"
                    yc, yck = yc_r.next()
                    P.op(eng, lambda e, yc=yc, c=c: e.tensor_scalar(out=v3(yc[:, 0:N]), in0=tap(c, 0), scalar1=cw[:, c, 0:1], scalar2=None, op0=ALU.mult),
                         reads=gink_list + ["cw"], writes=[yck])
                    for i in range(1, 4):
                        P.op(eng, lambda e, yc=yc, c=c, i=i: e.scalar_tensor_tensor(out=v3(yc[:, 0:N]), in0=tap(c, i), scalar=cw[:, c, i:i + 1], in1=v3(yc[:, 0:N]), op0=ALU.mult, op1=ALU.add),
                             reads=gink_list + ["cw", yck], writes=[yck])
                    ex, exk = silu_from(yc, yck, N)
                    h = c % 4
                    if c >= 8:
                        P.op("pool", lambda e, yc=yc, ex=ex, h=h: e.tensor_tensor(out=dsts(vT, h), in0=v3(yc[:, 0:N]), in1=v3(ex[:, 0:N]), op=ALU.mult), reads=[yck, exk, (vTk, h)], writes=[(vTk, h)])
                        continue
                    P.op("pool", lambda e, yc=yc, ex=ex: e.tensor_tensor(out=yc[:, 0:N], in0=yc[:, 0:N], in1=ex[:, 0:N], op=ALU.mult), reads=[yck, exk], writes=[yck])
                    sqb, sqbk = sqb_r.next()
                    P.op("act", lambda e, sqb=sqb, yc=yc: e.activation(out=sqb[:, 0:N], in_=yc[:, 0:N], func=AF.Square), reads=[yck], writes=[sqbk])
                    pq, pqk_ = pC_r.next()
                    P.op("pe", lambda e, pq=pq, sqb=sqb: e.matmul(out=pq[:, 0:N], lhsT=ones_bf[:], rhs=sqb[:, 0:N], start=True, stop=True), reads=["ones_bf", sqbk], writes=[pqk_])
                    rsb, rsbk = rs_r.next()
                    mul = 128.0 if c < 4 else 1.0
                    P.op("dve", lambda e, rsb=rsb, pq=pq, mul=mul: e.tensor_scalar(out=rsb[:, 0:N], in0=pq[:, 0:N], scalar1=EPS, scalar2=mul, op0=ALU.add, op1=ALU.mult), reads=[pqk_], writes=[rsbk])
                    P.op("act", lambda e, rsb=rsb: e.activation(out=rsb[:, 0:N], in_=rsb[:, 0:N], func=AF.Sqrt), reads=[rsbk], writes=[rsbk])
                    P.op("dve", lambda e, rsb=rsb: e.reciprocal(out=rsb[:, 0:N], in_=rsb[:, 0:N]), reads=[rsbk], writes=[rsbk])
                    dst, dstk = (qnT, qnTk) if c < 4 else (knT, knTk)
                    P.op("pool", lambda e, dst=dst, yc=yc, rsb=rsb, h=h: e.tensor_tensor(out=dsts(dst, h), in0=v3(yc[:, 0:N]), in1=v3(rsb[:, 0:N]), op=ALU.mult), reads=[yck, rsbk, (dstk, h)], writes=[(dstk, h)])

            masks_p = ((Bones, Bonesk), (Um, Uk), (Mi, Mik), (Ms, Msk))
            pcp = contextlib.ExitStack()
            Cp = lambda name, shape, dtype: sb(name, shape, dtype, pcp)
            gin_r = Rot(Cp, "gin", [128, 12, 515], BF16, 2)
            qnT_r = Rot(Cp, "qnT", [128, 4, 512], BF16, 2)
            knT_r = Rot(Cp, "knT", [128, 4, 512], BF16, 2)
            vT_r = Rot(Cp, "vTc", [128, 4, 512], BF16, 2)
            zb_r = Rot(Cp, "zbc", [128, 520], F32, 2)
            obT_r = Rot(Cp, "obT", [128, 4, 512], BF16, 2)
            for s in range(n_pseq):
                P.op("pool", lambda e: e.memset(Sst4[:], 0.0), writes=["Sst4"])
                P.op("pool", lambda e: e.memset(Sbf4[:], 0.0), writes=["Sbf4"])
                states_p = [(Sst4, "Sst4", Sbf4, "Sbf4")]

                def load_gin(u):
                    g0_ = s * SEQ + u * 512
                    gin, gink = gin_r.next()
                    if u == 0:
                        P.op("pool", lambda e, gin=gin: e.memset(gin[:, :, 0:3], 0.0), writes=[(gink, "h")])
                    else:
                        P.op("sp", lambda e, gin=gin, g0_=g0_: e.dma_start(out=gin[:, :, 0:3], in_=gT_d[:, :, g0_ - 3:g0_].rearrange("c p n -> p c n")), writes=[(gink, "h")], dma=(gink, "h"))
                    P.op("sp", lambda e, gin=gin, g0_=g0_: e.dma_start(out=gin[:, :, 3:515], in_=gT_d[:, :, g0_:g0_ + 512].rearrange("c p n -> p c n")), writes=[(gink, "m")], dma=(gink, "m"))
                    return (gin, [(gink, "h"), (gink, "m")]) + qnT_r.next() + knT_r.next() + vT_r.next()

                nxt = load_gin(0)
                conv_supertile(nxt[0], nxt[1], 512, *nxt[2:])
                for u in range(SEQ // 512):
                    g0 = s * SEQ + u * 512
                    cur = nxt
                    gin, ginkl, qnT, qnTk, knT, knTk, vT, vTk = cur
                    if u + 1 < SEQ // 512:
                        nxt = load_gin(u + 1)
                    obT, obTk = obT_r.next()
                    for j in range(4):
                        sl = slice(j * 128, (j + 1) * 128)
                        zb, zbk = zb_r.next()
                        P.op("sp", lambda e, zb=zb, g0=g0, j=j: e.dma_start(out=zb[:], in_=zbg_d[g0 + j * 128:g0 + (j + 1) * 128, :]), writes=[zbk], dma=zbk)
                        ob, obk = gdn_tile(qnT, qnTk, knT, knTk, vT, vTk, sl, zb, zbk, states_p,
                                           [(slice(0, 64), 0), (slice(64, 128), 0)], masks_p)
                        pb, pbk = pCb4_r.next()
                        for c in range(4):
                            P.op("pe", lambda e, pb=pb, ob=ob, c=c: e.transpose(out=pb[:, c, :], in_=ob[:, c * 128:(c + 1) * 128], identity=ident[:]), reads=[obk, "ident"], writes=[pbk])
                        P.op("act", lambda e, pb=pb, obT=obT, sl=sl: e.copy(out=obT[:, :, sl], in_=pb[:]), reads=[pbk], writes=[(obTk, j)])
                        if u + 1 < SEQ // 512:
                            conv_supertile(nxt[0], nxt[1], 512, *nxt[2:], chunks=range(3 * j, 3 * j + 3))
                    P.op("sp", lambda e, obT=obT, g0=g0: e.dma_start(out=oT_d[4:8, :, g0:g0 + 512].rearrange("c p n -> p c n"), in_=obT[:]), reads=[(obTk, j) for j in range(4)], dma=obTk)
                for h in H:
                    P.op("sp", lambda e, h=h, s=s: e.dma_start(out=ssm_p[s, h], in_=Sst4[:, h, :]), reads=["Sst4"], dma=("ssm_st", h))
            pcp.close()
            P.barrier()
            if NS:
                assert n_sseq == 16
                vmask = Ck("vmask", [128, 1], F32)
                P.op("pool", lambda e: e.memset(vmask[:], 0.0), writes=["vmask"])
                for i in range(2):
                    P.op("pool", lambda e, i=i: e.memset(vmask[64 * i:64 * i + 8, :], 1.0), reads=["vmask"], writes=["vmask"])
                sc32 = Ck("sc32", [48, 1536], F32)
                P.op("sp", lambda e: e.dma_start(out=sc32[:], in_=state_conv.rearrange("b i f -> (b i) f")), writes=["sc32"], dma="sc32")
                gins = Ck("gins", [128, 12, 16, 11], BF16)
                for c4 in range(3):
                    pt_, ptk_ = pC_r.next()
                    for cc in range(4):
                        c = c4 * 4 + cc
                        P.op("pe", lambda e, pt_=pt_, c=c, cc=cc: e.matmul(out=pt_[:, cc * 48:(cc + 1) * 48], lhsT=sc32[0:48, c * 128:(c + 1) * 128], rhs=identf[0:48, 0:48], start=True, stop=True),
                             reads=["sc32", "identf"], writes=[ptk_])
                    P.op("dve", lambda e, pt_=pt_, c4=c4: e.tensor_copy(out=gins[:, c4 * 4:(c4 + 1) * 4, :, 0:3], in_=pt_[:, 0:192].rearrange("p (c b i) -> p c b i", c=4, i=3)),
                         reads=[ptk_], writes=[("gins", "h", c4)])
                for c in range(12):
                    P.op("sp", lambda e, c=c: e.dma_start(out=gins[:, c, :, 3:11], in_=gT_d[c, :, NP:NP + 128].rearrange("p (b t) -> p b t", t=8)), writes=[("gins", "m")], dma="ginsm")
                ginsk = [("gins", "h", c4) for c4 in range(3)] + [("gins", "m")]
                qnS = Ck("qnS", [128, 4, 1024], BF16)
                knS = Ck("knS", [128, 4, 1024], BF16)
                vS = Ck("vS", [128, 4, 1024], BF16)
                for t_, k_ in ((qnS, "qnS"), (knS, "knS"), (vS, "vS")):
                    for h in H:
                        P.op("pool", lambda e, t_=t_, h=h: e.memset(t_[:, h, :], 0.0), writes=[(k_, h)])
                conv_supertile(gins, ginsk, 128, qnS, "qnS", knS, "knS", vS, "vS", sample=True)
                Ss = [Ck("Ss4_%d" % i, [128, 4, 128], F32) for i in range(2)]
                Sbs = [Ck("Sbs4_%d" % i, [128, 4, 128], BF16) for i in range(2)]
                zbs_r = Rot(Ck, "zbs", [128, 520], F32, 2)
                for zb_, zk_ in zip(zbs_r.bufs, zbs_r.keys):
                    P.op("pool", lambda e, zb_=zb_: e.memset(zb_[:], 0.0), writes=[zk_])
                obTs = Ck("obTs", [128, 4, 128], BF16)
                for u in range(8):
                    for i in range(2):
                        for h in H:
                            P.op("sp", lambda e, u=u, i=i, h=h: e.dma_start(out=Ss[i][:, h, :], in_=state_ssm[2 * u + i, h]), writes=["Ss4_%d" % i], dma="Ss4_%d" % i)
                        P.op("act", lambda e, i=i: e.copy(out=Sbs[i][:], in_=Ss[i][:]), reads=["Ss4_%d" % i], writes=["Sbs4_%d" % i])
                    states_s = [(Ss[i], "Ss4_%d" % i, Sbs[i], "Sbs4_%d" % i) for i in range(2)]
                    zb, zbk = zbs_r.next()
                    for i in range(2):
                        b0 = NP + 8 * (2 * u + i)
                        P.op("sp", lambda e, zb=zb, i=i, b0=b0: e.dma_start(out=zb[64 * i:64 * i + 8, :], in_=zbg_d[b0:b0 + 8, :]), writes=[zbk], dma=zbk)
                    sl = slice(u * 128, (u + 1) * 128)
                    ob, obk = gdn_tile(qnS, "qnS", knS, "knS", vS, "vS", sl, zb, zbk, states_s,
                                       [(slice(64 * i, 64 * i + 64), i) for i in range(2)], masks_p, valid=(vmask, "vmask"))
                    pb, pbk = pCb4_r.next()
                    for c in range(4):
                        P.op("pe", lambda e, pb=pb, ob=ob, c=c: e.transpose(out=pb[:, c, :], in_=ob[:, c * 128:(c + 1) * 128], identity=ident[:]), reads=[obk, "ident"], writes=[pbk])
                    P.op("act", lambda e, pb=pb: e.copy(out=obTs[:], in_=pb[:]), reads=[pbk], writes=["obTs"])
                    for c in range(4):
                        P.op("sp", lambda e, u=u, c=c: e.dma_start(out=oT_d[4 + c, :, NP + 16 * u:NP + 16 * u + 16].rearrange("p (i t) -> p i t", t=8),
                                                                   in_=obTs[:, c, :].rearrange("p (i s) -> p i s", s=64)[:, :, 0:8]), reads=["obTs"], dma="obTs")
                    for i in range(2):
                        for h in H:
                            P.op("sp", lambda e, u=u, i=i, h=h: e.dma_start(out=ssm_s[2 * u + i, h], in_=Ss[i][:, h, :]), reads=["Ss4_%d" % i], dma="Sso4_%d" % i)
            pp.close()
        P.barrier()

    if "D" in stages:
        with contextlib.ExitStack() as pd:
            Dk = lambda name, shape, dtype: sb(name, shape, dtype, pd)
            G = moe_g
            TG = G // 128
            wout = Dk("wout", [128, 8, D], BF16)
            wr = Dk("wr", [128, 8, 20], BF16)
            wsto_t = Dk("wsto", [128, 8, 1024], F32)
            P.op("sp", lambda e: e.dma_start(out=wsto_t[:], in_=w_out.rearrange("(k p) n -> p k n", p=128)), writes=["wsto"], dma="wsto")
            P.op("pool", lambda e: e.tensor_copy(out=wout[:], in_=wsto_t[:]), reads=["wsto"], writes=["wout"])
            wrs = Dk("wrs", [128, 8, 20], F32)
            for k in range(8):
                P.op("sp", lambda e, k=k: e.dma_start(out=wrs[:, k, 0:4], in_=w_group[k * 128:(k + 1) * 128, :]), writes=["wrs"], dma="wrs")
                for g in range(4):
                    P.op("sp", lambda e, k=k, g=g: e.dma_start(out=wrs[:, k, 4 + 4 * g:8 + 4 * g], in_=w_er[g, k * 128:(k + 1) * 128, :]), writes=["wrs"], dma="wrs")
            P.op("pool", lambda e: e.tensor_copy(out=wr[:], in_=wrs[:]), reads=["wrs"], writes=["wr"])
            wrr = ["wr"]
            brep = Dk("brep", [128, 20], F32)
            P.op("sp", lambda e: e.dma_start(out=brep[:, 0:4], in_=b_group.partition_broadcast(128)), writes=["brep"], dma="brep")
            P.op("sp", lambda e: e.dma_start(out=brep[:, 4:20], in_=b_er.partition_broadcast(128)), writes=["brep"], dma="brep")
            oTg_r = Rot(Dk, "oTg", [128, 8, G], BF16, 2)
            xg_r = Rot(Dk, "xg", [128, D], F32, 2)
            hacc = Dk("hacc", [128, TG, D], F32)
            hnT = Dk("hnT", [128, 8, G], BF16)
            gates = Dk("gates", [128, TG, 16], F32)
            hb_r = Rot(Dk, "hb", [128, D], BF16, 2)
            junkd = Dk("junkd", [128, D], BF16)
            ssq_r = Rot(Dk, "ssqd", [128, 1], F32, 2)
            rt_r = Rot(Dk, "rt", [128, 64], F32, 2)
            wgu_r = Rot(Dk, "wgu", [128, 8, 1024], BF16, 3)
            wd_r = Rot(Dk, "wd", [128, 4, 1024], BF16, 3)
            sg_r = Rot(Dk, "sg", [128, 512], F32, 2)
            actT_r = Rot(Dk, "actT", [128, 4, G], BF16, 2)
            pp = contextlib.ExitStack()
            pH_r = Rot(lambda n, s, d: ps(n, s, d, pp), "pH", [128, 512], F32, 2)
            pG_r = Rot(lambda n, s, d: ps(n, s, d, pp), "pG", [128, 512], F32, 4)
            pT_r = Rot(lambda n, s, d: ps(n, s, d, pp), "pTd", [128, 8, 128], BF16, 1)
            pR_r = Rot(lambda n, s, d: ps(n, s, d, pp), "pR", [128, 512], F32, 1)

            def rstd_from(ssq, key, scale):
                P.op("dve", lambda e: e.tensor_scalar(out=ssq, in0=ssq, scalar1=scale, scalar2=EPS,
                                                      op0=ALU.mult, op1=ALU.add), reads=[key], writes=[key])
                P.op("act", lambda e: e.activation(out=ssq, in_=ssq, func=AF.Sqrt), reads=[key], writes=[key])
                P.op("dve", lambda e: e.reciprocal(out=ssq, in_=ssq), reads=[key], writes=[key])

            groups = [tiles[i:i + TG] for i in range(0, len(tiles), TG)]
            if os.environ.get('NGRP'):
                groups = groups[:int(os.environ['NGRP'])]
            for grp in groups:
                ng = len(grp)
                NG = ng * 128
                g0 = gtok(*grp[0])
                contiguous = all(gtok(*grp[j]) == g0 + 128 * j for j in range(ng))
                assert contiguous
                oTg, oTgk = oTg_r.next()
                nko = 8 if "C" in stages else 4
                P.op("sp", lambda e, oTg=oTg, g0=g0, NG=NG, nko=nko: e.dma_start(out=oTg[:, 0:nko, 0:NG], in_=oT_d[0:nko, :, g0:g0 + NG].rearrange("c p n -> p c n")),
                     writes=[oTgk], dma=oTgk)
                for j, (kind, t) in enumerate(grp):
                    nrows = 128 if kind == "p" else NS
                    sl = slice(j * 128, (j + 1) * 128)
                    xg, xgk = xg_r.next()
                    if nrows < 128:
                        P.op("pool", lambda e, xg=xg: e.memset(xg[:], 0.0), writes=[xgk])
                    P.op("sp", lambda e, xg=xg, kind=kind, t=t, nrows=nrows: e.dma_start(out=xg[0:nrows, :], in_=x_rows(kind, t, nrows)), writes=[xgk], dma=xgk)
                    for n2 in range(2):
                        pH, pHk = pH_r.next()
                        for k in range(nko):
                            P.op("pe", lambda e, pH=pH, oTg=oTg, k=k, sl=sl, n2=n2, nko=nko: e.matmul(
                                out=pH[:], lhsT=oTg[:, k, sl], rhs=wout[:, k, n2 * 512:(n2 + 1) * 512], start=(k == 0), stop=(k == nko - 1)),
                                reads=[oTgk, "wout"], writes=[pHk])
                        P.op("dve", lambda e, pH=pH, xg=xg, j=j, n2=n2: e.tensor_tensor(out=hacc[:, j, n2 * 512:(n2 + 1) * 512], in0=pH[:], in1=xg[:, n2 * 512:(n2 + 1) * 512], op=ALU.add),
                             reads=[pHk, xgk], writes=[("hacc", j, n2)])
                    hr = [("hacc", j, 0), ("hacc", j, 1)]
                    if debug:
                        P.op("sp", lambda e, j=j, gtt=gtok(kind, t): e.dma_start(out=dbg_h[gtt:gtt + 128, :], in_=hacc[:, j, :]), reads=hr, dma="dbg_h")
                    ssq, ssqk = ssq_r.next()
                    P.op("act", lambda e, j=j, ssq=ssq: e.activation(out=junkd[:], in_=hacc[:, j, :], func=AF.Square, accum_out=ssq[:]),
                         reads=hr, writes=["junkd", ssqk])
                    rstd_from(ssq[:], ssqk, 1.0 / D)
                    hb, hbk = hb_r.next()
                    P.op("dve", lambda e, hb=hb, j=j, ssq=ssq: e.tensor_scalar(out=hb[:], in0=hacc[:, j, :], scalar1=ssq[:], scalar2=None, op0=ALU.mult),
                         reads=hr + [ssqk], writes=[hbk])
                    pT, pTk = pT_r.next()
                    for k in range(8):
                        P.op("pe", lambda e, pT=pT, hb=hb, k=k: e.transpose(out=pT[:, k, :], in_=hb[:, k * 128:(k + 1) * 128], identity=ident[:]),
                             reads=[hbk, "ident"], writes=[pTk])
                    P.op("dve", lambda e, pT=pT, sl=sl: e.tensor_tensor(out=hnT[:, :, sl], in0=pT[:], in1=n2col[:].unsqueeze(2).to_broadcast([128, 8, 128]), op=ALU.mult),
                         reads=[pTk, "n2col"], writes=[("hnT", j)])
                    pR, pRk = pR_r.next()
                    for k in range(8):
                        P.op("pe", lambda e, pR=pR, k=k, sl=sl: e.matmul(out=pR[:, 0:20], lhsT=hnT[:, k, sl], rhs=wr[:, k, :], start=(k == 0), stop=(k == 7)),
                             reads=[("hnT", j)] + wrr, writes=[pRk])
                    rt, rtk = rt_r.next()
                    V = lambda a, b, rt=rt: rt[:, a:b]

                    def dv(fn, rtk=rtk):
                        P.op("dve", fn, reads=[rtk], writes=[rtk])
                    P.op("dve", lambda e, rt=rt, pR=pR: e.tensor_tensor(out=rt[:, 0:20], in0=pR[:, 0:20], in1=brep[:], op=ALU.add),
                         reads=[pRk, "brep"], writes=[rtk])
                    dv(lambda e, V=V: e.tensor_reduce(out=V(20, 21), in_=V(0, 4), axis=AX.X, op=ALU.max))
                    dv(lambda e, V=V: e.tensor_scalar(out=V(21, 25), in0=V(0, 4), scalar1=V(20, 21), scalar2=None, op0=ALU.is_ge))
                    dv(lambda e, V=V: e.tensor_scalar(out=V(25, 29), in0=V(0, 4), scalar1=V(20, 21), scalar2=None, op0=ALU.subtract))
                    P.op("act", lambda e, V=V: e.activation(out=V(25, 29), in_=V(25, 29), func=AF.Exp, accum_out=V(29, 30)), reads=[rtk], writes=[rtk])
                    dv(lambda e, V=V: e.reciprocal(out=V(30, 31), in_=V(29, 30)))
                    dv(lambda e, V=V, rt=rt: e.tensor_tensor(out=rt[:, 31:47].rearrange("p (g x) -> p g x", x=4), in0=rt[:, 4:20].rearrange("p (g x) -> p g x", x=4),
                                                              in1=rt[:, 21:25].unsqueeze(2).to_broadcast([128, 4, 4]), op=ALU.mult))
                    dv(lambda e, V=V, rt=rt: e.tensor_reduce(out=V(47, 51), in_=rt[:, 31:47].rearrange("p (g x) -> p x g", x=4), axis=AX.X, op=ALU.add))
                    dv(lambda e, V=V: e.tensor_reduce(out=V(51, 52), in_=V(47, 51), axis=AX.X, op=ALU.max))
                    dv(lambda e, V=V: e.tensor_scalar(out=V(52, 56), in0=V(47, 51), scalar1=V(51, 52), scalar2=None, op0=ALU.is_ge))
                    dv(lambda e, V=V: e.scalar_tensor_tensor(out=V(56, 60), in0=V(52, 56), scalar=-1e30, in1=V(47, 51), op0=ALU.mult, op1=ALU.add))
                    dv(lambda e, V=V: e.tensor_reduce(out=V(60, 61), in_=V(56, 60), axis=AX.X, op=ALU.max))
                    dv(lambda e, V=V: e.tensor_scalar(out=V(56, 60), in0=V(56, 60), scalar1=V(60, 61), scalar2=None, op0=ALU.is_ge))
                    dv(lambda e, V=V: e.tensor_tensor(out=V(61, 62), in0=V(60, 61), in1=V(51, 52), op=ALU.subtract))
                    P.op("act", lambda e, V=V: e.activation(out=V(62, 63), in_=V(61, 62), func=AF.Exp), reads=[rtk], writes=[rtk])
                    dv(lambda e, V=V: e.tensor_scalar(out=V(63, 64), in0=V(62, 63), scalar1=1.0, scalar2=None, op0=ALU.add))
                    dv(lambda e, V=V: e.reciprocal(out=V(63, 64), in_=V(63, 64)))
                    dv(lambda e, V=V: e.tensor_tensor(out=V(63, 64), in0=V(63, 64), in1=V(30, 31), op=ALU.mult))
                    dv(lambda e, V=V: e.tensor_tensor(out=V(62, 63), in0=V(62, 63), in1=V(63, 64), op=ALU.mult))
                    dv(lambda e, V=V: e.tensor_scalar(out=V(52, 56), in0=V(52, 56), scalar1=V(63, 64), scalar2=None, op0=ALU.mult))
                    dv(lambda e, V=V: e.scalar_tensor_tensor(out=V(52, 56), in0=V(56, 60), scalar=V(62, 63), in1=V(52, 56), op0=ALU.mult, op1=ALU.add))
                    for g in range(4):
                        P.op("dve", lambda e, V=V, j=j, g=g: e.tensor_scalar(out=gates[:, j, 4 * g:4 * g + 4], in0=V(52, 56), scalar1=V(21 + g, 22 + g), scalar2=None, op0=ALU.mult),
                             reads=[rtk], writes=[("gates", j, g)])
                gr = [("gates", j, g) for j in range(ng) for g in range(4)]
                hnr = [("hnT", j) for j in range(ng)]
                if debug:
                    P.op("sp", lambda e, g0=g0, ng=ng: e.dma_start(out=dbg_g[g0 // 128:g0 // 128 + ng].rearrange("t p x -> p t x"), in_=gates[:, 0:ng, :]), reads=gr, dma="dbg_g")
                    P.op("sp", lambda e, g0=g0, NG=NG: e.dma_start(out=dbg_hn[:, :, g0:g0 + NG].rearrange("c p n -> p c n"), in_=hnT[:, :, 0:NG]), reads=hnr, dma="dbg_hn")
                def emit_gu(ex):
                    wgu, wguk = wgu_r.next()
                    wd, wdk = wd_r.next()
                    P.op("sp", lambda e, wgu=wgu, ex=ex: e.dma_start(out=wgu[:], in_=wgu_bf[ex].rearrange("(k p) n -> p k n", p=128)), reads=["wbf"], writes=[wguk], dma=wguk)
                    P.op("sp", lambda e, wd=wd, ex=ex: e.dma_start(out=wd[:], in_=wd_bf[ex].rearrange("(k p) n -> p k n", p=128)), reads=["wbf"], writes=[wdk], dma=wdk)
                    aT, aTk = actT_r.next()
                    for n0 in range(0, NG, 512):
                        nn = min(512, NG - n0)
                        for f in range(4):
                            pGa, pGak = pG_r.next()
                            pGb, pGbk = pG_r.next()
                            for k in range(8):
                                P.op("pe", lambda e, pGa=pGa, wgu=wgu, k=k, f=f, n0=n0, nn=nn: e.matmul(
                                    out=pGa[:, 0:nn], lhsT=wgu[:, k, f * 128:(f + 1) * 128], rhs=hnT[:, k, n0:n0 + nn], start=(k == 0), stop=(k == 7)),
                                    reads=[wguk] + hnr, writes=[pGak])
                            for k in range(8):
                                P.op("pe", lambda e, pGb=pGb, wgu=wgu, k=k, f=f, n0=n0, nn=nn: e.matmul(
                                    out=pGb[:, 0:nn], lhsT=wgu[:, k, 512 + f * 128:512 + (f + 1) * 128], rhs=hnT[:, k, n0:n0 + nn], start=(k == 0), stop=(k == 7)),
                                    reads=[wguk] + hnr, writes=[pGbk])
                            sg, sgk = sg_r.next()
                            P.op("act", lambda e, sg=sg, pGa=pGa, nn=nn: e.activation(out=sg[:, 0:nn], in_=pGa[:, 0:nn], func=AF.Exp, scale=-1.0), reads=[pGak], writes=[sgk])
                            P.op("dve", lambda e, sg=sg, nn=nn: e.tensor_scalar(out=sg[:, 0:nn], in0=sg[:, 0:nn], scalar1=1.0, scalar2=None, op0=ALU.add), reads=[sgk], writes=[sgk])
                            P.op("dve", lambda e, sg=sg, nn=nn: e.reciprocal(out=sg[:, 0:nn], in_=sg[:, 0:nn]), reads=[sgk], writes=[sgk])
                            P.op("dve", lambda e, sg=sg, pGa=pGa, nn=nn: e.tensor_tensor(out=sg[:, 0:nn], in0=sg[:, 0:nn], in1=pGa[:, 0:nn], op=ALU.mult), reads=[sgk, pGak], writes=[sgk])
                            P.op("dve", lambda e, sg=sg, pGb=pGb, aT=aT, f=f, n0=n0, nn=nn: e.tensor_tensor(out=aT[:, f, n0:n0 + nn], in0=sg[:, 0:nn], in1=pGb[:, 0:nn], op=ALU.mult),
                                 reads=[sgk, pGbk], writes=[(aTk, f, n0)])
                    return wd, wdk, aT, aTk

                def emit_down(ex, wd, wdk, aT, aTk):
                    ar = [(aTk, f, n0) for f in range(4) for n0 in range(0, NG, 512)]
                    for j in range(ng):
                        sl = slice(j * 128, (j + 1) * 128)
                        for n2 in range(2):
                            pH, pHk = pH_r.next()
                            for f in range(4):
                                P.op("pe", lambda e, pH=pH, wd=wd, aT=aT, f=f, sl=sl, n2=n2: e.matmul(
                                    out=pH[:], lhsT=aT[:, f, sl], rhs=wd[:, f, n2 * 512:(n2 + 1) * 512], start=(f == 0), stop=(f == 3)),
                                    reads=ar + [wdk], writes=[pHk])
                            P.op("dve", lambda e, pH=pH, j=j, n2=n2, ex=ex: e.scalar_tensor_tensor(
                                out=hacc[:, j, n2 * 512:(n2 + 1) * 512], in0=pH[:], scalar=gates[:, j, ex:ex + 1], in1=hacc[:, j, n2 * 512:(n2 + 1) * 512],
                                op0=ALU.mult, op1=ALU.add), reads=[pHk, ("hacc", j, n2)] + gr, writes=[("hacc", j, n2)])

                n_ex = int(os.environ.get('NEXP', n_exp))
                pend = emit_gu(0) if n_ex else None
                for ex in range(n_ex):
                    cur_ = pend
                    if ex + 1 < n_ex:
                        pend = emit_gu(ex + 1)
                    emit_down(ex, *cur_)
                for j, (kind, t) in enumerate(grp):
                    if kind == "p":
                        P.op("sp", lambda e, j=j, t=t: e.dma_start(out=y_p[t * 128:(t + 1) * 128, :], in_=hacc[:, j, :]),
                             reads=[("hacc", j, 0), ("hacc", j, 1)], dma=("ysto", j))
                    else:
                        P.op("sp", lambda e, j=j: e.dma_start(out=y_s[0:NS, :], in_=hacc[0:NS, j, :]),
                             reads=[("hacc", j, 0), ("hacc", j, 1)], dma=("ysto", j))
            pp.close()
        P.barrier()

    print('semaphores:', len(P.semkeys), 'ops:', {e: len(P.ops[e]) for e in ENGS}, flush=True)
    P.emit()
    es.close()
    return nc


N_CORES = 8
_cache = {}


def kernel(x_prompt, x_sample, cache_win_k, cache_win_v, state_conv, state_ssm, norm1_w, w_in,
           qnorm_w, knorm_w, conv_w, a_log, dt_bias, onorm_w, w_out, norm2_w, w_group, b_group,
           w_expert_router, b_expert_router, w_gate_up, w_down):
    f = lambda a: np.ascontiguousarray(np.asarray(a, dtype=np.float32))
    B, S, _ = x_prompt.shape
    DB = x_sample.shape[0]
    n_pseq = B // N_CORES
    n_sseq = DB // N_CORES
    key = (n_pseq, n_sseq)
    if key not in _cache:
        _cache[key] = build_program(n_pseq, n_sseq, stages=("A", "B", "E", "C", "D"))
    nc = _cache[key]
    shared = dict(norm1_w=f(norm1_w), w_in=f(w_in), qnorm_w=f(qnorm_w), knorm_w=f(knorm_w), conv_w=f(conv_w),
                  a_log=f(a_log), dt_bias=f(dt_bias), onorm_w=f(onorm_w), w_out=f(w_out), norm2_w=f(norm2_w),
                  w_group=f(w_group), b_group=f(b_group), w_er=f(w_expert_router), b_er=f(b_expert_router).reshape(16),
                  w_gate_up=f(w_gate_up), w_down=f(w_down))
    in_maps = []
    for c in range(N_CORES):
        m = dict(shared)
        m["xp"] = f(x_prompt[c * n_pseq:(c + 1) * n_pseq]).reshape(n_pseq * S, D)
        m["xs"] = f(x_sample[c * n_sseq:(c + 1) * n_sseq]).reshape(n_sseq * 8, D)
        m["cache_k"] = f(cache_win_k[c * n_sseq:(c + 1) * n_sseq]).reshape(n_sseq, 2048, 512)
        m["cache_v"] = f(cache_win_v[c * n_sseq:(c + 1) * n_sseq]).reshape(n_sseq, 2048, 512)
        m["state_conv"] = f(state_conv[c * n_sseq:(c + 1) * n_sseq])
        m["state_ssm"] = f(state_ssm[c * n_sseq:(c + 1) * n_sseq])
        in_maps.append(m)
    res = run_bass_kernel_spmd(nc, in_maps, core_ids=list(range(N_CORES)))
    R = res.results
    cat = lambda name: np.concatenate([np.asarray(r[name]) for r in R], axis=0)
    y_prompt = cat("y_p").reshape(B, S, D)
    y_sample = cat("y_s").reshape(DB, 8, D)
    wk_p = cat("wk_p").reshape(B, 2048, 8, 64)
    wv_p = cat("wv_p").reshape(B, 2048, 8, 64)
    conv_p = cat("conv_p").reshape(B, 3, 1536)
    ssm_p = cat("ssm_p").reshape(B, 4, 128, 128)
    wk_s = cat("wk_s").reshape(DB, 8, 8, 64)
    wv_s = cat("wv_s").reshape(DB, 8, 8, 64)
    conv_s = cat("conv_s").reshape(DB, 3, 1536)
    ssm_s = cat("ssm_s").reshape(DB, 4, 128, 128)
    return (y_prompt, y_sample, wk_p, wv_p, conv_p, ssm_p, wk_s, wv_s, conv_s, ssm_s)
```

```python
import contextlib
import os
import numpy as np
import concourse.bass as bass
import concourse.mybir as mybir
from concourse.bass_utils import run_bass_kernel_spmd

F32 = mybir.dt.float32
BF16 = mybir.dt.bfloat16
AF = mybir.ActivationFunctionType
ALU = mybir.AluOpType
AX = mybir.AxisListType

EPOCH = 30000
ENGS = ("pe", "act", "dve", "pool", "sp")

SEQ = 4096
D = 1024
NIN = 3592
EPS = 1e-6
PATTERNS = (1, 4, 16)


class Prog:
    def __init__(self, nc):
        self.nc = nc
        self.ops = {e: [] for e in ENGS}
        self.count = {e: 0 for e in ENGS}
        self.dmacount = {}
        self.last_w = {}
        self.readers = {}
        self.waited = {e: {} for e in ENGS}
        self.pending = {e: {} for e in ENGS}
        self.semkeys = []
        self.semkey_set = set()
        self.excl = set()

    def _tok_sem(self, key):
        if key not in self.semkey_set:
            self.semkey_set.add(key)
            self.semkeys.append(key)

    def barrier(self):
        cur = {}
        for e in ENGS:
            c = self.count[e]
            if c:
                cur[("e", e, (c - 1) // EPOCH)] = (c - 1) % EPOCH + 1
        for k, c in self.dmacount.items():
            cur[("d", k)] = c
        for e in ENGS:
            p = self.pending[e]
            for k, v in cur.items():
                if p.get(k, 0) < v:
                    p[k] = v

    def op(self, eng, fn, reads=(), writes=(), dma=None):
        deps = dict(self.pending[eng])
        self.pending[eng] = {}
        writes = list(writes) + [r for r in reads if r in self.excl]

        def add(tok):
            if tok is None:
                return
            k, v = tok
            if deps.get(k, 0) < v:
                deps[k] = v

        for r in reads:
            add(self.last_w.get(r))
        for w in writes:
            add(self.last_w.get(w))
            for k, v in self.readers.get(w, {}).items():
                add((k, v))
        if dma is None:
            c = self.count[eng]
            self.count[eng] = c + 1
            tok = (("e", eng, c // EPOCH), c % EPOCH + 1)
        else:
            c = self.dmacount.get(dma, 0) + 16
            self.dmacount[dma] = c
            tok = (("d", dma), c)
        self._tok_sem(tok[0])
        waits = []
        wd = self.waited[eng]
        for k, v in deps.items():
            if eng == "pe" and k[0] == "e" and k[1] == "pe":
                continue
            if wd.get(k, 0) >= v:
                continue
            wd[k] = v
            waits.append((k, v))
        self.ops[eng].append((fn, waits, tok))
        for r in reads:
            d = self.readers.setdefault(r, {})
            if d.get(tok[0], 0) < tok[1]:
                d[tok[0]] = tok[1]
        for w in writes:
            self.last_w[w] = tok
            self.readers[w] = {}
        return tok

    def emit(self):
        nc = self.nc
        with contextlib.ExitStack() as es:
            sems = {}
            for i, k in enumerate(self.semkeys):
                sems[k] = es.enter_context(nc.semaphore("s%d" % i))
            final = []
            for e in ENGS:
                c = self.count[e]
                if c:
                    final.append((("e", e, (c - 1) // EPOCH), (c - 1) % EPOCH + 1))
            for k, c in self.dmacount.items():
                final.append((("d", k), c))
            block = es.enter_context(nc.Block())

            def run(engname, e):
                for fn, waits, tok in self.ops[engname]:
                    for k, v in waits:
                        e.wait_ge(sems[k], v)
                    ins = fn(e)
                    ins.then_inc(sems[tok[0]], 16 if tok[0][0] == "d" else 1)
                if engname == "sp":
                    for k, v in final:
                        e.wait_ge(sems[k], v)

            @block.tensor
            def _(e):
                run("pe", e)

            @block.scalar
            def _(e):
                run("act", e)

            @block.vector
            def _(e):
                run("dve", e)

            @block.gpsimd
            def _(e):
                run("pool", e)

            @block.sync
            def _(e):
                run("sp", e)


class Rot:
    def __init__(self, alloc, name, shape, dtype, n):
        self.bufs = [alloc(name + str(i), shape, dtype) for i in range(n)]
        self.keys = [name + str(i) for i in range(n)]
        self.i = 0

    def next(self):
        j = self.i % len(self.bufs)
        self.i += 1
        return self.bufs[j], self.keys[j]


def build_program(n_pseq, n_sseq, stages=("A", "B", "D"), debug=False, moe_g=512, n_exp=16):
    nc = bass.Bass("TRN2", target_bir_lowering=False)
    NP = n_pseq * SEQ
    NS = n_sseq * 8
    NT = NP + NS
    assert NS <= 128
    dk = "ExternalOutput" if debug else "Internal"

    def din(name, shape):
        return nc.dram_tensor(name, list(shape), F32, kind="ExternalInput").ap()

    def dout(name, shape):
        return nc.dram_tensor(name, list(shape), F32, kind="ExternalOutput").ap()

    xp = din("xp", [NP, D])
    xs = din("xs", [max(NS, 1), D])
    cache_k = din("cache_k", [max(n_sseq, 1), 2048, 512])
    cache_v = din("cache_v", [max(n_sseq, 1), 2048, 512])
    state_conv = din("state_conv", [max(n_sseq, 1), 3, 1536])
    state_ssm = din("state_ssm", [max(n_sseq, 1), 4, 128, 128])
    norm1_w = din("norm1_w", [D])
    w_in = din("w_in", [D, NIN])
    qnorm_w = din("qnorm_w", [64])
    knorm_w = din("knorm_w", [64])
    conv_w = din("conv_w", [4, 1536])
    a_log = din("a_log", [4])
    dt_bias = din("dt_bias", [4])
    onorm_w = din("onorm_w", [128])
    w_out = din("w_out", [D, D])
    norm2_w = din("norm2_w", [D])
    w_group = din("w_group", [D, 4])
    b_group = din("b_group", [4])
    w_er = din("w_er", [4, D, 4])
    b_er = din("b_er", [16])
    w_gate_up = din("w_gate_up", [16, D, 1024])
    w_down = din("w_down", [16, 512, D])

    y_p = dout("y_p", [NP, D])
    y_s = dout("y_s", [max(NS, 1), D])
    wk_p = dout("wk_p", [n_pseq, 2048, 512])
    wv_p = dout("wv_p", [n_pseq, 2048, 512])
    conv_p = dout("conv_p", [n_pseq, 3, 1536])
    ssm_p = dout("ssm_p", [n_pseq, 4, 128, 128])
    wk_s = dout("wk_s", [max(NS, 1), 512])
    wv_s = dout("wv_s", [max(NS, 1), 512])
    conv_s = dout("conv_s", [max(n_sseq, 1), 3, 1536])
    ssm_s = dout("ssm_s", [max(n_sseq, 1), 4, 128, 128])

    qT_d = nc.dram_tensor("qT_d", [4, 128, NT], BF16, kind=dk).ap()
    kT_d = nc.dram_tensor("kT_d", [4, 128, NT], BF16, kind=dk).ap()
    V_d = nc.dram_tensor("V_d", [NT, 640], BF16, kind=dk).ap()
    gT_d = nc.dram_tensor("gT_d", [12, 128, NT], BF16, kind=dk).ap()
    zbg_d = nc.dram_tensor("zbg_d", [NT, 520], F32, kind=dk).ap()
    oT_d = nc.dram_tensor("oT_d", [8, 128, NT], BF16, kind=dk).ap()

    wgu_bf = nc.dram_tensor("wgu_bf", [16, D, 1024], BF16, kind="Internal").ap()
    wd_bf = nc.dram_tensor("wd_bf", [16, 512, D], BF16, kind="Internal").ap()

    P = Prog(nc)
    es = contextlib.ExitStack()
    if "D" in stages:
        for ex in range(16):
            P.op("pool", lambda e, ex=ex: e.dma_start(out=wgu_bf[ex], in_=w_gate_up[ex]), writes=["wbf"], dma="wbf")
            P.op("pool", lambda e, ex=ex: e.dma_start(out=wd_bf[ex], in_=w_down[ex]), writes=["wbf"], dma="wbf")
    if debug:
        dbg_g = nc.dram_tensor("dbg_g", [NT // 128, 128, 16], F32, kind="ExternalOutput").ap()
        dbg_hn = nc.dram_tensor("dbg_hn", [8, 128, NT], BF16, kind="ExternalOutput").ap()
        dbg_h = nc.dram_tensor("dbg_h", [NT, D], F32, kind="ExternalOutput").ap()
        xdump = nc.dram_tensor("xdump", [NT // 512, 128, 8, 512], BF16, kind="ExternalOutput").ap()

    def sb(name, shape, dtype, stack=None):
        return (stack or es).enter_context(nc.sbuf_tensor(name, list(shape), dtype))

    def ps(name, shape, dtype, stack=None):
        P.excl.add(name)
        return (stack or es).enter_context(nc.psum_tensor(name, list(shape), dtype))

    ident = sb("ident", [128, 128], BF16)
    P.op("pool", lambda e: e.memset(ident[:], 0.0), writes=["ident"])
    P.op("pool", lambda e: e.affine_select(out=ident[:], in_=ident[:], pattern=[[-1, 128]],
                                           compare_op=ALU.not_equal, fill=1.0, base=0,
                                           channel_multiplier=1), reads=["ident"], writes=["ident"])
    n1col = sb("n1col", [128, 8], F32)
    n2col = sb("n2col", [128, 8], F32)
    identf = sb("identf", [128, 128], F32)
    P.op("pool", lambda e: e.memset(identf[:], 0.0), writes=["identf"])
    P.op("pool", lambda e: e.affine_select(out=identf[:], in_=identf[:], pattern=[[-1, 128]],
                                           compare_op=ALU.not_equal, fill=1.0, base=0,
                                           channel_multiplier=1), reads=["identf"], writes=["identf"])
    nrow = sb("nrow", [8, 2, 128], F32)
    P.op("sp", lambda e: e.dma_start(out=nrow[:, 0, :], in_=norm1_w.rearrange("(k p) -> k p", p=128)), writes=["nrow"], dma="c0")
    P.op("sp", lambda e: e.dma_start(out=nrow[:, 1, :], in_=norm2_w.rearrange("(k p) -> k p", p=128)), writes=["nrow"], dma="c0")
    with nc.psum_tensor("pN", [128, 16], F32) as pN:
        P.op("pe", lambda e: e.matmul(out=pN[:, 0:8], lhsT=nrow[:, 0, :], rhs=identf[0:8, 0:8], start=True, stop=True), reads=["nrow", "identf"], writes=["pN"])
        P.op("pe", lambda e: e.matmul(out=pN[:, 8:16], lhsT=nrow[:, 1, :], rhs=identf[0:8, 0:8], start=True, stop=True), reads=["nrow", "identf"], writes=["pN"])
        P.op("dve", lambda e: e.tensor_copy(out=n1col[:], in_=pN[:, 0:8]), reads=["pN"], writes=["n1col"])
        P.op("dve", lambda e: e.tensor_copy(out=n2col[:], in_=pN[:, 8:16]), reads=["pN"], writes=["n2col"])
    P.barrier()

    tiles = []
    for t in range(NP // 128):
        tiles.append(("p", t))
    if NS:
        tiles.append(("s", 0))

    def x_rows(kind, t, n=128):
        if kind == "p":
            return xp[t * 128:t * 128 + n, :]
        return xs[0:n, :]

    def gtok(kind, t):
        return t * 128 if kind == "p" else NP

    if "A" in stages:
        with contextlib.ExitStack() as pa:
            A = lambda name, shape, dtype: sb(name, shape, dtype, pa)
            win = A("win", [128, 8, NIN], BF16)
            wst_r = Rot(A, "wst", [128, NIN], F32, 2)
            for k in range(8):
                wst, wstk = wst_r.next()
                P.op("sp", lambda e, k=k, wst=wst: e.dma_start(out=wst[:], in_=w_in[k * 128:(k + 1) * 128, :]), writes=[wstk], dma=wstk)
                P.op("pool" if k % 2 else "dve", lambda e, k=k, wst=wst: e.tensor_copy(out=win[:, k, :], in_=wst[:]), reads=[wstk], writes=[("win", k)])
            winr = [("win", k) for k in range(8)]
            qw_rep = A("qw_rep", [128, 8, 64], F32)
            kw_rep = A("kw_rep", [128, 8, 64], F32)
            for h in range(8):
                P.op("sp", lambda e, h=h: e.dma_start(out=qw_rep[:, h, :], in_=qnorm_w.partition_broadcast(128)),
                     writes=["qw_rep"], dma="qwr")
                P.op("sp", lambda e, h=h: e.dma_start(out=kw_rep[:, h, :], in_=knorm_w.partition_broadcast(128)),
                     writes=["kw_rep"], dma="kwr")
            qwr = ["qw_rep"]
            kwr = ["kw_rep"]

            xt_r = Rot(A, "xt", [128, D], F32, 2)
            junk = A("junk", [128, D], BF16)
            ssq_r = Rot(A, "ssq", [128, 1], F32, 2)
            xn_r = Rot(A, "xn", [128, D], BF16, 2)
            xnT_r = Rot(A, "xnT", [128, 8, 512], BF16, 2)
            sq_r = Rot(A, "sq", [128, 512], F32, 2)
            ss8_r = Rot(A, "ss8", [128, 8], F32, 2)
            qn32_r = Rot(A, "qn32", [128, 8, 64], F32, 2)
            kn32_r = Rot(A, "kn32", [128, 8, 64], F32, 2)
            qnb_r = Rot(A, "qnb", [128, 512], BF16, 2)
            v32_r = Rot(A, "v32", [128, 512], F32, 2)
            vext_r = Rot(A, "vext", [128, 8, 80], BF16, 2)
            for vb, vk in zip(vext_r.bufs, vext_r.keys):
                P.op("pool", lambda e, vb=vb: e.memset(vb[:], 1.0), writes=[vk])
            zbg_r = Rot(A, "zbg", [128, 520], F32, 2)
            qT_r = Rot(A, "qTs", [128, 4, 512], BF16, 2)
            kT_r = Rot(A, "kTs", [128, 4, 512], BF16, 2)
            gT_r = Rot(A, "gTs", [128, 12, 512], BF16, 2)
            cv_r = Rot(A, "cv", [128, 1536], F32, 1)

            pp = contextlib.ExitStack()
            pT_r = Rot(lambda n, s, d: ps(n, s, d, pp), "pT", [128, 8, 128], BF16, 2)
            pM_r = Rot(lambda n, s, d: ps(n, s, d, pp), "pM", [128, 512], F32, 4)
            pS_r = Rot(lambda n, s, d: ps(n, s, d, pp), "pS", [128, 512], F32, 1)

            def rstd_from(ssq, key, scale):
                P.op("dve", lambda e: e.tensor_scalar(out=ssq, in0=ssq, scalar1=scale, scalar2=EPS,
                                                      op0=ALU.mult, op1=ALU.add), reads=[key], writes=[key])
                P.op("act", lambda e: e.activation(out=ssq, in_=ssq, func=AF.Sqrt), reads=[key], writes=[key])
                P.op("dve", lambda e: e.reciprocal(out=ssq, in_=ssq), reads=[key], writes=[key])

            def qk_norm(pm, pmk, wrep, wrk, out32, out32k):
                sq, sqk = sq_r.next()
                ss8, ss8k = ss8_r.next()
                P.op("act", lambda e: e.activation(out=sq[:], in_=pm[:], func=AF.Square), reads=[pmk], writes=[sqk])
                P.op("dve", lambda e: e.tensor_reduce(out=ss8[:], in_=sq[:].rearrange("p (h d) -> p h d", d=64),
                                                      axis=AX.X, op=ALU.add), reads=[sqk], writes=[ss8k])
                rstd_from(ss8[:], ss8k, 1.0 / 64)
                P.op("dve", lambda e: e.tensor_tensor(out=out32[:], in0=pm[:].rearrange("p (h d) -> p h d", d=64),
                                                      in1=ss8[:].unsqueeze(2).to_broadcast([128, 8, 64]), op=ALU.mult),
                     reads=[pmk, ss8k], writes=[out32k])
                P.op("dve", lambda e: e.tensor_tensor(out=out32[:], in0=out32[:], in1=wrep[:], op=ALU.mult),
                     reads=[out32k] + wrk, writes=[out32k])

            sts = []
            for s in range(n_pseq):
                for u in range(SEQ // 512):
                    sts.append([("p", s * 32 + u * 4 + j) for j in range(4)])
            if NS:
                sts.append([("s", 0)])
            if os.environ.get("MAXST"):
                sts = sts[-int(os.environ["MAXST"]):]
            for st in sts:
                nt = len(st)
                N = nt * 128
                g0 = gtok(*st[0])
                xnT, xnTk = xnT_r.next()
                qTs, qTk = qT_r.next()
                kTs, kTk = kT_r.next()
                for j, (kind, t) in enumerate(st):
                    nrows = 128 if kind == "p" else NS
                    xt, xtk = xt_r.next()
                    if nrows < 128:
                        P.op("pool", lambda e, xt=xt: e.memset(xt[:], 0.0), writes=[xtk])
                    P.op("sp", lambda e, xt=xt, kind=kind, t=t, nrows=nrows: e.dma_start(out=xt[0:nrows, :], in_=x_rows(kind, t, nrows)),
                         writes=[xtk], dma=xtk)
                    ssq, ssqk = ssq_r.next()
                    P.op("act", lambda e, xt=xt, ssq=ssq: e.activation(out=junk[:], in_=xt[:], func=AF.Square, accum_out=ssq[:]),
                         reads=[xtk], writes=["junk", ssqk])
                    rstd_from(ssq[:], ssqk, 1.0 / D)
                    xn, xnk = xn_r.next()
                    P.op("dve", lambda e, xn=xn, xt=xt, ssq=ssq: e.tensor_scalar(out=xn[:], in0=xt[:], scalar1=ssq[:], scalar2=None, op0=ALU.mult),
                         reads=[xtk, ssqk], writes=[xnk])
                    pT, pTk = pT_r.next()
                    for k in range(8):
                        P.op("pe", lambda e, pT=pT, xn=xn, k=k: e.transpose(out=pT[:, k, :], in_=xn[:, k * 128:(k + 1) * 128], identity=ident[:]),
                             reads=[xnk, "ident"], writes=[pTk])
                    P.op("dve", lambda e, pT=pT, xnT=xnT, j=j: e.tensor_tensor(
                        out=xnT[:, :, j * 128:(j + 1) * 128], in0=pT[:],
                        in1=n1col[:].unsqueeze(2).to_broadcast([128, 8, 128]), op=ALU.mult),
                        reads=[pTk, "n1col"], writes=[(xnTk, j)])
                    xr = [(xnTk, j)]
                    sl = slice(j * 128, (j + 1) * 128)
                    gt = gtok(kind, t)
                    CUT = int(os.environ.get("CUT", "9"))
                    if CUT < 2:
                        continue
                    pm, pmk = pM_r.next()
                    for k in range(8):
                        P.op("pe", lambda e, pm=pm, xnT=xnT, k=k, sl=sl: e.matmul(out=pm[:], lhsT=xnT[:, k, sl], rhs=win[:, k, 0:512], start=(k == 0), stop=(k == 7)),
                             reads=xr + winr, writes=[pmk])
                    qn32, qn32k = qn32_r.next()
                    qk_norm(pm, pmk, qw_rep, qwr, qn32, qn32k)
                    qnb, qnbk = qnb_r.next()
                    P.op("act", lambda e, qnb=qnb, qn32=qn32: e.copy(out=qnb[:], in_=qn32[:].rearrange("p h d -> p (h d)")), reads=[qn32k], writes=[qnbk])
                    pT, pTk = pT_r.next()
                    for c in range(4):
                        P.op("pe", lambda e, pT=pT, qnb=qnb, c=c: e.transpose(out=pT[:, c, :], in_=qnb[:, c * 128:(c + 1) * 128], identity=ident[:]),
                             reads=[qnbk, "ident"], writes=[pTk])
                    P.op("act", lambda e, pT=pT, qTs=qTs, sl=sl: e.copy(out=qTs[:, :, sl], in_=pT[:, 0:4, :]), reads=[pTk], writes=[(qTk, j)])
                    if CUT < 3:
                        continue
                    pm, pmk = pM_r.next()
                    for k in range(8):
                        P.op("pe", lambda e, pm=pm, xnT=xnT, k=k, sl=sl: e.matmul(out=pm[:], lhsT=xnT[:, k, sl], rhs=win[:, k, 512:1024], start=(k == 0), stop=(k == 7)),
                             reads=xr + winr, writes=[pmk])
                    kn32, kn32k = kn32_r.next()
                    qk_norm(pm, pmk, kw_rep, kwr, kn32, kn32k)
                    if kind == "p":
                        pos = (t * 128) % SEQ
                        if pos >= SEQ - 2048:
                            sidx = (t * 128) // SEQ
                            P.op("sp", lambda e, kn32=kn32, sidx=sidx, pos=pos: e.dma_start(
                                out=wk_p[sidx, pos - (SEQ - 2048):pos - (SEQ - 2048) + 128, :], in_=kn32[:].rearrange("p h d -> p (h d)")),
                                reads=[kn32k], dma=kn32k)
                    elif not os.environ.get("NOWK"):
                        P.op("sp", lambda e, kn32=kn32: e.dma_start(out=wk_s[0:NS, :], in_=kn32[0:NS].rearrange("p h d -> p (h d)")),
                             reads=[kn32k], dma=kn32k)
                    qnb, qnbk = qnb_r.next()
                    P.op("act", lambda e, qnb=qnb, kn32=kn32: e.copy(out=qnb[:], in_=kn32[:].rearrange("p h d -> p (h d)")), reads=[kn32k], writes=[qnbk])
                    pT, pTk = pT_r.next()
                    for c in range(4):
                        P.op("pe", lambda e, pT=pT, qnb=qnb, c=c: e.transpose(out=pT[:, c, :], in_=qnb[:, c * 128:(c + 1) * 128], identity=ident[:]),
                             reads=[qnbk, "ident"], writes=[pTk])
                    P.op("act", lambda e, pT=pT, kTs=kTs, sl=sl: e.copy(out=kTs[:, :, sl], in_=pT[:, 0:4, :]), reads=[pTk], writes=[(kTk, j)])
                    if CUT < 4:
                        continue
                    pm, pmk = pM_r.next()
                    for k in range(8):
                        P.op("pe", lambda e, pm=pm, xnT=xnT, k=k, sl=sl: e.matmul(out=pm[:], lhsT=xnT[:, k, sl], rhs=win[:, k, 1024:1536], start=(k == 0), stop=(k == 7)),
                             reads=xr + winr, writes=[pmk])
                    v32, v32k = v32_r.next()
                    P.op("act", lambda e, v32=v32, pm=pm: e.copy(out=v32[:], in_=pm[:]), reads=[pmk], writes=[v32k])
                    vext, vextk = vext_r.next()
                    P.op("dve", lambda e, vext=vext, v32=v32: e.tensor_copy(out=vext[:, :, 0:64], in_=v32[:].rearrange("p (h d) -> p h d", d=64)),
                         reads=[v32k], writes=[vextk])
                    if kind == "p":
                        pos = (t * 128) % SEQ
                        if pos >= SEQ - 2048:
                            sidx = (t * 128) // SEQ
                            P.op("sp", lambda e, v32=v32, sidx=sidx, pos=pos: e.dma_start(
                                out=wv_p[sidx, pos - (SEQ - 2048):pos - (SEQ - 2048) + 128, :], in_=v32[:]),
                                reads=[v32k], dma=v32k)
                    else:
                        P.op("sp", lambda e, v32=v32: e.dma_start(out=wv_s[0:NS, :], in_=v32[0:NS, :]), reads=[v32k], dma=v32k)
                    P.op("sp", lambda e, vext=vext, gt=gt: e.dma_start(out=V_d[gt:gt + 128, :], in_=vext[:].rearrange("p h d -> p (h d)")),
                         reads=[vextk], dma=vextk)
                    if CUT < 5:
                        continue
                    pm, pmk = pM_r.next()
                    for k in range(8):
                        P.op("pe", lambda e, pm=pm, xnT=xnT, k=k, sl=sl: e.matmul(out=pm[:], lhsT=xnT[:, k, sl], rhs=win[:, k, 3072:3584], start=(k == 0), stop=(k == 7)),
                             reads=xr + winr, writes=[pmk])
                    pS, pSk = pS_r.next()
                    for k in range(8):
                        P.op("pe", lambda e, pS=pS, xnT=xnT, k=k, sl=sl: e.matmul(out=pS[:, 0:8], lhsT=xnT[:, k, sl], rhs=win[:, k, 3584:3592], start=(k == 0), stop=(k == 7)),
                             reads=xr + winr, writes=[pSk])
                    zbg, zbgk = zbg_r.next()
                    P.op("act", lambda e, zbg=zbg, pm=pm: e.copy(out=zbg[:, 0:512], in_=pm[:]), reads=[pmk], writes=[(zbgk, 0)])
                    P.op("dve", lambda e, zbg=zbg, pS=pS: e.tensor_copy(out=zbg[:, 512:520], in_=pS[:, 0:8]), reads=[pSk], writes=[(zbgk, 1)])
                    P.op("sp", lambda e, zbg=zbg, gt=gt: e.dma_start(out=zbg_d[gt:gt + 128, :], in_=zbg[:]),
                         reads=[(zbgk, 0), (zbgk, 1)], dma=zbgk)
                    if CUT < 6:
                        continue
                    is_last = (kind == "p" and (t * 128) % SEQ == SEQ - 128) or kind == "s"
                    if is_last:
                        cv, cvk = cv_r.next()
                        for c3 in range(3):
                            pm, pmk = pM_r.next()
                            for k in range(8):
                                P.op("pe", lambda e, pm=pm, xnT=xnT, k=k, sl=sl, c3=c3: e.matmul(
                                    out=pm[:], lhsT=xnT[:, k, sl], rhs=win[:, k, 1536 + c3 * 512:1536 + (c3 + 1) * 512], start=(k == 0), stop=(k == 7)),
                                    reads=xr + winr, writes=[pmk])
                            P.op("act", lambda e, cv=cv, pm=pm, c3=c3: e.copy(out=cv[:, c3 * 512:(c3 + 1) * 512], in_=pm[:]), reads=[pmk], writes=[(cvk, c3)])
                        cvr = [(cvk, c3) for c3 in range(3)]
                        if kind == "p":
                            sidx = (t * 128) // SEQ
                            P.op("sp", lambda e, cv=cv, sidx=sidx: e.dma_start(out=conv_p[sidx], in_=cv[125:128, :]), reads=cvr, dma=cvk)
                        else:
                            for b in range(n_sseq):
                                P.op("sp", lambda e, cv=cv, b=b: e.dma_start(out=conv_s[b], in_=cv[b * 8 + 5:b * 8 + 8, :]), reads=cvr, dma=cvk)
                if CUT < 7:
                    continue
                gTs, gTk = gT_r.next()
                for j in range(nt):
                    sl = slice(j * 128, (j + 1) * 128)
                    for c4 in range(3):
                        pm, pmk = pM_r.next()
                        for cc in range(4):
                            c = c4 * 4 + cc
                            for k in range(8):
                                P.op("pe", lambda e, pm=pm, xnT=xnT, k=k, c=c, cc=cc, sl=sl: e.matmul(
                                    out=pm[:, cc * 128:(cc + 1) * 128], lhsT=win[:, k, 1536 + c * 128:1536 + (c + 1) * 128], rhs=xnT[:, k, sl], start=(k == 0), stop=(k == 7)),
                                    reads=[(xnTk, j)] + winr, writes=[pmk])
                        if c4 % 2 == 0:
                            P.op("act", lambda e, pm=pm, gTs=gTs, c4=c4, sl=sl: e.copy(out=gTs[:, c4 * 4:(c4 + 1) * 4, sl], in_=pm[:].rearrange("p (c n) -> p c n", n=128)), reads=[pmk], writes=[(gTk, c4, j)])
                        else:
                            P.op("dve", lambda e, pm=pm, gTs=gTs, c4=c4, sl=sl: e.tensor_copy(out=gTs[:, c4 * 4:(c4 + 1) * 4, sl], in_=pm[:].rearrange("p (c n) -> p c n", n=128)), reads=[pmk], writes=[(gTk, c4, j)])
                if debug and os.environ.get("SNAP") and N == 512:
                    P.op("sp", lambda e, xnT=xnT, g0=g0: e.dma_start(out=xdump[g0 // 512], in_=xnT[:]), reads=[(xnTk, j) for j in range(nt)], dma="xdump")
                P.op("sp", lambda e, gTs=gTs, g0=g0, N=N: e.dma_start(out=gT_d[:, :, g0:g0 + N].rearrange("c p n -> p c n"), in_=gTs[:, :, 0:N]),
                     reads=[(gTk, c4, j) for c4 in range(3) for j in range(nt)], dma=gTk)
                P.op("sp", lambda e, qTs=qTs, g0=g0, N=N: e.dma_start(out=qT_d[:, :, g0:g0 + N].rearrange("c p n -> p c n"), in_=qTs[:, :, 0:N]),
                     reads=[(qTk, j) for j in range(nt)], dma=qTk)
                P.op("sp", lambda e, kTs=kTs, g0=g0, N=N: e.dma_start(out=kT_d[:, :, g0:g0 + N].rearrange("c p n -> p c n"), in_=kTs[:, :, 0:N]),
                     reads=[(kTk, j) for j in range(nt)], dma=kTk)
            pp.close()
        P.barrier()
        if debug and os.environ.get("SNAP"):
            snap = nc.dram_tensor("snap", [12, 128, NT], BF16, kind="ExternalOutput").ap()
            P.op("sp", lambda e: e.dma_start(out=snap, in_=gT_d), dma="snap")
            P.barrier()

    if "B" in stages:
        with contextlib.ExitStack() as pb:
            Bk = lambda name, shape, dtype: sb(name, shape, dtype, pb)
            mask = Bk("mask", [128, 256], BF16)
            P.op("pool", lambda e: e.memset(mask[:], 1.0), writes=["mask"])
            P.op("pool", lambda e: e.affine_select(out=mask[:, 0:128], in_=mask[:, 0:128], pattern=[[-1, 128]], compare_op=ALU.is_ge,
                                                   fill=0.0, base=0, channel_multiplier=1), reads=["mask"], writes=["mask"])
            P.op("pool", lambda e: e.affine_select(out=mask[:, 128:256], in_=mask[:, 128:256], pattern=[[1, 128]], compare_op=ALU.is_ge,
                                                   fill=0.0, base=0, channel_multiplier=-1), reads=["mask"], writes=["mask"])
            negm = Bk("negm", [128, 1], F32)
            P.op("pool", lambda e: e.memset(negm[:], -8.0), writes=["negm"])
            ones_r = Bk("ones_r", [65, 64], F32)
            P.op("pool", lambda e: e.memset(ones_r[:], 0.0), writes=["ones_r"])
            P.op("pool", lambda e: e.memset(ones_r[64:65, :], 1.0), reads=["ones_r"], writes=["ones_r"])
            qTb_r = Rot(Bk, "qTb", [128, SEQ], BF16, 2)
            kTb_r = Rot(Bk, "kTb", [128, SEQ], BF16, 2)
            vf_r = Rot(Bk, "vf", [128, 32, 2, 80], BF16, 2)
            acc = Bk("acc", [65, 2, SEQ], F32)
            rl = Bk("rl", [65, 2, SEQ], F32)
            P.op("pool", lambda e: e.memset(rl[:], 0.0), writes=["rl"])
            pt_r = Rot(Bk, "pt", [128, 2, 256], BF16, 4)
            oo_r = Rot(Bk, "oo", [64, SEQ], BF16, 2)
            pp = contextlib.ExitStack()
            pS_r = Rot(lambda n, s, d: ps(n, s, d, pp), "pSb", [128, 2, 512], F32, 3)
            pO_r = Rot(lambda n, s, d: ps(n, s, d, pp), "pOb", [128, 512], F32, 2)
            pB_r = pO_r
            for s in range(n_pseq):
                t0 = s * SEQ
                for hp in range(4):
                    qTb, qTbk = qTb_r.next()
                    kTb, kTbk = kTb_r.next()
                    P.op("sp", lambda e, qTb=qTb, hp=hp, t0=t0: e.dma_start(out=qTb[:], in_=qT_d[hp, :, t0:t0 + SEQ]), writes=[qTbk], dma=qTbk)
                    P.op("sp", lambda e, kTb=kTb, hp=hp, t0=t0: e.dma_start(out=kTb[:], in_=kT_d[hp, :, t0:t0 + SEQ]), writes=[kTbk], dma=kTbk)
                    for pi, d in enumerate(PATTERNS):
                        nb = 32 // d
                        vf, vfk = vf_r.next()
                        src = V_d[t0:t0 + SEQ, hp * 160:(hp + 1) * 160].rearrange("(b p r) f -> p r b f", p=128, r=d)
                        for r in range(d):
                            P.op("sp", lambda e, vf=vf, src=src, r=r, nb=nb: e.dma_start(
                                out=vf[:, r * nb:(r + 1) * nb, :, :].rearrange("p b h f -> p b (h f)"), in_=src[:, r, :, :]),
                                writes=[vfk], dma=vfk)
                        for r in range(d):
                            for b in range(nb):
                                c0 = r + 128 * b * d
                                qc = slice(c0, c0 + 127 * d + 1, d)
                                pc = slice(c0 - 128 * d, c0 - d + 1, d)
                                pS, pSk = pS_r.next()
                                for hh in range(2):
                                    rs = slice(64 * hh, 64 * hh + 64)
                                    if b > 0:
                                        P.op("pe", lambda e, pS=pS, hh=hh, rs=rs, pc=pc, qc=qc, kTb=kTb, qTb=qTb: e.matmul(
                                            out=pS[:, hh, 0:128], lhsT=kTb[rs, pc], rhs=qTb[rs, qc], start=True, stop=True),
                                            reads=[kTbk, qTbk], writes=[pSk])
                                    P.op("pe", lambda e, pS=pS, hh=hh, rs=rs, qc=qc, kTb=kTb, qTb=qTb: e.matmul(
                                        out=pS[:, hh, 128:256], lhsT=kTb[rs, qc], rhs=qTb[rs, qc], start=True, stop=True),
                                        reads=[kTbk, qTbk], writes=[pSk])
                                pt, ptk = pt_r.next()
                                cs = slice(0, 256) if b > 0 else slice(128, 256)
                                P.op("act", lambda e, pt=pt, pS=pS, cs=cs: e.activation(out=pt[:, :, cs], in_=pS[:, :, cs], func=AF.Exp, bias=negm[:], scale=0.125),
                                     reads=[pSk, "negm"], writes=[ptk])
                                ncs = 256 if b > 0 else 128
                                P.op("dve", lambda e, pt=pt, cs=cs, ncs=ncs: e.tensor_tensor(
                                    out=pt[:, :, cs], in0=pt[:, :, cs], in1=mask[:, cs].unsqueeze(1).to_broadcast([128, 2, ncs]), op=ALU.mult),
                                    reads=[ptk, "mask"], writes=[ptk])
                                pO, pOk = pO_r.next()
                                for hh in range(2):
                                    if b > 0:
                                        P.op("pe", lambda e, pO=pO, hh=hh, vf=vf, pt=pt, r=r, b=b, nb=nb: e.matmul(
                                            out=pO[0:65, hh * 128:(hh + 1) * 128], lhsT=vf[:, r * nb + b - 1, hh, 0:65], rhs=pt[:, hh, 0:128], start=True, stop=False),
                                            reads=[vfk, ptk], writes=[pOk])
                                    P.op("pe", lambda e, pO=pO, hh=hh, vf=vf, pt=pt, r=r, b=b, nb=nb: e.matmul(
                                        out=pO[0:65, hh * 128:(hh + 1) * 128], lhsT=vf[:, r * nb + b, hh, 0:65], rhs=pt[:, hh, 128:256], start=(b == 0), stop=True),
                                        reads=[vfk, ptk], writes=[pOk])
                                if pi == 0:
                                    P.op("dve", lambda e, pO=pO, qc=qc: e.tensor_copy(out=acc[:, :, qc], in_=pO[0:65, 0:256].rearrange("p (h q) -> p h q", h=2)), reads=[pOk], writes=["acc"])
                                else:
                                    P.op("dve", lambda e, pO=pO, qc=qc: e.tensor_tensor(out=acc[:, :, qc], in0=acc[:, :, qc], in1=pO[0:65, 0:256].rearrange("p (h q) -> p h q", h=2), op=ALU.add),
                                         reads=[pOk, "acc"], writes=["acc"])
                    P.op("dve", lambda e: e.reciprocal(out=rl[64:65, :, :], in_=acc[64:65, :, :]), reads=["acc"], writes=["rl"])
                    for hh in range(2):
                        oo, ook = oo_r.next()
                        for cb in range(SEQ // 512):
                            cs = slice(cb * 512, (cb + 1) * 512)
                            pB, pBk = pB_r.next()
                            P.op("pe", lambda e, pB=pB, hh=hh, cs=cs: e.matmul(out=pB[0:64, :], lhsT=ones_r[:, :], rhs=rl[:, hh, cs], start=True, stop=True),
                                 reads=["rl", "ones_r"], writes=[pBk])
                            P.op("dve", lambda e, pB=pB, oo=oo, hh=hh, cs=cs: e.tensor_tensor(out=oo[:, cs], in0=acc[0:64, hh, cs], in1=pB[0:64, :], op=ALU.mult),
                                 reads=[pBk, "acc"], writes=[(ook, cb)])
                        h = 2 * hp + hh
                        P.op("sp", lambda e, oo=oo, h=h, t0=t0: e.dma_start(out=oT_d[h // 2, (h % 2) * 64:(h % 2) * 64 + 64, t0:t0 + SEQ], in_=oo[:]),
                             reads=[(ook, cb) for cb in range(SEQ // 512)], dma=ook)
            pp.close()
        P.barrier()

    if "E" in stages and NS:
        with contextlib.ExitStack() as pe_:
            Ek = lambda name, shape, dtype: sb(name, shape, dtype, pe_)
            pp = contextlib.ExitStack()
            pTe_r = Rot(lambda n, s_, d: ps(n, s_, d, pp), "pTe", [128, 4, 128], F32, 2)
            pSc = [ps("pSc%d" % hh, [128, 16, 4, 8], F32, pp) for hh in range(2)]
            pSn = [ps("pSn%d" % hh, [128, 512], F32, pp) for hh in range(2)]
            pOe_full = ps("pOe", [128, 512], F32, pp)
            pOe = pOe_full[0:65, 0:64].rearrange("p (h q) -> p h q", q=8)
            pBe_full = ps("pBe", [128, 512], F32, pp)
            pBe = pBe_full[0:64, 0:64]
            I32 = mybir.dt.int32
            Di = Ek("Di", [128, 17, 8], I32)
            Dt = Ek("Dt", [128, 17, 8], I32)
            Df = Ek("Df", [128, 17, 8], F32)
            cm1 = Ek("cm1", [128, 17, 8], F32)
            cm2 = Ek("cm2", [128, 17, 8], F32)
            cnt = Ek("cnt", [128, 17, 8], BF16)
            P.op("pool", lambda e: e.iota(Di[:], pattern=[[-128, 17], [1, 8]], base=2048, channel_multiplier=-1), writes=["Di"])
            P.op("dve", lambda e: e.tensor_copy(out=Df[:], in_=Di[:]), reads=["Di"], writes=["Df"])
            P.op("dve", lambda e: e.tensor_scalar(out=cm1[:], in0=Df[:], scalar1=128.0, scalar2=None, op0=ALU.is_le), reads=["Df"], writes=["cm1"])
            P.op("dve", lambda e: e.tensor_single_scalar(out=Dt[:], in_=Di[:], scalar=3, op=ALU.bitwise_and), reads=["Di"], writes=["Dt"])
            P.op("dve", lambda e: e.tensor_scalar(out=cm2[:], in0=Dt[:], scalar1=0.0, scalar2=None, op0=ALU.is_equal), reads=["Dt"], writes=["cm2"])
            P.op("dve", lambda e: e.scalar_tensor_tensor(out=cm2[:], in0=Df[:], scalar=512.0, in1=cm2[:], op0=ALU.is_le, op1=ALU.mult), reads=["Df", "cm2"], writes=["cm2"])
            P.op("dve", lambda e: e.tensor_tensor(out=cm1[:], in0=cm1[:], in1=cm2[:], op=ALU.add), reads=["cm1", "cm2"], writes=["cm1"])
            P.op("dve", lambda e: e.tensor_single_scalar(out=Dt[:], in_=Di[:], scalar=15, op=ALU.bitwise_and), reads=["Di", "Dt"], writes=["Dt"])
            P.op("dve", lambda e: e.tensor_scalar(out=cm2[:], in0=Dt[:], scalar1=0.0, scalar2=None, op0=ALU.is_equal), reads=["Dt", "cm2"], writes=["cm2"])
            P.op("dve", lambda e: e.tensor_tensor(out=cm1[:], in0=cm1[:], in1=cm2[:], op=ALU.add), reads=["cm1", "cm2"], writes=["cm1"])
            P.op("dve", lambda e: e.scalar_tensor_tensor(out=cnt[:], in0=Df[:], scalar=0.0, in1=cm1[:], op0=ALU.is_ge, op1=ALU.mult), reads=["Df", "cm1"], writes=["cnt"])
            negme = Ek("negme", [128, 1], F32)
            P.op("pool", lambda e: e.memset(negme[:], -8.0), writes=["negme"])
            sele = Ek("sele", [65, 64], F32)
            P.op("pool", lambda e: e.memset(sele[:], 0.0), writes=["sele"])
            P.op("pool", lambda e: e.memset(sele[64:65, :], 1.0), reads=["sele"], writes=["sele"])
            rle = Ek("rle", [65, 64], F32)
            P.op("pool", lambda e: e.memset(rle[:], 0.0), writes=["rle"])
            kc32 = Ek("kc32", [128, 16, 512], F32)
            vc32 = Ek("vc32", [128, 16, 512], F32)
            kcT = Ek("kcT", [128, 4, 2056], BF16)
            Vc = Ek("Vc", [128, 17, 8, 80], BF16)
            P.op("pool", lambda e: e.memset(Vc[:, 16, :, :], 0.0), writes=["Vc16"])
            for t in range(16):
                P.op("pool", lambda e, t=t: e.memset(Vc[:, t, :, 64:65], 1.0), writes=[("Vc", t)])
            qs_r = Rot(Ek, "qse", [128, 4, 8], BF16, 2)
            Pe = [Ek("Pe%d" % hh, [128, 16, 4, 8], BF16) for hh in range(2)]
            Pn = [Ek("Pne%d" % hh, [8, 4, 8], BF16) for hh in range(2)]
            ou = Ek("oue", [64, 64], F32)
            osall = Ek("osall", [64, 8, 128], BF16)
            for b in range(n_sseq):
                c0 = NP + 8 * b
                P.op("sp", lambda e, b=b: e.dma_start(out=kc32[:], in_=cache_k[b].rearrange("(t p) f -> p t f", p=128)), writes=["kc32"], dma="kc32")
                P.op("sp", lambda e, b=b: e.dma_start(out=vc32[:], in_=cache_v[b].rearrange("(t p) f -> p t f", p=128)), writes=["vc32"], dma="vc32")
                P.op("sp", lambda e, c0=c0: e.dma_start(out=kcT[:, :, 2048:2056], in_=kT_d[:, :, c0:c0 + 8].rearrange("c p n -> p c n")), writes=["kcTn"], dma="kcTn")
                P.op("sp", lambda e, c0=c0: e.dma_start(out=Vc[0:8, 16, :, :].rearrange("p h f -> p (h f)"), in_=V_d[c0:c0 + 8, :]), reads=["Vc16"], writes=["Vc16"], dma="Vc16")
                qs, qsk = qs_r.next()
                P.op("sp", lambda e, qs=qs, c0=c0: e.dma_start(out=qs[:], in_=qT_d[:, :, c0:c0 + 8].rearrange("c p n -> p c n")), writes=[qsk], dma=qsk)
                for t in range(16):
                    pT, pTk = pTe_r.next()
                    for c in range(4):
                        P.op("pe", lambda e, pT=pT, t=t, c=c: e.transpose(out=pT[:, c, :], in_=kc32[:, t, c * 128:(c + 1) * 128], identity=identf[:]), reads=["kc32", "identf"], writes=[pTk])
                    if t % 2 == 0:
                        P.op("act", lambda e, pT=pT, t=t: e.copy(out=kcT[:, :, t * 128:(t + 1) * 128], in_=pT[:]), reads=[pTk], writes=[("kcT", t)])
                    else:
                        P.op("dve", lambda e, pT=pT, t=t: e.tensor_copy(out=kcT[:, :, t * 128:(t + 1) * 128], in_=pT[:]), reads=[pTk], writes=[("kcT", t)])
                    P.op("pool" if t % 2 == 0 else ("dve" if t % 4 == 1 else "act"), (lambda e, t=t: e.tensor_copy(out=Vc[:, t, :, 0:64], in_=vc32[:, t, :].rearrange("p (h d) -> p h d", d=64))) if t % 4 != 3 else
                         (lambda e, t=t: e.copy(out=Vc[:, t, :, 0:64], in_=vc32[:, t, :].rearrange("p (h d) -> p h d", d=64))), reads=["vc32", ("Vc", t)], writes=[("Vc", t)])
                kr = [("kcT", t) for t in range(16)]
                for hp in range(4):
                    for hh in range(2):
                        rs = slice(64 * hh, 64 * hh + 64)
                        for t in range(16):
                            P.op("pe", lambda e, hp=hp, hh=hh, rs=rs, t=t, qs=qs: e.matmul(out=pSc[hh][:, t, hp, :], lhsT=kcT[rs, hp, t * 128:(t + 1) * 128], rhs=qs[rs, hp, :], start=True, stop=True),
                                 reads=[("kcT", t), qsk], writes=["pSc%d" % hh])
                        P.op("pe", lambda e, hp=hp, hh=hh, rs=rs, qs=qs: e.matmul(out=pSn[hh][0:8, hp * 8:(hp + 1) * 8], lhsT=kcT[rs, hp, 2048:2056], rhs=qs[rs, hp, :], start=True, stop=True),
                             reads=["kcTn", qsk], writes=["pSn%d" % hh])
                for hh in range(2):
                    P.op("act", lambda e, hh=hh: e.activation(out=Pe[hh][:], in_=pSc[hh][:], func=AF.Exp, bias=negme[:], scale=0.125), reads=["pSc%d" % hh, "negme"], writes=["Pe%d" % hh])
                    P.op("pool", lambda e, hh=hh: e.tensor_tensor(out=Pe[hh][:], in0=Pe[hh][:], in1=cnt[:, 0:16, :].unsqueeze(2).to_broadcast([128, 16, 4, 8]), op=ALU.mult),
                         reads=["Pe%d" % hh, "cnt"], writes=["Pe%d" % hh])
                    P.op("act", lambda e, hh=hh: e.activation(out=Pn[hh][:].rearrange("p a q -> p (a q)"), in_=pSn[hh][0:8, 0:32], func=AF.Exp, bias=negme[0:8, :], scale=0.125),
                         reads=["pSn%d" % hh, "negme"], writes=["Pne%d" % hh])
                    P.op("pool", lambda e, hh=hh: e.tensor_tensor(out=Pn[hh][:], in0=Pn[hh][:], in1=cnt[0:8, 16, :].unsqueeze(1).to_broadcast([8, 4, 8]), op=ALU.mult),
                         reads=["Pne%d" % hh, "cnt"], writes=["Pne%d" % hh])
                for h in range(8):
                    hp, hh = h // 2, h % 2
                    for t in range(16):
                        P.op("pe", lambda e, h=h, hp=hp, hh=hh, t=t: e.matmul(out=pOe[:, h, :], lhsT=Vc[:, t, h, 0:65], rhs=Pe[hh][:, t, hp, :], start=(t == 0), stop=False),
                             reads=[("Vc", t), "Pe%d" % hh], writes=["pOe"])
                    P.op("pe", lambda e, h=h, hp=hp, hh=hh: e.matmul(out=pOe[:, h, :], lhsT=Vc[0:8, 16, h, 0:65], rhs=Pn[hh][:, hp, :], start=False, stop=True),
                         reads=["Vc16", "Pne%d" % hh], writes=["pOe"])
                P.op("dve", lambda e: e.reciprocal(out=rle[64:65, :], in_=pOe[64:65, :, :].rearrange("p h q -> p (h q)")), reads=["pOe", "rle"], writes=["rle"])
                P.op("act", lambda e: e.copy(out=ou[:], in_=pOe[0:64, :, :].rearrange("p h q -> p (h q)")), reads=["pOe"], writes=["oue"])
                P.op("pe", lambda e: e.matmul(out=pBe[:], lhsT=sele[:], rhs=rle[:], start=True, stop=True), reads=["sele", "rle"], writes=["pBe"])
                P.op("dve", lambda e, b=b: e.tensor_tensor(out=osall[:, :, 8 * b:8 * b + 8], in0=ou[:].rearrange("p (h q) -> p h q", q=8), in1=pBe[:].rearrange("p (h q) -> p h q", q=8), op=ALU.mult),
                     reads=["oue", "pBe"], writes=[("osall", b)])
            for h in range(8):
                P.op("sp", lambda e, h=h: e.dma_start(out=oT_d[h // 2, (h % 2) * 64:(h % 2) * 64 + 64, NP:NP + NS], in_=osall[:, h, :]), reads=[("osall", b) for b in range(n_sseq)], dma="osall")
            pp.close()
        P.barrier()

    if "C" in stages:
        with contextlib.ExitStack() as pc:
            Ck = lambda name, shape, dtype: sb(name, shape, dtype, pc)
            pp = contextlib.ExitStack()
            pC_r = Rot(lambda n, s_, d: ps(n, s_, d, pp), "pC", [128, 512], F32, 1)
            crow = Ck("crow", [4, 1536], F32)
            P.op("sp", lambda e: e.dma_start(out=crow[:], in_=conv_w), writes=["crow"], dma="crow")
            cw = Ck("cw", [128, 12, 4], F32)
            pcw, pcwk = pC_r.next()
            for c in range(12):
                P.op("pe", lambda e, c=c, pcw=pcw: e.matmul(out=pcw[:, c * 4:(c + 1) * 4], lhsT=crow[0:4, c * 128:(c + 1) * 128], rhs=identf[0:4, 0:4], start=True, stop=True),
                     reads=["crow", "identf"], writes=[pcwk])
            P.op("dve", lambda e, pcw=pcw: e.tensor_copy(out=cw[:].rearrange("p c i -> p (c i)"), in_=pcw[:, 0:48]), reads=[pcwk], writes=["cw"])
            arep = Ck("arep", [128, 4], F32)
            dtrep = Ck("dtrep", [128, 4], F32)
            onrep = Ck("onrep", [128, 128], F32)
            P.op("sp", lambda e: e.dma_start(out=arep[:], in_=a_log.partition_broadcast(128)), writes=["arep"], dma="arep")
            P.op("sp", lambda e: e.dma_start(out=dtrep[:], in_=dt_bias.partition_broadcast(128)), writes=["dtrep"], dma="dtrep")
            P.op("sp", lambda e: e.dma_start(out=onrep[:], in_=onorm_w.partition_broadcast(128)), writes=["onrep"], dma="onrep")
            P.op("act", lambda e: e.activation(out=arep[:], in_=arep[:], func=AF.Exp), reads=["arep"], writes=["arep"])
            P.op("dve", lambda e: e.tensor_scalar(out=arep[:], in0=arep[:], scalar1=-1.0, scalar2=None, op0=ALU.mult), reads=["arep"], writes=["arep"])
            ones_bf = Ck("ones_bf", [128, 128], BF16)
            P.op("pool", lambda e: e.memset(ones_bf[:], 1.0), writes=["ones_bf"])
            ones_f = Ck("ones_f", [128, 128], F32)
            P.op("pool", lambda e: e.memset(ones_f[:], 1.0), writes=["ones_f"])

            def make_masks(bs, tag):
                nb_ = 128 // bs
                Em = Ck("Em" + tag, [nb_, 128], F32)
                P.op("pool", lambda e: e.memset(Em[:], 1.0), writes=["Em" + tag])
                P.op("pool", lambda e: e.affine_select(out=Em[:], in_=Em[:], pattern=[[1, 128]], compare_op=ALU.is_ge, fill=0.0, base=0, channel_multiplier=-bs),
                     reads=["Em" + tag], writes=["Em" + tag])
                P.op("pool", lambda e: e.affine_select(out=Em[:], in_=Em[:], pattern=[[-1, 128]], compare_op=ALU.is_ge, fill=0.0, base=bs - 1, channel_multiplier=bs),
                     reads=["Em" + tag], writes=["Em" + tag])
                pb_, pbk_ = pC_r.next()
                P.op("pe", lambda e: e.matmul(out=pb_[:, 0:128], lhsT=Em[:], rhs=Em[:], start=True, stop=True), reads=["Em" + tag], writes=[pbk_])
                Bones = Ck("Bones" + tag, [128, 128], F32)
                P.op("dve", lambda e: e.tensor_copy(out=Bones[:], in_=pb_[:, 0:128]), reads=[pbk_], writes=["Bones" + tag])
                outs = []
                for nm, pat, cm, base in (("U", [[1, 128]], -1, 0), ("Mi", [[-1, 128]], 1, 0), ("Ms", [[-1, 128]], 1, -1)):
                    t_ = Ck(nm + tag, [128, 128], F32)
                    k_ = nm + tag
                    P.op("pool", lambda e, t_=t_: e.memset(t_[:], 1.0), writes=[k_])
                    P.op("pool", lambda e, t_=t_, pat=pat, cm=cm, base=base: e.affine_select(out=t_[:], in_=t_[:], pattern=pat, compare_op=ALU.is_ge, fill=0.0, base=base, channel_multiplier=cm),
                         reads=[k_], writes=[k_])
                    P.op("pool", lambda e, t_=t_: e.tensor_tensor(out=t_[:], in0=t_[:], in1=Bones[:], op=ALU.mult), reads=[k_, "Bones" + tag], writes=[k_])
                    outs.append((t_, k_))
                return (Bones, "Bones" + tag), outs[0], outs[1], outs[2]

            (Bones, Bonesk), (Um, Uk), (Mi, Mik), (Ms, Msk) = make_masks(64, "p")

            yc_r = Rot(Ck, "yc", [128, 512], F32, 3)
            ex_r = Rot(Ck, "exc", [128, 512], F32, 3)
            sqb_r = Rot(Ck, "sqb", [128, 512], BF16, 2)
            rs_r = Rot(Ck, "rsc", [128, 512], F32, 2)
            sm_r = Rot(Ck, "smc", [128, 40], F32, 2)
            otok_r = Rot(Ck, "otok", [128, 4, 128], F32, 2)
            H = range(4)
            Sst4 = Ck("Sst4", [128, 4, 128], F32)
            Sbf4 = Ck("Sbf4", [128, 4, 128], BF16)
            sq4_r = Rot(Ck, "sq4", [128, 512], F32, 2)
            ss4_r = Rot(Ck, "ss4", [128, 4], F32, 2)
            on_r = Rot(Ck, "onc", [128, 4, 128], F32, 2)
            zs_r = Rot(Ck, "zsc", [128, 512], F32, 2)
            ob_r = Rot(Ck, "obc", [128, 512], BF16, 2)

            def silu_from(yc, yck, nn=512):
                ex, exk = ex_r.next()
                P.op("act", lambda e: e.activation(out=ex[:, 0:nn], in_=yc[:, 0:nn], func=AF.Exp, scale=-1.0), reads=[yck], writes=[exk])
                P.op("dve", lambda e: e.tensor_scalar(out=ex[:, 0:nn], in0=ex[:, 0:nn], scalar1=1.0, scalar2=None, op0=ALU.add), reads=[exk], writes=[exk])
                P.op("dve", lambda e: e.reciprocal(out=ex[:, 0:nn], in_=ex[:, 0:nn]), reads=[exk], writes=[exk])
                return ex, exk

            def rstd_small(ap, key, scale):
                P.op("dve", lambda e: e.tensor_scalar(out=ap, in0=ap, scalar1=scale, scalar2=EPS, op0=ALU.mult, op1=ALU.add), reads=[key], writes=[key])
                P.op("act", lambda e: e.activation(out=ap, in_=ap, func=AF.Sqrt), reads=[key], writes=[key])
                P.op("dve", lambda e: e.reciprocal(out=ap, in_=ap), reads=[key], writes=[key])

            ones_f4 = Ck("ones_f4", [128, 4, 128], F32)
            P.op("pool", lambda e: e.memset(ones_f4[:], 1.0), writes=["ones_f4"])
            grep4_r = Rot(Ck, "grep4", [128, 4, 128], F32, 2)
            Erow4_r = Rot(Ck, "Erow4", [128, 4, 128], F32, 2)
            t4_r = Rot(Ck, "t4", [128, 4, 128], F32, 2)
            Gs4_r = Rot(Ck, "Gs4", [128, 4, 128], F32, 2)
            Gi4_r = Rot(Ck, "Gi4", [128, 4, 128], F32, 2)
            qd4_r = Rot(Ck, "qd4", [128, 4, 128], BF16, 2)
            A4_r = Rot(Ck, "A4", [128, 4, 128], BF16, 2)
            at4_r = Rot(Ck, "at4", [128, 4, 128], BF16, 2)
            attnT4_r = Rot(Ck, "attnT4", [128, 4, 128], BF16, 2)
            Pm4_r = Rot(Ck, "Pm4", [128, 4, 128], BF16, 3)
            PmT4_r = Rot(Ck, "PmT4", [128, 4, 128], BF16, 3)
            R4_r = Rot(Ck, "R4", [128, 4, 128], BF16, 3)
            kbd4_r = Rot(Ck, "kbd4", [128, 4, 128], BF16, 2)
            kd4_r = Rot(Ck, "kd4", [128, 4, 128], BF16, 2)
            vb4_r = Rot(Ck, "vb4", [128, 4, 128], BF16, 2)
            wT4_r = Rot(Ck, "wT4", [128, 4, 128], BF16, 2)
            u4_r = Rot(Ck, "u4", [128, 4, 128], F32, 2)
            vn4_r = Rot(Ck, "vn4", [128, 4, 128], BF16, 2)
            pC4_r = Rot(lambda n, s_, d: ps(n, s_, d, pp), "pC4", [128, 4, 128], F32, 5)
            _pCb4_full = Rot(lambda n, s_, d: ps(n, s_, d, pp), "pCb4", [128, 8, 128], BF16, 2)

            class _HalfRot:
                def next(self_inner):
                    t_, k_ = _pCb4_full.next()
                    return t_[:, 0:4, :], k_
            pCb4_r = _HalfRot()

            def bc(ap_p4):
                return ap_p4.unsqueeze(2).to_broadcast([128, 4, 128])

            def gdn_tile(qnT, qnTk, knT, knTk, vT, vTk, sl, zb, zbk, states, blocks, masks, valid=None):
                (Bones_, Bonesk_), (Um_, Uk_), (Mi_, Mik_), (Ms_, Msk_) = masks
                qk_ = [(qnTk, h) for h in H]
                kk_ = [(knTk, h) for h in H]
                vk_ = [(vTk, h) for h in H]
                sm, smk = sm_r.next()
                P.op("act", lambda e: e.activation(out=sm[:, 0:4], in_=zb[:, 512:516], func=AF.Exp, scale=-1.0), reads=[zbk], writes=[smk])
                P.op("dve", lambda e: e.tensor_scalar(out=sm[:, 0:4], in0=sm[:, 0:4], scalar1=1.0, scalar2=None, op0=ALU.add), reads=[smk], writes=[smk])
                P.op("dve", lambda e: e.reciprocal(out=sm[:, 0:4], in_=sm[:, 0:4]), reads=[smk], writes=[smk])
                P.op("dve", lambda e: e.tensor_tensor(out=sm[:, 8:12], in0=zb[:, 516:520], in1=dtrep[:], op=ALU.add), reads=[zbk, "dtrep", smk], writes=[smk])
                P.op("act", lambda e: e.activation(out=sm[:, 8:12], in_=sm[:, 8:12], func=AF.Exp), reads=[smk], writes=[smk])
                P.op("dve", lambda e: e.tensor_scalar(out=sm[:, 8:12], in0=sm[:, 8:12], scalar1=1.0, scalar2=None, op0=ALU.add), reads=[smk], writes=[smk])
                P.op("act", lambda e: e.activation(out=sm[:, 8:12], in_=sm[:, 8:12], func=AF.Ln), reads=[smk], writes=[smk])
                P.op("dve", lambda e: e.tensor_tensor(out=sm[:, 4:8], in0=sm[:, 8:12], in1=arep[:], op=ALU.mult), reads=[smk, "arep"], writes=[smk])
                if valid is not None:
                    vm, vmk = valid
                    P.op("dve", lambda e: e.tensor_scalar(out=sm[:, 0:8], in0=sm[:, 0:8], scalar1=vm[:, 0:1], scalar2=None, op0=ALU.mult), reads=[smk, vmk], writes=[smk])
                pkk, pkkk = pC4_r.next()
                pqk, pqkk = pC4_r.next()
                for h in H:
                    P.op("pe", lambda e, h=h: e.matmul(out=pkk[:, h, :], lhsT=knT[:, h, sl], rhs=knT[:, h, sl], start=True, stop=True), reads=[(knTk, h)], writes=[pkkk])
                for h in H:
                    P.op("pe", lambda e, h=h: e.matmul(out=pqk[:, h, :], lhsT=qnT[:, h, sl], rhs=knT[:, h, sl], start=True, stop=True), reads=[(qnTk, h), (knTk, h)], writes=[pqkk])
                pbk_, pbkk = pCb4_r.next()
                pbv_, pbvk = pCb4_r.next()
                for h in H:
                    P.op("pe", lambda e, h=h: e.transpose(out=pbk_[:, h, :], in_=knT[:, h, sl], identity=ident[:]), reads=[(knTk, h), "ident"], writes=[pbkk])
                for h in H:
                    P.op("pe", lambda e, h=h: e.transpose(out=pbv_[:, h, :], in_=vT[:, h, sl], identity=ident[:]), reads=[(vTk, h), "ident"], writes=[pbvk])
                pd, pdk = pC4_r.next()
                P.op("pe", lambda e: e.matmul(out=pd[:, 0, 0:4], lhsT=Um_[:], rhs=sm[:, 4:8], start=True, stop=True), reads=[Uk_, smk], writes=[pdk])
                P.op("pe", lambda e: e.matmul(out=pd[:, 0, 4:8], lhsT=Bones_[:], rhs=sm[:, 4:8], start=True, stop=True), reads=[Bonesk_, smk], writes=[pdk])
                P.op("dve", lambda e: e.tensor_copy(out=sm[:, 12:20], in_=pd[:, 0, 0:8]), reads=[pdk, smk], writes=[smk])
                P.op("act", lambda e: e.activation(out=sm[:, 20:24], in_=sm[:, 12:16], func=AF.Exp), reads=[smk], writes=[smk])
                P.op("dve", lambda e: e.tensor_tensor(out=sm[:, 20:24], in0=sm[:, 20:24], in1=sm[:, 0:4], op=ALU.mult), reads=[smk], writes=[smk])
                P.op("dve", lambda e: e.tensor_tensor(out=sm[:, 24:28], in0=sm[:, 16:20], in1=sm[:, 12:16], op=ALU.subtract), reads=[smk], writes=[smk])
                P.op("act", lambda e: e.activation(out=sm[:, 24:28], in_=sm[:, 24:28], func=AF.Exp), reads=[smk], writes=[smk])
                kbd, kbdk = kbd4_r.next()
                kd, kdk = kd4_r.next()
                vbm, vbmk = vb4_r.next()
                P.op("dve", lambda e: e.tensor_tensor(out=kbd[:], in0=pbk_[:], in1=bc(sm[:, 20:24]), op=ALU.mult), reads=[pbkk, smk], writes=[kbdk])
                P.op("dve", lambda e: e.tensor_tensor(out=kd[:], in0=pbk_[:], in1=bc(sm[:, 24:28]), op=ALU.mult), reads=[pbkk, smk], writes=[kdk])
                P.op("dve", lambda e: e.tensor_tensor(out=vbm[:], in0=pbv_[:], in1=bc(sm[:, 0:4]), op=ALU.mult), reads=[pbvk, smk], writes=[vbmk])
                grep, grepk = grep4_r.next()
                P.op("pool", lambda e: e.tensor_tensor(out=grep[:], in0=ones_f4[:], in1=bc(sm[:, 4:8]), op=ALU.mult), reads=["ones_f4", smk], writes=[grepk])
                pdr, pdrk = pC4_r.next()
                for h in H:
                    P.op("pe", lambda e, h=h: e.matmul(out=pdr[:, h, :], lhsT=grep[:, h, :], rhs=Um_[:], start=True, stop=True), reads=[grepk, Uk_], writes=[pdrk])
                Erow, Erowk = Erow4_r.next()
                P.op("act", lambda e: e.activation(out=Erow[:], in_=pdr[:], func=AF.Exp), reads=[pdrk], writes=[Erowk])
                t4, t4k = t4_r.next()
                P.op("dve", lambda e: e.tensor_tensor(out=t4[:], in0=pdr[:], in1=bc(sm[:, 12:16]), op=ALU.subtract), reads=[pdrk, smk], writes=[t4k])
                P.op("act", lambda e: e.activation(out=t4[:], in_=t4[:], func=AF.Exp, scale=-1.0), reads=[t4k], writes=[t4k])
                Gs, Gsk = Gs4_r.next()
                Gi, Gik = Gi4_r.next()
                P.op("dve", lambda e: e.scalar_tensor_tensor(out=Gs[:], in0=t4[:], scalar=1.0, in1=Ms_[:].unsqueeze(1).to_broadcast([128, 4, 128]), op0=ALU.min, op1=ALU.mult),
                     reads=[t4k, Msk_], writes=[Gsk])
                P.op("dve", lambda e: e.scalar_tensor_tensor(out=Gi[:], in0=t4[:], scalar=1.0, in1=Mi_[:].unsqueeze(1).to_broadcast([128, 4, 128]), op0=ALU.min, op1=ALU.mult),
                     reads=[t4k, Mik_], writes=[Gik])
                P.op("pool", lambda e: e.tensor_tensor(out=Gs[:], in0=Gs[:], in1=bc(sm[:, 0:4]), op=ALU.mult), reads=[Gsk, smk], writes=[Gsk])
                qd, qdk = qd4_r.next()
                P.op("pool", lambda e: e.tensor_tensor(out=qd[:], in0=qnT[:, :, sl], in1=Erow[:], op=ALU.mult), reads=qk_ + [Erowk], writes=[qdk])
                Am, Amk = A4_r.next()
                at, atk = at4_r.next()
                P.op("dve", lambda e: e.tensor_tensor(out=Am[:], in0=pkk[:], in1=Gs[:], op=ALU.mult), reads=[pkkk, Gsk], writes=[Amk])
                P.op("dve", lambda e: e.tensor_tensor(out=at[:], in0=pqk[:], in1=Gi[:], op=ALU.mult), reads=[pqkk, Gik], writes=[atk])
                pbA, pbAk = pCb4_r.next()
                pbT, pbTk = pCb4_r.next()
                for h in H:
                    P.op("pe", lambda e, h=h: e.transpose(out=pbA[:, h, :], in_=Am[:, h, :], identity=ident[:]), reads=[Amk, "ident"], writes=[pbAk])
                for h in H:
                    P.op("pe", lambda e, h=h: e.transpose(out=pbT[:, h, :], in_=at[:, h, :], identity=ident[:]), reads=[atk, "ident"], writes=[pbTk])
                Bm, Bmk = Pm4_r.next()
                attnT, attnTk = attnT4_r.next()
                P.op("act", lambda e: e.copy(out=Bm[:], in_=pbA[:]), reads=[pbAk], writes=[Bmk])
                P.op("act", lambda e: e.copy(out=attnT[:], in_=pbT[:]), reads=[pbTk], writes=[attnTk])
                Rm, Rmk = R4_r.next()
                P.op("pool", lambda e, Rm=Rm: e.tensor_tensor(out=Rm[:], in0=ident[:].unsqueeze(1).to_broadcast([128, 4, 128]), in1=Bm[:], op=ALU.subtract), reads=["ident", Bmk], writes=[Rmk])
                Pc, Pck, PcT, PcTk = Bm, Bmk, Am, Amk
                nlev = 5
                for lev in range(1, nlev + 1):
                    last = lev == nlev
                    if not last:
                        pn, pnk = pC4_r.next()
                        for h in H:
                            P.op("pe", lambda e, h=h, pn=pn, Pc=Pc, PcT=PcT: e.matmul(out=pn[:, h, :], lhsT=PcT[:, h, :], rhs=Pc[:, h, :], start=True, stop=True), reads=[Pck, PcTk], writes=[pnk])
                    pnT, pnTk = pC4_r.next()
                    for h in H:
                        P.op("pe", lambda e, h=h, pnT=pnT, Pc=Pc, PcT=PcT: e.matmul(out=pnT[:, h, :], lhsT=Pc[:, h, :], rhs=PcT[:, h, :], start=True, stop=True), reads=[Pck, PcTk], writes=[pnTk])
                    PnT, PnTk = PmT4_r.next()
                    P.op("dve", lambda e, PnT=PnT, pnT=pnT: e.tensor_copy(out=PnT[:], in_=pnT[:]), reads=[pnTk], writes=[PnTk])
                    if not last:
                        Pn, Pnk = Pm4_r.next()
                        P.op("act", lambda e, Pn=Pn, pn=pn: e.copy(out=Pn[:], in_=pn[:]), reads=[pnk], writes=[Pnk])
                    pr, prk = pC4_r.next()
                    for h in H:
                        P.op("pe", lambda e, h=h, pr=pr, PnT=PnT, Rm=Rm: e.matmul(out=pr[:, h, :], lhsT=PnT[:, h, :], rhs=Rm[:, h, :], start=True, stop=True), reads=[PnTk, Rmk], writes=[prk])
                    Rn, Rnk = R4_r.next()
                    P.op("dve", lambda e, Rn=Rn, pr=pr, Rm=Rm: e.tensor_tensor(out=Rn[:], in0=pr[:], in1=Rm[:], op=ALU.add), reads=[prk, Rmk], writes=[Rnk])
                    Rm, Rmk = Rn, Rnk
                    if not last:
                        Pc, Pck = Pn, Pnk
                    PcT, PcTk = PnT, PnTk
                pw, pwk = pC4_r.next()
                pu, puk = pC4_r.next()
                for h in H:
                    P.op("pe", lambda e, h=h, Rm=Rm: e.matmul(out=pw[:, h, :], lhsT=kbd[:, h, :], rhs=Rm[:, h, :], start=True, stop=True), reads=[kbdk, Rmk], writes=[pwk])
                for h in H:
                    P.op("pe", lambda e, h=h, Rm=Rm: e.matmul(out=pu[:, h, :], lhsT=Rm[:, h, :], rhs=vbm[:, h, :], start=True, stop=True), reads=[vbmk, Rmk], writes=[puk])
                wT, wTk = wT4_r.next()
                uu, uuk = u4_r.next()
                P.op("act", lambda e: e.copy(out=wT[:], in_=pw[:]), reads=[pwk], writes=[wTk])
                P.op("dve", lambda e: e.tensor_copy(out=uu[:], in_=pu[:]), reads=[puk], writes=[uuk])
                vn, vnk = vn4_r.next()
                otok, otokk = otok_r.next()
                for bi, (rs, si) in enumerate(blocks):
                    r1 = rs.stop
                    S4, S4k, Sb4, Sb4k = states[si]
                    pW, pWk = pC4_r.next()
                    for h in H:
                        P.op("pe", lambda e, h=h, pW=pW, rs=rs, Sb4=Sb4: e.matmul(out=pW[rs, h, :], lhsT=wT[:, h, rs], rhs=Sb4[:, h, :], start=True, stop=True), reads=[wTk, Sb4k], writes=[pWk])
                    P.op("dve", lambda e, pW=pW, rs=rs: e.tensor_tensor(out=vn[rs, :, :], in0=uu[rs, :, :], in1=pW[rs, :, :], op=ALU.subtract), reads=[pWk, uuk], writes=[(vnk, bi)])
                    pO, pOk = pC4_r.next()
                    for h in H:
                        P.op("pe", lambda e, h=h, pO=pO, rs=rs, Sb4=Sb4: e.matmul(out=pO[rs, h, :], lhsT=qd[:, h, rs], rhs=Sb4[:, h, :], start=True, stop=True), reads=[qdk, Sb4k], writes=[pOk])
                    P.op("act", lambda e, pO=pO, rs=rs: e.copy(out=otok[rs, :, :], in_=pO[rs, :, :]), reads=[pOk], writes=[(otokk, bi)])
                    pO2, pO2k = pC4_r.next()
                    for h in H:
                        P.op("pe", lambda e, h=h, pO2=pO2, rs=rs: e.matmul(out=pO2[rs, h, :], lhsT=attnT[rs, h, rs], rhs=vn[rs, h, :], start=True, stop=True), reads=[attnTk, (vnk, bi)], writes=[pO2k])
                    P.op("dve", lambda e, pO2=pO2, rs=rs: e.tensor_tensor(out=otok[rs, :, :], in0=otok[rs, :, :], in1=pO2[rs, :, :], op=ALU.add), reads=[pO2k, (otokk, bi)], writes=[(otokk, bi)])
                    pN, pNk = pC4_r.next()
                    for h in H:
                        P.op("pe", lambda e, h=h, pN=pN, rs=rs: e.matmul(out=pN[:, h, :], lhsT=kd[rs, h, :], rhs=vn[rs, h, :], start=True, stop=True), reads=[kdk, (vnk, bi)], writes=[pNk])
                    P.op("dve", lambda e, S4=S4, r1=r1: e.tensor_tensor(out=S4[:], in0=S4[:], in1=Erow[:, :, r1 - 1:r1].to_broadcast([128, 4, 128]), op=ALU.mult), reads=[S4k, Erowk], writes=[S4k])
                    P.op("dve", lambda e, S4=S4, pN=pN: e.tensor_tensor(out=S4[:], in0=S4[:], in1=pN[:], op=ALU.add), reads=[S4k, pNk], writes=[S4k])
                    P.op("act", lambda e, S4=S4, Sb4=Sb4: e.copy(out=Sb4[:], in_=S4[:]), reads=[S4k], writes=[Sb4k])
                orr = [(otokk, bi) for bi in range(len(blocks))]
                sq4, sq4k = sq4_r.next()
                ss4, ss4k = ss4_r.next()
                P.op("act", lambda e: e.activation(out=sq4[:], in_=otok[:].rearrange("p h d -> p (h d)"), func=AF.Square), reads=orr, writes=[sq4k])
                P.op("dve", lambda e: e.tensor_reduce(out=ss4[:], in_=sq4[:].rearrange("p (h d) -> p h d", d=128), axis=AX.X, op=ALU.add), reads=[sq4k], writes=[ss4k])
                rstd_small(ss4[:], ss4k, 1.0 / 128)
                on, onk = on_r.next()
                P.op("dve", lambda e: e.tensor_tensor(out=on[:], in0=otok[:], in1=ss4[:].unsqueeze(2).to_broadcast([128, 4, 128]), op=ALU.mult), reads=orr + [ss4k], writes=[onk])
                P.op("pool", lambda e: e.tensor_tensor(out=on[:], in0=on[:], in1=onrep[:].unsqueeze(1).to_broadcast([128, 4, 128]), op=ALU.mult), reads=[onk, "onrep"], writes=[onk])
                zs, zsk = zs_r.next()
                P.op("act", lambda e: e.activation(out=zs[:], in_=zb[:, 0:512], func=AF.Exp, scale=-1.0), reads=[zbk], writes=[zsk])
                P.op("dve", lambda e: e.tensor_scalar(out=zs[:], in0=zs[:], scalar1=1.0, scalar2=None, op0=ALU.add), reads=[zsk], writes=[zsk])
                P.op("dve", lambda e: e.reciprocal(out=zs[:], in_=zs[:]), reads=[zsk], writes=[zsk])
                P.op("pool", lambda e: e.tensor_tensor(out=zs[:], in0=zs[:], in1=zb[:, 0:512], op=ALU.mult), reads=[zsk, zbk], writes=[zsk])
                ob, obk = ob_r.next()
                P.op("dve", lambda e: e.tensor_tensor(out=ob[:], in0=on[:].rearrange("p h d -> p (h d)"), in1=zs[:], op=ALU.mult), reads=[onk, zsk], writes=[obk])
                return ob, obk

            def conv_supertile(gin, gink_list, N, qnT, qnTk, knT, knTk, vT, vTk, sample=False, chunks=range(12)):
                def tap(c, i):
                    if sample:
                        return gin[:, c, :, i:i + 8]
                    return gin[:, c, i:i + N]

                def v3(ap2):
                    return ap2.rearrange("p (b t) -> p b t", t=8) if sample else ap2

                def dsts(dst, h):
                    if sample:
                        return dst[:, h, :].rearrange("p (b s) -> p b s", s=64)[:, :, 0:8]
                    return dst[:, h, 0:N]
                for c in chunks:
                    eng = "dve"
                    yc, yck = yc_r.next()
                    P.op(eng, lambda e, yc=yc, c=c: e.tensor_scalar(out=v3(yc[:, 0:N]), in0=tap(c, 0), scalar1=cw[:, c, 0:1], scalar2=None, op0=ALU.mult),
                         reads=gink_list + ["cw"], writes=[yck])
                    for i in range(1, 4):
                        P.op(eng, lambda e, yc=yc, c=c, i=i: e.scalar_tensor_tensor(out=v3(yc[:, 0:N]), in0=tap(c, i), scalar=cw[:, c, i:i + 1], in1=v3(yc[:, 0:N]), op0=ALU.mult, op1=ALU.add),
                             reads=gink_list + ["cw", yck], writes=[yck])
                    ex, exk = silu_from(yc, yck, N)
                    h = c % 4
                    if c >= 8:
                        P.op("pool", lambda e, yc=yc, ex=ex, h=h: e.tensor_tensor(out=dsts(vT, h), in0=v3(yc[:, 0:N]), in1=v3(ex[:, 0:N]), op=ALU.mult), reads=[yck, exk, (vTk, h)], writes=[(vTk, h)])
                        continue
                    P.op("pool", lambda e, yc=yc, ex=ex: e.tensor_tensor(out=yc[:, 0:N], in0=yc[:, 0:N], in1=ex[:, 0:N], op=ALU.mult), reads=[yck, exk], writes=[yck])
                    sqb, sqbk = sqb_r.next()
                    P.op("act", lambda e, sqb=sqb, yc=yc: e.activation(out=sqb[:, 0:N], in_=yc[:, 0:N], func=AF.Square), reads=[yck], writes=[sqbk])
                    pq, pqk_ = pC_r.next()
                    P.op("pe", lambda e, pq=pq, sqb=sqb: e.matmul(out=pq[:, 0:N], lhsT=ones_bf[:], rhs=sqb[:, 0:N], start=True, stop=True), reads=["ones_bf", sqbk], writes=[pqk_])
                    rsb, rsbk = rs_r.next()
                    mul = 128.0 if c < 4 else 1.0
                    P.op("dve", lambda e, rsb=rsb, pq=pq, mul=mul: e.tensor_scalar(out=rsb[:, 0:N], in0=pq[:, 0:N], scalar1=EPS, scalar2=mul, op0=ALU.add, op1=ALU.mult), reads=[pqk_], writes=[rsbk])
                    P.op("act", lambda e, rsb=rsb: e.activation(out=rsb[:, 0:N], in_=rsb[:, 0:N], func=AF.Sqrt), reads=[rsbk], writes=[rsbk])
                    P.op("dve", lambda e, rsb=rsb: e.reciprocal(out=rsb[:, 0:N], in_=rsb[:, 0:N]), reads=[rsbk], writes=[rsbk])
                    dst, dstk = (qnT, qnTk) if c < 4 else (knT, knTk)
                    P.op("pool", lambda e, dst=dst, yc=yc, rsb=rsb, h=h: e.tensor_tensor(out=dsts(dst, h), in0=v3(yc[:, 0:N]), in1=v3(rsb[:, 0:N]), op=ALU.mult), reads=[yck, rsbk, (dstk, h)], writes=[(dstk, h)])

            masks_p = ((Bones, Bonesk), (Um, Uk), (Mi, Mik), (Ms, Msk))
            pcp = contextlib.ExitStack()
            Cp = lambda name, shape, dtype: sb(name, shape, dtype, pcp)
            gin_r = Rot(Cp, "gin", [128, 12, 515], BF16, 2)
            qnT_r = Rot(Cp, "qnT", [128, 4, 512], BF16, 2)
            knT_r = Rot(Cp, "knT", [128, 4, 512], BF16, 2)
            vT_r = Rot(Cp, "vTc", [128, 4, 512], BF16, 2)
            zb_r = Rot(Cp, "zbc", [128, 520], F32, 2)
            obT_r = Rot(Cp, "obT", [128, 4, 512], BF16, 2)
            for s in range(n_pseq):
                P.op("pool", lambda e: e.memset(Sst4[:], 0.0), writes=["Sst4"])
                P.op("pool", lambda e: e.memset(Sbf4[:], 0.0), writes=["Sbf4"])
                states_p = [(Sst4, "Sst4", Sbf4, "Sbf4")]

                def load_gin(u):
                    g0_ = s * SEQ + u * 512
                    gin, gink = gin_r.next()
                    if u == 0:
                        P.op("pool", lambda e, gin=gin: e.memset(gin[:, :, 0:3], 0.0), writes=[(gink, "h")])
                    else:
                        P.op("sp", lambda e, gin=gin, g0_=g0_: e.dma_start(out=gin[:, :, 0:3], in_=gT_d[:, :, g0_ - 3:g0_].rearrange("c p n -> p c n")), writes=[(gink, "h")], dma=(gink, "h"))
                    P.op("sp", lambda e, gin=gin, g0_=g0_: e.dma_start(out=gin[:, :, 3:515], in_=gT_d[:, :, g0_:g0_ + 512].rearrange("c p n -> p c n")), writes=[(gink, "m")], dma=(gink, "m"))
                    return (gin, [(gink, "h"), (gink, "m")]) + qnT_r.next() + knT_r.next() + vT_r.next()

                nxt = load_gin(0)
                conv_supertile(nxt[0], nxt[1], 512, *nxt[2:])
                for u in range(SEQ // 512):
                    g0 = s * SEQ + u * 512
                    cur = nxt
                    gin, ginkl, qnT, qnTk, knT, knTk, vT, vTk = cur
                    if u + 1 < SEQ // 512:
                        nxt = load_gin(u + 1)
                    obT, obTk = obT_r.next()
                    for j in range(4):
                        sl = slice(j * 128, (j + 1) * 128)
                        zb, zbk = zb_r.next()
                        P.op("sp", lambda e, zb=zb, g0=g0, j=j: e.dma_start(out=zb[:], in_=zbg_d[g0 + j * 128:g0 + (j + 1) * 128, :]), writes=[zbk], dma=zbk)
                        ob, obk = gdn_tile(qnT, qnTk, knT, knTk, vT, vTk, sl, zb, zbk, states_p,
                                           [(slice(0, 64), 0), (slice(64, 128), 0)], masks_p)
                        pb, pbk = pCb4_r.next()
                        for c in range(4):
                            P.op("pe", lambda e, pb=pb, ob=ob, c=c: e.transpose(out=pb[:, c, :], in_=ob[:, c * 128:(c + 1) * 128], identity=ident[:]), reads=[obk, "ident"], writes=[pbk])
                        P.op("act", lambda e, pb=pb, obT=obT, sl=sl: e.copy(out=obT[:, :, sl], in_=pb[:]), reads=[pbk], writes=[(obTk, j)])
                        if u + 1 < SEQ // 512:
                            conv_supertile(nxt[0], nxt[1], 512, *nxt[2:], chunks=range(3 * j, 3 * j + 3))
                    P.op("sp", lambda e, obT=obT, g0=g0: e.dma_start(out=oT_d[4:8, :, g0:g0 + 512].rearrange("c p n -> p c n"), in_=obT[:]), reads=[(obTk, j) for j in range(4)], dma=obTk)
                for h in H:
                    P.op("sp", lambda e, h=h, s=s: e.dma_start(out=ssm_p[s, h], in_=Sst4[:, h, :]), reads=["Sst4"], dma=("ssm_st", h))
            pcp.close()
            P.barrier()
            if NS:
                assert n_sseq == 16
                vmask = Ck("vmask", [128, 1], F32)
                P.op("pool", lambda e: e.memset(vmask[:], 0.0), writes=["vmask"])
                for i in range(2):
                    P.op("pool", lambda e, i=i: e.memset(vmask[64 * i:64 * i + 8, :], 1.0), reads=["vmask"], writes=["vmask"])
                sc32 = Ck("sc32", [48, 1536], F32)
                P.op("sp", lambda e: e.dma_start(out=sc32[:], in_=state_conv.rearrange("b i f -> (b i) f")), writes=["sc32"], dma="sc32")
                gins = Ck("gins", [128, 12, 16, 11], BF16)
                for c4 in range(3):
                    pt_, ptk_ = pC_r.next()
                    for cc in range(4):
                        c = c4 * 4 + cc
                        P.op("pe", lambda e, pt_=pt_, c=c, cc=cc: e.matmul(out=pt_[:, cc * 48:(cc + 1) * 48], lhsT=sc32[0:48, c * 128:(c + 1) * 128], rhs=identf[0:48, 0:48], start=True, stop=True),
                             reads=["sc32", "identf"], writes=[ptk_])
                    P.op("dve", lambda e, pt_=pt_, c4=c4: e.tensor_copy(out=gins[:, c4 * 4:(c4 + 1) * 4, :, 0:3], in_=pt_[:, 0:192].rearrange("p (c b i) -> p c b i", c=4, i=3)),
                         reads=[ptk_], writes=[("gins", "h", c4)])
                for c in range(12):
                    P.op("sp", lambda e, c=c: e.dma_start(out=gins[:, c, :, 3:11], in_=gT_d[c, :, NP:NP + 128].rearrange("p (b t) -> p b t", t=8)), writes=[("gins", "m")], dma="ginsm")
                ginsk = [("gins", "h", c4) for c4 in range(3)] + [("gins", "m")]
                qnS = Ck("qnS", [128, 4, 1024], BF16)
                knS = Ck("knS", [128, 4, 1024], BF16)
                vS = Ck("vS", [128, 4, 1024], BF16)
                for t_, k_ in ((qnS, "qnS"), (knS, "knS"), (vS, "vS")):
                    for h in H:
                        P.op("pool", lambda e, t_=t_, h=h: e.memset(t_[:, h, :], 0.0), writes=[(k_, h)])
                conv_supertile(gins, ginsk, 128, qnS, "qnS", knS, "knS", vS, "vS", sample=True)
                Ss = [Ck("Ss4_%d" % i, [128, 4, 128], F32) for i in range(2)]
                Sbs = [Ck("Sbs4_%d" % i, [128, 4, 128], BF16) for i in range(2)]
                zbs_r = Rot(Ck, "zbs", [128, 520], F32, 2)
                for zb_, zk_ in zip(zbs_r.bufs, zbs_r.keys):
                    P.op("pool", lambda e, zb_=zb_: e.memset(zb_[:], 0.0), writes=[zk_])
                obTs = Ck("obTs", [128, 4, 128], BF16)
                for u in range(8):
                    for i in range(2):
                        for h in H:
                            P.op("sp", lambda e, u=u, i=i, h=h: e.dma_start(out=Ss[i][:, h, :], in_=state_ssm[2 * u + i, h]), writes=["Ss4_%d" % i], dma="Ss4_%d" % i)
                        P.op("act", lambda e, i=i: e.copy(out=Sbs[i][:], in_=Ss[i][:]), reads=["Ss4_%d" % i], writes=["Sbs4_%d" % i])
                    states_s = [(Ss[i], "Ss4_%d" % i, Sbs[i], "Sbs4_%d" % i) for i in range(2)]
                    zb, zbk = zbs_r.next()
                    for i in range(2):
                        b0 = NP + 8 * (2 * u + i)
                        P.op("sp", lambda e, zb=zb, i=i, b0=b0: e.dma_start(out=zb[64 * i:64 * i + 8, :], in_=zbg_d[b0:b0 + 8, :]), writes=[zbk], dma=zbk)
                    sl = slice(u * 128, (u + 1) * 128)
                    ob, obk = gdn_tile(qnS, "qnS", knS, "knS", vS, "vS", sl, zb, zbk, states_s,
                                       [(slice(64 * i, 64 * i + 64), i) for i in range(2)], masks_p, valid=(vmask, "vmask"))
                    pb, pbk = pCb4_r.next()
                    for c in range(4):
                        P.op("pe", lambda e, pb=pb, ob=ob, c=c: e.transpose(out=pb[:, c, :], in_=ob[:, c * 128:(c + 1) * 128], identity=ident[:]), reads=[obk, "ident"], writes=[pbk])
                    P.op("act", lambda e, pb=pb: e.copy(out=obTs[:], in_=pb[:]), reads=[pbk], writes=["obTs"])
                    for c in range(4):
                        P.op("sp", lambda e, u=u, c=c: e.dma_start(out=oT_d[4 + c, :, NP + 16 * u:NP + 16 * u + 16].rearrange("p (i t) -> p i t", t=8),
                                                                   in_=obTs[:, c, :].rearrange("p (i s) -> p i s", s=64)[:, :, 0:8]), reads=["obTs"], dma="obTs")
                    for i in range(2):
                        for h in H:
                            P.op("sp", lambda e, u=u, i=i, h=h: e.dma_start(out=ssm_s[2 * u + i, h], in_=Ss[i][:, h, :]), reads=["Ss4_%d" % i], dma="Sso4_%d" % i)
            pp.close()
        P.barrier()

    if "D" in stages:
        with contextlib.ExitStack() as pd:
            Dk = lambda name, shape, dtype: sb(name, shape, dtype, pd)
            G = moe_g
            TG = G // 128
            wout = Dk("wout", [128, 8, D], BF16)
            wr = Dk("wr", [128, 8, 20], BF16)
            wsto_t = Dk("wsto", [128, 8, 1024], F32)
            P.op("sp", lambda e: e.dma_start(out=wsto_t[:], in_=w_out.rearrange("(k p) n -> p k n", p=128)), writes=["wsto"], dma="wsto")
            P.op("pool", lambda e: e.tensor_copy(out=wout[:], in_=wsto_t[:]), reads=["wsto"], writes=["wout"])
            wrs = Dk("wrs", [128, 8, 20], F32)
            for k in range(8):
                P.op("sp", lambda e, k=k: e.dma_start(out=wrs[:, k, 0:4], in_=w_group[k * 128:(k + 1) * 128, :]), writes=["wrs"], dma="wrs")
                for g in range(4):
                    P.op("sp", lambda e, k=k, g=g: e.dma_start(out=wrs[:, k, 4 + 4 * g:8 + 4 * g], in_=w_er[g, k * 128:(k + 1) * 128, :]), writes=["wrs"], dma="wrs")
            P.op("pool", lambda e: e.tensor_copy(out=wr[:], in_=wrs[:]), reads=["wrs"], writes=["wr"])
            wrr = ["wr"]
            brep = Dk("brep", [128, 20], F32)
            P.op("sp", lambda e: e.dma_start(out=brep[:, 0:4], in_=b_group.partition_broadcast(128)), writes=["brep"], dma="brep")
            P.op("sp", lambda e: e.dma_start(out=brep[:, 4:20], in_=b_er.partition_broadcast(128)), writes=["brep"], dma="brep")
            oTg_r = Rot(Dk, "oTg", [128, 8, G], BF16, 2)
            xg_r = Rot(Dk, "xg", [128, D], F32, 2)
            hacc = Dk("hacc", [128, TG, D], F32)
            hnT = Dk("hnT", [128, 8, G], BF16)
            gates = Dk("gates", [128, TG, 16], F32)
            hb_r = Rot(Dk, "hb", [128, D], BF16, 2)
            junkd = Dk("junkd", [128, D], BF16)
            ssq_r = Rot(Dk, "ssqd", [128, 1], F32, 2)
            rt_r = Rot(Dk, "rt", [128, 64], F32, 2)
            wgu_r = Rot(Dk, "wgu", [128, 8, 1024], BF16, 3)
            wd_r = Rot(Dk, "wd", [128, 4, 1024], BF16, 3)
            sg_r = Rot(Dk, "sg", [128, 512], F32, 2)
            actT_r = Rot(Dk, "actT", [128, 4, G], BF16, 2)
            pp = contextlib.ExitStack()
            pH_r = Rot(lambda n, s, d: ps(n, s, d, pp), "pH", [128, 512], F32, 2)
            pG_r = Rot(lambda n, s, d: ps(n, s, d, pp), "pG", [128, 512], F32, 4)
            pT_r = Rot(lambda n, s, d: ps(n, s, d, pp), "pTd", [128, 8, 128], BF16, 1)
            pR_r = Rot(lambda n, s, d: ps(n, s, d, pp), "pR", [128, 512], F32, 1)

            def rstd_from(ssq, key, scale):
                P.op("dve", lambda e: e.tensor_scalar(out=ssq, in0=ssq, scalar1=scale, scalar2=EPS,
                                                      op0=ALU.mult, op1=ALU.add), reads=[key], writes=[key])
                P.op("act", lambda e: e.activation(out=ssq, in_=ssq, func=AF.Sqrt), reads=[key], writes=[key])
                P.op("dve", lambda e: e.reciprocal(out=ssq, in_=ssq), reads=[key], writes=[key])

            groups = [tiles[i:i + TG] for i in range(0, len(tiles), TG)]
            if os.environ.get('NGRP'):
                groups = groups[:int(os.environ['NGRP'])]
            for grp in groups:
                ng = len(grp)
                NG = ng * 128
                g0 = gtok(*grp[0])
                contiguous = all(gtok(*grp[j]) == g0 + 128 * j for j in range(ng))
                assert contiguous
                oTg, oTgk = oTg_r.next()
                nko = 8 if "C" in stages else 4
                P.op("sp", lambda e, oTg=oTg, g0=g0, NG=NG, nko=nko: e.dma_start(out=oTg[:, 0:nko, 0:NG], in_=oT_d[0:nko, :, g0:g0 + NG].rearrange("c p n -> p c n")),
                     writes=[oTgk], dma=oTgk)
                for j, (kind, t) in enumerate(grp):
                    nrows = 128 if kind == "p" else NS
                    sl = slice(j * 128, (j + 1) * 128)
                    xg, xgk = xg_r.next()
                    if nrows < 128:
                        P.op("pool", lambda e, xg=xg: e.memset(xg[:], 0.0), writes=[xgk])
                    P.op("sp", lambda e, xg=xg, kind=kind, t=t, nrows=nrows: e.dma_start(out=xg[0:nrows, :], in_=x_rows(kind, t, nrows)), writes=[xgk], dma=xgk)
                    for n2 in range(2):
                        pH, pHk = pH_r.next()
                        for k in range(nko):
                            P.op("pe", lambda e, pH=pH, oTg=oTg, k=k, sl=sl, n2=n2, nko=nko: e.matmul(
                                out=pH[:], lhsT=oTg[:, k, sl], rhs=wout[:, k, n2 * 512:(n2 + 1) * 512], start=(k == 0), stop=(k == nko - 1)),
                                reads=[oTgk, "wout"], writes=[pHk])
                        P.op("dve", lambda e, pH=pH, xg=xg, j=j, n2=n2: e.tensor_tensor(out=hacc[:, j, n2 * 512:(n2 + 1) * 512], in0=pH[:], in1=xg[:, n2 * 512:(n2 + 1) * 512], op=ALU.add),
                             reads=[pHk, xgk], writes=[("hacc", j, n2)])
                    hr = [("hacc", j, 0), ("hacc", j, 1)]
                    if debug:
                        P.op("sp", lambda e, j=j, gtt=gtok(kind, t): e.dma_start(out=dbg_h[gtt:gtt + 128, :], in_=hacc[:, j, :]), reads=hr, dma="dbg_h")
                    ssq, ssqk = ssq_r.next()
                    P.op("act", lambda e, j=j, ssq=ssq: e.activation(out=junkd[:], in_=hacc[:, j, :], func=AF.Square, accum_out=ssq[:]),
                         reads=hr, writes=["junkd", ssqk])
                    rstd_from(ssq[:], ssqk, 1.0 / D)
                    hb, hbk = hb_r.next()
                    P.op("dve", lambda e, hb=hb, j=j, ssq=ssq: e.tensor_scalar(out=hb[:], in0=hacc[:, j, :], scalar1=ssq[:], scalar2=None, op0=ALU.mult),
                         reads=hr + [ssqk], writes=[hbk])
                    pT, pTk = pT_r.next()
                    for k in range(8):
                        P.op("pe", lambda e, pT=pT, hb=hb, k=k: e.transpose(out=pT[:, k, :], in_=hb[:, k * 128:(k + 1) * 128], identity=ident[:]),
                             reads=[hbk, "ident"], writes=[pTk])
                    P.op("dve", lambda e, pT=pT, sl=sl: e.tensor_tensor(out=hnT[:, :, sl], in0=pT[:], in1=n2col[:].unsqueeze(2).to_broadcast([128, 8, 128]), op=ALU.mult),
                         reads=[pTk, "n2col"], writes=[("hnT", j)])
                    pR, pRk = pR_r.next()
                    for k in range(8):
                        P.op("pe", lambda e, pR=pR, k=k, sl=sl: e.matmul(out=pR[:, 0:20], lhsT=hnT[:, k, sl], rhs=wr[:, k, :], start=(k == 0), stop=(k == 7)),
                             reads=[("hnT", j)] + wrr, writes=[pRk])
                    rt, rtk = rt_r.next()
                    V = lambda a, b, rt=rt: rt[:, a:b]

                    def dv(fn, rtk=rtk):
                        P.op("dve", fn, reads=[rtk], writes=[rtk])
                    P.op("dve", lambda e, rt=rt, pR=pR: e.tensor_tensor(out=rt[:, 0:20], in0=pR[:, 0:20], in1=brep[:], op=ALU.add),
                         reads=[pRk, "brep"], writes=[rtk])
                    dv(lambda e, V=V: e.tensor_reduce(out=V(20, 21), in_=V(0, 4), axis=AX.X, op=ALU.max))
                    dv(lambda e, V=V: e.tensor_scalar(out=V(21, 25), in0=V(0, 4), scalar1=V(20, 21), scalar2=None, op0=ALU.is_ge))
                    dv(lambda e, V=V: e.tensor_scalar(out=V(25, 29), in0=V(0, 4), scalar1=V(20, 21), scalar2=None, op0=ALU.subtract))
                    P.op("act", lambda e, V=V: e.activation(out=V(25, 29), in_=V(25, 29), func=AF.Exp, accum_out=V(29, 30)), reads=[rtk], writes=[rtk])
                    dv(lambda e, V=V: e.reciprocal(out=V(30, 31), in_=V(29, 30)))
                    dv(lambda e, V=V, rt=rt: e.tensor_tensor(out=rt[:, 31:47].rearrange("p (g x) -> p g x", x=4), in0=rt[:, 4:20].rearrange("p (g x) -> p g x", x=4),
                                                              in1=rt[:, 21:25].unsqueeze(2).to_broadcast([128, 4, 4]), op=ALU.mult))
                    dv(lambda e, V=V, rt=rt: e.tensor_reduce(out=V(47, 51), in_=rt[:, 31:47].rearrange("p (g x) -> p x g", x=4), axis=AX.X, op=ALU.add))
                    dv(lambda e, V=V: e.tensor_reduce(out=V(51, 52), in_=V(47, 51), axis=AX.X, op=ALU.max))
                    dv(lambda e, V=V: e.tensor_scalar(out=V(52, 56), in0=V(47, 51), scalar1=V(51, 52), scalar2=None, op0=ALU.is_ge))
                    dv(lambda e, V=V: e.scalar_tensor_tensor(out=V(56, 60), in0=V(52, 56), scalar=-1e30, in1=V(47, 51), op0=ALU.mult, op1=ALU.add))
                    dv(lambda e, V=V: e.tensor_reduce(out=V(60, 61), in_=V(56, 60), axis=AX.X, op=ALU.max))
                    dv(lambda e, V=V: e.tensor_scalar(out=V(56, 60), in0=V(56, 60), scalar1=V(60, 61), scalar2=None, op0=ALU.is_ge))
                    dv(lambda e, V=V: e.tensor_tensor(out=V(61, 62), in0=V(60, 61), in1=V(51, 52), op=ALU.subtract))
                    P.op("act", lambda e, V=V: e.activation(out=V(62, 63), in_=V(61, 62), func=AF.Exp), reads=[rtk], writes=[rtk])
                    dv(lambda e, V=V: e.tensor_scalar(out=V(63, 64), in0=V(62, 63), scalar1=1.0, scalar2=None, op0=ALU.add))
                    dv(lambda e, V=V: e.reciprocal(out=V(63, 64), in_=V(63, 64)))
                    dv(lambda e, V=V: e.tensor_tensor(out=V(63, 64), in0=V(63, 64), in1=V(30, 31), op=ALU.mult))
                    dv(lambda e, V=V: e.tensor_tensor(out=V(62, 63), in0=V(62, 63), in1=V(63, 64), op=ALU.mult))
                    dv(lambda e, V=V: e.tensor_scalar(out=V(52, 56), in0=V(52, 56), scalar1=V(63, 64), scalar2=None, op0=ALU.mult))
                    dv(lambda e, V=V: e.scalar_tensor_tensor(out=V(52, 56), in0=V(56, 60), scalar=V(62, 63), in1=V(52, 56), op0=ALU.mult, op1=ALU.add))
                    for g in range(4):
                        P.op("dve", lambda e, V=V, j=j, g=g: e.tensor_scalar(out=gates[:, j, 4 * g:4 * g + 4], in0=V(52, 56), scalar1=V(21 + g, 22 + g), scalar2=None, op0=ALU.mult),
                             reads=[rtk], writes=[("gates", j, g)])
                gr = [("gates", j, g) for j in range(ng) for g in range(4)]
                hnr = [("hnT", j) for j in range(ng)]
                if debug:
                    P.op("sp", lambda e, g0=g0, ng=ng: e.dma_start(out=dbg_g[g0 // 128:g0 // 128 + ng].rearrange("t p x -> p t x"), in_=gates[:, 0:ng, :]), reads=gr, dma="dbg_g")
                    P.op("sp", lambda e, g0=g0, NG=NG: e.dma_start(out=dbg_hn[:, :, g0:g0 + NG].rearrange("c p n -> p c n"), in_=hnT[:, :, 0:NG]), reads=hnr, dma="dbg_hn")
                def emit_gu(ex):
                    wgu, wguk = wgu_r.next()
                    wd, wdk = wd_r.next()
                    P.op("sp", lambda e, wgu=wgu, ex=ex: e.dma_start(out=wgu[:], in_=wgu_bf[ex].rearrange("(k p) n -> p k n", p=128)), reads=["wbf"], writes=[wguk], dma=wguk)
                    P.op("sp", lambda e, wd=wd, ex=ex: e.dma_start(out=wd[:], in_=wd_bf[ex].rearrange("(k p) n -> p k n", p=128)), reads=["wbf"], writes=[wdk], dma=wdk)
                    aT, aTk = actT_r.next()
                    for n0 in range(0, NG, 512):
                        nn = min(512, NG - n0)
                        for f in range(4):
                            pGa, pGak = pG_r.next()
                            pGb, pGbk = pG_r.next()
                            for k in range(8):
                                P.op("pe", lambda e, pGa=pGa, wgu=wgu, k=k, f=f, n0=n0, nn=nn: e.matmul(
                                    out=pGa[:, 0:nn], lhsT=wgu[:, k, f * 128:(f + 1) * 128], rhs=hnT[:, k, n0:n0 + nn], start=(k == 0), stop=(k == 7)),
                                    reads=[wguk] + hnr, writes=[pGak])
                            for k in range(8):
                                P.op("pe", lambda e, pGb=pGb, wgu=wgu, k=k, f=f, n0=n0, nn=nn: e.matmul(
                                    out=pGb[:, 0:nn], lhsT=wgu[:, k, 512 + f * 128:512 + (f + 1) * 128], rhs=hnT[:, k, n0:n0 + nn], start=(k == 0), stop=(k == 7)),
                                    reads=[wguk] + hnr, writes=[pGbk])
                            sg, sgk = sg_r.next()
                            P.op("act", lambda e, sg=sg, pGa=pGa, nn=nn: e.activation(out=sg[:, 0:nn], in_=pGa[:, 0:nn], func=AF.Exp, scale=-1.0), reads=[pGak], writes=[sgk])
                            P.op("dve", lambda e, sg=sg, nn=nn: e.tensor_scalar(out=sg[:, 0:nn], in0=sg[:, 0:nn], scalar1=1.0, scalar2=None, op0=ALU.add), reads=[sgk], writes=[sgk])
                            P.op("dve", lambda e, sg=sg, nn=nn: e.reciprocal(out=sg[:, 0:nn], in_=sg[:, 0:nn]), reads=[sgk], writes=[sgk])
                            P.op("dve", lambda e, sg=sg, pGa=pGa, nn=nn: e.tensor_tensor(out=sg[:, 0:nn], in0=sg[:, 0:nn], in1=pGa[:, 0:nn], op=ALU.mult), reads=[sgk, pGak], writes=[sgk])
                            P.op("dve", lambda e, sg=sg, pGb=pGb, aT=aT, f=f, n0=n0, nn=nn: e.tensor_tensor(out=aT[:, f, n0:n0 + nn], in0=sg[:, 0:nn], in1=pGb[:, 0:nn], op=ALU.mult),
                                 reads=[sgk, pGbk], writes=[(aTk, f, n0)])
                    return wd, wdk, aT, aTk

                def emit_down(ex, wd, wdk, aT, aTk):
                    ar = [(aTk, f, n0) for f in range(4) for n0 in range(0, NG, 512)]
                    for j in range(ng):
                        sl = slice(j * 128, (j + 1) * 128)
                        for n2 in range(2):
                            pH, pHk = pH_r.next()
                            for f in range(4):
                                P.op("pe", lambda e, pH=pH, wd=wd, aT=aT, f=f, sl=sl, n2=n2: e.matmul(
                                    out=pH[:], lhsT=aT[:, f, sl], rhs=wd[:, f, n2 * 512:(n2 + 1) * 512], start=(f == 0), stop=(f == 3)),
                                    reads=ar + [wdk], writes=[pHk])
                            P.op("dve", lambda e, pH=pH, j=j, n2=n2, ex=ex: e.scalar_tensor_tensor(
                                out=hacc[:, j, n2 * 512:(n2 + 1) * 512], in0=pH[:], scalar=gates[:, j, ex:ex + 1], in1=hacc[:, j, n2 * 512:(n2 + 1) * 512],
                                op0=ALU.mult, op1=ALU.add), reads=[pHk, ("hacc", j, n2)] + gr, writes=[("hacc", j, n2)])

                n_ex = int(os.environ.get('NEXP', n_exp))
                pend = emit_gu(0) if n_ex else None
                for ex in range(n_ex):
                    cur_ = pend
                    if ex + 1 < n_ex:
                        pend = emit_gu(ex + 1)
                    emit_down(ex, *cur_)
                for j, (kind, t) in enumerate(grp):
                    if kind == "p":
                        P.op("sp", lambda e, j=j, t=t: e.dma_start(out=y_p[t * 128:(t + 1) * 128, :], in_=hacc[:, j, :]),
                             reads=[("hacc", j, 0), ("hacc", j, 1)], dma=("ysto", j))
                    else:
                        P.op("sp", lambda e, j=j: e.dma_start(out=y_s[0:NS, :], in_=hacc[0:NS, j, :]),
                             reads=[("hacc", j, 0), ("hacc", j, 1)], dma=("ysto", j))
            pp.close()
        P.barrier()

    print('semaphores:', len(P.semkeys), 'ops:', {e: len(P.ops[e]) for e in ENGS}, flush=True)
    P.emit()
    es.close()
    return nc


N_CORES = 8
_cache = {}


def kernel(x_prompt, x_sample, cache_win_k, cache_win_v, state_conv, state_ssm, norm1_w, w_in,
           qnorm_w, knorm_w, conv_w, a_log, dt_bias, onorm_w, w_out, norm2_w, w_group, b_group,
           w_expert_router, b_expert_router, w_gate_up, w_down):
    f = lambda a: np.ascontiguousarray(np.asarray(a, dtype=np.float32))
    B, S, _ = x_prompt.shape
    DB = x_sample.shape[0]
    n_pseq = B // N_CORES
    n_sseq = DB // N_CORES
    key = (n_pseq, n_sseq)
    if key not in _cache:
        _cache[key] = build_program(n_pseq, n_sseq, stages=("A", "B", "E", "C", "D"))
    nc = _cache[key]
    shared = dict(norm1_w=f(norm1_w), w_in=f(w_in), qnorm_w=f(qnorm_w), knorm_w=f(knorm_w), conv_w=f(conv_w),
                  a_log=f(a_log), dt_bias=f(dt_bias), onorm_w=f(onorm_w), w_out=f(w_out), norm2_w=f(norm2_w),
                  w_group=f(w_group), b_group=f(b_group), w_er=f(w_expert_router), b_er=f(b_expert_router).reshape(16),
                  w_gate_up=f(w_gate_up), w_down=f(w_down))
    in_maps = []
    for c in range(N_CORES):
        m = dict(shared)
        m["xp"] = f(x_prompt[c * n_pseq:(c + 1) * n_pseq]).reshape(n_pseq * S, D)
        m["xs"] = f(x_sample[c * n_sseq:(c + 1) * n_sseq]).reshape(n_sseq * 8, D)
        m["cache_k"] = f(cache_win_k[c * n_sseq:(c + 1) * n_sseq]).reshape(n_sseq, 2048, 512)
        m["cache_v"] = f(cache_win_v[c * n_sseq:(c + 1) * n_sseq]).reshape(n_sseq, 2048, 512)
        m["state_conv"] = f(state_conv[c * n_sseq:(c + 1) * n_sseq])
        m["state_ssm"] = f(state_ssm[c * n_sseq:(c + 1) * n_sseq])
        in_maps.append(m)
    res = run_bass_kernel_spmd(nc, in_maps, core_ids=list(range(N_CORES)))
    R = res.results
    cat = lambda name: np.concatenate([np.asarray(r[name]) for r in R], axis=0)
    y_prompt = cat("y_p").reshape(B, S, D)
    y_sample = cat("y_s").reshape(DB, 8, D)
    wk_p = cat("wk_p").reshape(B, 2048, 8, 64)
    wv_p = cat("wv_p").reshape(B, 2048, 8, 64)
    conv_p = cat("conv_p").reshape(B, 3, 1536)
    ssm_p = cat("ssm_p").reshape(B, 4, 128, 128)
    wk_s = cat("wk_s").reshape(DB, 8, 8, 64)
    wv_s = cat("wv_s").reshape(DB, 8, 8, 64)
    conv_s = cat("conv_s").reshape(DB, 3, 1536)
    ssm_s = cat("ssm_s").reshape(DB, 4, 128, 128)
    return (y_prompt, y_sample, wk_p, wv_p, conv_p, ssm_p, wk_s, wv_s, conv_s, ssm_s)
```
